# Optimizing a Trainium2 kernel written in Bass

```python
import jax, jax.numpy as jnp
from jax import lax
import numpy as np

D_MODEL = 1024
BATCH = 2
SEQ = 8192
DEPTH = 2

GRID_W = 64
POOL_W = D_MODEL // 4
N_POOL_GROUPS = 4
POOL_GROUP_W = POOL_W // N_POOL_GROUPS
POOL_WINDOWS = (2, 4, 8, 16)
NA_HEAD_DIM = 64
NA_W = D_MODEL // 2
NA_HEADS = NA_W // NA_HEAD_DIM
NA_ROWS = 8
NA_COLS = 16
NA_QR = 2
NA_QC = 16
CONV_W = D_MODEL // 4
CONV_K = 31
D_MIX = POOL_W + NA_W + CONV_W
OFF_POOL = 0
OFF_Q = OFF_POOL + POOL_W
OFF_K = OFF_Q + NA_W
OFF_V = OFF_K + NA_W
OFF_CA = OFF_V + NA_W
OFF_CG = OFF_CA + CONV_W
D_IN = OFF_CG + CONV_W
N_EXPERTS = 32
TOP_K = 4
D_FF = D_MODEL
SWIGLU_ALPHA = 1.702
SWIGLU_LIMIT = 7.0
DEEPNORM_ALPHA = (2.0 * DEPTH) ** 0.25
DEEPNORM_BETA = (8.0 * DEPTH) ** -0.25
LN_EPS = 1e-5
NEG_INF = -1e30

kernel_name = "hybrid_pool_natten_conformer_moe_deepnorm"


def _layer_norm(x, g, b):
    xf = x.astype(jnp.float32)
    mu = jnp.mean(xf, axis=-1, keepdims=True)
    var = jnp.mean(jnp.square(xf - mu), axis=-1, keepdims=True)
    y = (xf - mu) * lax.rsqrt(var + LN_EPS)
    return (y * g + b).astype(x.dtype)


def _pool_mixer(u, w_pool, scale):
    B, S, C = u.shape
    uf = u.astype(jnp.float32)
    cs = jnp.concatenate([jnp.zeros((B, 1, C), jnp.float32), jnp.cumsum(uf, axis=1)], axis=1)
    t = np.arange(S)
    outs = []
    for g, w in enumerate(POOL_WINDOWS):
        sl = slice(g * POOL_GROUP_W, (g + 1) * POOL_GROUP_W)
        lo = np.clip(t - w // 2, 0, S)
        hi = np.clip(t + w // 2, 0, S)
        cnt = (hi - lo).astype(np.float32)
        mean = (cs[:, hi, sl] - cs[:, lo, sl]) / cnt[None, :, None]
        d = (mean - uf[..., sl]).astype(u.dtype)
        outs.append(jnp.einsum('bsc,cd->bsd', d, w_pool[g]))
    return jnp.concatenate(outs, axis=-1) * scale


def _na_tables(rows):
    kr = min(NA_ROWS, rows)
    band_r = min(rows, kr + NA_QR - 1)
    band_c = min(GRID_W, NA_COLS + NA_QC - 1)
    r0 = np.arange(0, rows, NA_QR)
    c0 = np.arange(0, GRID_W, NA_QC)
    br = np.clip(r0 - kr // 2, 0, rows - band_r)
    bc = np.clip(c0 - NA_COLS // 2, 0, GRID_W - band_c)
    qr, qc = np.broadcast_arrays(r0[:, None, None, None] + np.arange(NA_QR)[None, None, :, None],
                                 c0[None, :, None, None] + np.arange(NA_QC)[None, None, None, :])
    qr = qr.reshape(-1, NA_QR * NA_QC)
    qc = qc.reshape(-1, NA_QR * NA_QC)
    kr_, kc_ = np.broadcast_arrays(br[:, None, None, None] + np.arange(band_r)[None, None, :, None],
                                   bc[None, :, None, None] + np.arange(band_c)[None, None, None, :])
    kr_ = kr_.reshape(-1, band_r * band_c)
    kc_ = kc_.reshape(-1, band_r * band_c)
    sr = np.clip(qr - kr // 2, 0, rows - kr)[..., None]
    sc = np.clip(qc - NA_COLS // 2, 0, GRID_W - NA_COLS)[..., None]
    krk = kr_[:, None, :]
    kck = kc_[:, None, :]
    mask = (krk >= sr) & (krk < sr + kr) & (kck >= sc) & (kck < sc + NA_COLS)
    r_off = np.clip(krk - qr[..., None] + NA_ROWS - 1, 0, 2 * NA_ROWS - 2)
    c_off = np.clip(kck - qc[..., None] + NA_COLS - 1, 0, 2 * NA_COLS - 2)
    q_idx = qr * GRID_W + qc
    k_idx = kr_ * GRID_W + kc_
    inv = np.argsort(q_idx.reshape(-1))
    return q_idx, k_idx, r_off, c_off, mask, inv


def _neighbourhood_attention(q, k, v, rpb):
    B, S, H, Dh = q.shape
    rows = S // GRID_W
    q_idx, k_idx, r_off, c_off, mask, inv = _na_tables(rows)
    qb = jnp.take(q, q_idx, axis=1)
    kb = jnp.take(k, k_idx, axis=1)
    vb = jnp.take(v, k_idx, axis=1)
    bias = jnp.where(mask, rpb[:, r_off, c_off].astype(jnp.float32), NEG_INF)
    s = jnp.einsum('bnqhd,bnkhd->bhnqk', qb, kb).astype(jnp.float32) * (Dh ** -0.5) + bias
    p = jax.nn.softmax(s, axis=-1).astype(v.dtype)
    o = jnp.einsum('bhnqk,bnkhd->bnqhd', p, vb).reshape(B, S, H * Dh)
    return jnp.take(o, inv, axis=1)


def _conv_module(a, gate, w_dw, b_dw, ln_g, ln_b, w_pw, b_pw):
    h = a * jax.nn.sigmoid(gate)
    h = lax.conv_general_dilated(h, w_dw, window_strides=(1,),
                                 padding=[(CONV_K // 2, CONV_K // 2)],
                                 dimension_numbers=('NWC', 'WIO', 'NWC'),
                                 feature_group_count=h.shape[-1]) + b_dw
    h = jax.nn.silu(_layer_norm(h, ln_g, ln_b))
    return h @ w_pw + b_pw


def _moe(h, w_router, b_router, w_gate, b_gate, w_up, b_up, w_down, b_down):
    B, S, D = h.shape
    hf = h.reshape(-1, D)
    logits = (hf @ w_router + b_router).astype(jnp.float32)
    top_v, top_i = lax.top_k(logits, TOP_K)
    probs = jax.nn.softmax(top_v, axis=-1)
    gates = jnp.sum(jax.nn.one_hot(top_i, N_EXPERTS, dtype=jnp.float32) * probs[..., None], axis=1).astype(h.dtype)
    out = jnp.zeros_like(hf)
    for e in range(N_EXPERTS):
        g = jnp.minimum(hf @ w_gate[e] + b_gate[e], SWIGLU_LIMIT)
        u = jnp.clip(hf @ w_up[e] + b_up[e], -SWIGLU_LIMIT, SWIGLU_LIMIT)
        act = (u + 1.0) * g * jax.nn.sigmoid(SWIGLU_ALPHA * g)
        out = out + gates[:, e:e + 1] * (act @ w_down[e] + b_down[e])
    return out.reshape(B, S, D)


def setup_inputs(seed: int = 0) -> dict:
    key = jax.random.key(seed)
    ks = jax.random.split(key, 26)

    def nrm(k, shape, scale):
        return jax.random.normal(k, shape, jnp.float32) * scale

    col_scale = np.ones((D_IN,), np.float32)
    col_scale[OFF_V:OFF_V + NA_W] = DEEPNORM_BETA
    return {
        "x": nrm(ks[0], (BATCH, SEQ, D_MODEL), 1.0),
        "w_in": nrm(ks[1], (DEPTH, D_MODEL, D_IN), D_MODEL ** -0.5) * jnp.asarray(col_scale),
        "b_in": nrm(ks[2], (DEPTH, D_IN), 0.02),
        "w_pool": nrm(ks[3], (DEPTH, N_POOL_GROUPS, POOL_GROUP_W, POOL_GROUP_W), POOL_GROUP_W ** -0.5),
        "pool_scale": 1.0 + nrm(ks[4], (DEPTH, POOL_W), 0.02),
        "rpb": nrm(ks[5], (DEPTH, NA_HEADS, 2 * NA_ROWS - 1, 2 * NA_COLS - 1), 0.1),
        "conv_dw": nrm(ks[6], (DEPTH, CONV_K, 1, CONV_W), CONV_K ** -0.5),
        "conv_dw_b": nrm(ks[7], (DEPTH, CONV_W), 0.02),
        "conv_ln_g": 1.0 + nrm(ks[8], (DEPTH, CONV_W), 0.02),
        "conv_ln_b": nrm(ks[9], (DEPTH, CONV_W), 0.02),
        "w_conv_pw": nrm(ks[10], (DEPTH, CONV_W, CONV_W), CONV_W ** -0.5),
        "b_conv_pw": nrm(ks[11], (DEPTH, CONV_W), 0.02),
        "w_out": nrm(ks[12], (DEPTH, D_MIX, D_MODEL), (D_MIX ** -0.5) * DEEPNORM_BETA),
        "b_out": nrm(ks[13], (DEPTH, D_MODEL), 0.02),
        "ln1_g": 1.0 + nrm(ks[14], (DEPTH, D_MODEL), 0.02),
        "ln1_b": nrm(ks[15], (DEPTH, D_MODEL), 0.02),
        "w_router": nrm(ks[16], (DEPTH, D_MODEL, N_EXPERTS), D_MODEL ** -0.5),
        "b_router": nrm(ks[17], (DEPTH, N_EXPERTS), 0.01),
        "w_gate": nrm(ks[18], (DEPTH, N_EXPERTS, D_MODEL, D_FF), D_MODEL ** -0.5),
        "b_gate": nrm(ks[19], (DEPTH, N_EXPERTS, D_FF), 0.02),
        "w_up": nrm(ks[20], (DEPTH, N_EXPERTS, D_MODEL, D_FF), D_MODEL ** -0.5),
        "b_up": nrm(ks[21], (DEPTH, N_EXPERTS, D_FF), 0.02),
        "w_down": nrm(ks[22], (DEPTH, N_EXPERTS, D_FF, D_MODEL), (D_FF ** -0.5) * DEEPNORM_BETA),
        "b_down": nrm(ks[23], (DEPTH, N_EXPERTS, D_MODEL), 0.02),
        "ln2_g": 1.0 + nrm(ks[24], (DEPTH, D_MODEL), 0.02),
        "ln2_b": nrm(ks[25], (DEPTH, D_MODEL), 0.02),
    }


def reference(x, w_in, b_in, w_pool, pool_scale, rpb, conv_dw, conv_dw_b, conv_ln_g, conv_ln_b,
              w_conv_pw, b_conv_pw, w_out, b_out, ln1_g, ln1_b, w_router, b_router,
              w_gate, b_gate, w_up, b_up, w_down, b_down, ln2_g, ln2_b):
    B, S, _ = x.shape
    for l in range(DEPTH):
        z = x @ w_in[l] + b_in[l]
        q = z[..., OFF_Q:OFF_K].reshape(B, S, NA_HEADS, NA_HEAD_DIM)
        k = z[..., OFF_K:OFF_V].reshape(B, S, NA_HEADS, NA_HEAD_DIM)
        v = z[..., OFF_V:OFF_CA].reshape(B, S, NA_HEADS, NA_HEAD_DIM)
        y_pool = _pool_mixer(z[..., OFF_POOL:OFF_Q], w_pool[l], pool_scale[l])
        y_attn = _neighbourhood_attention(q, k, v, rpb[l])
        y_conv = _conv_module(z[..., OFF_CA:OFF_CG], z[..., OFF_CG:D_IN], conv_dw[l], conv_dw_b[l],
                              conv_ln_g[l], conv_ln_b[l], w_conv_pw[l], b_conv_pw[l])
        mix = jnp.concatenate([y_pool, y_attn, y_conv], axis=-1) @ w_out[l] + b_out[l]
        x = _layer_norm(DEEPNORM_ALPHA * x + mix, ln1_g[l], ln1_b[l])
        ffn = _moe(x, w_router[l], b_router[l], w_gate[l], b_gate[l], w_up[l], b_up[l], w_down[l], b_down[l])
        x = _layer_norm(DEEPNORM_ALPHA * x + ffn, ln2_g[l], ln2_b[l])
    return x
```

```python
import numpy as np
import concourse.bass as bass
import concourse.mybir as mybir
from concourse.bass_utils import run_bass_kernel_spmd

F32 = mybir.dt.float32
BF16 = mybir.dt.bfloat16
I32 = mybir.dt.int32
AF = mybir.ActivationFunctionType
ALU = mybir.AluOpType

D = 1024
NE = 32
S_OWN = 2048
HALO = 256
GRID_W = 64
ALPHA = (2.0 * 2) ** 0.25
EPS = 1e-5
NEG = -1e30
DIN = 2304


class Buf:
    __slots__ = ("name", "ws", "reads", "sem", "cum")

    def __init__(self, name):
        self.name = name
        self.ws = []
        self.reads = []
        self.sem = None
        self.cum = 0


class Op:
    __slots__ = ("eng", "fn", "deps", "is_dma", "sem", "val", "signal", "seq")

    def __init__(self, eng, fn, is_dma):
        self.seq = 0
        self.eng = eng
        self.fn = fn
        self.deps = []
        self.is_dma = is_dma
        self.sem = None
        self.val = 0
        self.signal = False


class Prog:
    ENG = ("sync", "act", "dve", "pe", "pool")

    def __init__(self, nc):
        self.nc = nc
        self.ops = {e: [] for e in self.ENG}
        self.dma_sems = []
        self.fence_k = {}
        self.seq = 0
        self.marks = {}
        self.limit = None
        self.env = {"bc_vals": {}}

    def mark(self, name):
        self.marks[name] = self.seq

    def buf(self, name, scope=None):
        b = Buf(name)
        b.reads = list(self.fence_k.values())
        if scope is not None:
            scope.append(b)
        return b

    def close(self, scope_bufs):
        for b in scope_bufs:
            for o in list(b.ws) + list(b.reads):
                if o.is_dma:
                    k = ("dma", id(o.sem))
                    if k not in self.fence_k or self.fence_k[k].val < o.val:
                        self.fence_k[k] = o
                else:
                    k = ("eng", o.eng)
                    if k not in self.fence_k or self.fence_k[k].seq < o.seq:
                        self.fence_k[k] = o

    def _deps(self, op, reads, writes, indep=False):
        deps = []
        for b in reads:
            deps.extend(b.ws)
        for b in writes:
            if not indep:
                deps.extend(b.ws)
            deps.extend(b.reads)
        seen = set()
        for d in deps:
            if d.is_dma or op.is_dma or d.eng != op.eng or op.eng != "pe":
                if id(d) not in seen:
                    seen.add(id(d))
                    op.deps.append(d)
                d.signal = True
        for b in writes:
            if indep:
                b.ws.append(op)
            else:
                b.ws = [op]
                b.reads = []
        for b in reads:
            if op not in b.ws:
                b.reads.append(op)

    def op(self, eng, fn, reads=(), writes=(), indep=False):
        o = Op(eng, fn, False)
        self.seq += 1
        o.seq = self.seq
        self._deps(o, reads, writes, indep)
        self.ops[eng].append(o)
        return o

    def dma(self, eng, fn, reads=(), writes=(), sembuf=None, indep=False):
        o = Op(eng, fn, True)
        self.seq += 1
        o.seq = self.seq
        sb = sembuf if sembuf is not None else writes[0]
        if sb.sem is None:
            sb.sem = ("dma", len(self.dma_sems))
            self.dma_sems.append(sb)
        sb.cum += 16
        o.sem = sb
        o.val = sb.cum
        o.signal = True
        self._deps(o, reads, writes, indep)
        self.ops[eng].append(o)
        return o

    def emit(self, final_waits):
        nc = self.nc
        if self.limit is not None:
            lim = self.marks.get(self.limit, self.limit)
            lim = int(lim)
            for e in self.ENG:
                self.ops[e] = [o for o in self.ops[e] if o.seq <= lim]
            final_waits = [o for e in self.ENG for o in self.ops[e] if o.is_dma]
            for e in self.ENG:
                if self.ops[e] and not self.ops[e][-1].is_dma:
                    self.ops[e][-1].signal = True
                    final_waits.append(self.ops[e][-1])
        for e in self.ENG:
            c = 0
            for o in self.ops[e]:
                if not o.is_dma and o.signal:
                    c += 1
                    o.val = c
        import contextlib
        with contextlib.ExitStack() as st:
            esem = {e: st.enter_context(nc.semaphore("es_" + e)) for e in self.ENG}
            dsem = [st.enter_context(nc.semaphore("ds_%d" % i)) for i in range(len(self.dma_sems))]
            block = st.enter_context(nc.Block())

            def semof(o):
                if o.is_dma:
                    return dsem[o.sem.sem[1]]
                return esem[o.eng]

            def run(e, eng):
                waited = {}
                if e == "pool":
                    for L, v in self.env["bc_vals"].items():
                        self.env["bc_reg%d" % L] = eng.alloc_register("bc_reg%d" % L)
                        eng.reg_mov(self.env["bc_reg%d" % L], int(v))
                for o in self.ops[e]:
                    need = {}
                    for d in o.deps:
                        s = semof(d)
                        k = id(s)
                        if k not in need or d.val > need[k][1]:
                            need[k] = (s, d.val)
                    for k, (s, v) in need.items():
                        if waited.get(k, 0) >= v:
                            continue
                        waited[k] = v
                        eng.wait_ge(s, v)
                    ins = o.fn(eng)
                    if o.is_dma:
                        ins.then_inc(semof(o), 16)
                    elif o.signal:
                        ins.then_inc(esem[e], 1)
                if e == "sync":
                    for o in final_waits:
                        eng.wait_ge(semof(o), o.val)

            block.sync(lambda eng: run("sync", eng))
            block.scalar(lambda eng: run("act", eng))
            block.vector(lambda eng: run("dve", eng))
            block.tensor(lambda eng: run("pe", eng))
            block.gpsimd(lambda eng: run("pool", eng))


def build_program(stage="full", limit=None):
    nc = bass.Bass("TRN2", target_bir_lowering=False)
    P = Prog(nc)
    P.limit = limit
    P.env["bc_vals"] = {}
    import contextlib
    with contextlib.ExitStack() as glob:
        _names = {}

        def sb_global(name, shape, dt=F32, st=glob):
            k = _names.get(name, 0)
            _names[name] = k + 1
            if k:
                name = "%s_%d" % (name, k)
            return st.enter_context(nc.sbuf_tensor(name, list(shape), dt))

        sb = sb_global
        banks = [glob.enter_context(nc.psum_tensor("bank%d" % i, [128, 512], F32)) for i in range(8)]
        bankb = [Buf("bank%d" % i) for i in range(8)]

        ident_f = sb("ident_f", [128, 128])
        ident_b = sb("ident_b", [128, 128], BF16)
        ones_b = sb("ones_b", [128, 128], BF16)
        ones256 = sb("ones256", [128, 128], BF16)
        ustrict = sb("ustrict", [128, 128], BF16)
        B_const = Buf("const")
        iota_i = sb("iota_i", [128, 128], I32)
        iota_f = sb("iota_f", [128, 128])
        pidx_i = sb("pidx_i", [128, 1], I32)
        pidx_f = sb("pidx_f", [128, 1])
        P.op("pool", lambda g: g.iota(iota_i[:], [[1, 128]], base=0, channel_multiplier=0), writes=[B_const])
        P.op("pool", lambda g: g.iota(pidx_i[:], [[0, 1]], base=0, channel_multiplier=1), writes=[B_const])
        P.op("dve", lambda v: v.tensor_copy(out=iota_f[:], in_=iota_i[:]), reads=[B_const], writes=[B_const])
        P.op("dve", lambda v: v.tensor_copy(out=pidx_f[:], in_=pidx_i[:]), reads=[B_const], writes=[B_const])
        P.op("dve", lambda v: v.tensor_scalar(out=ident_f[:], in0=iota_f[:], scalar1=pidx_f[:, 0:1], scalar2=None,
                                              op0=ALU.is_equal), reads=[B_const], writes=[B_const])
        P.op("dve", lambda v: v.tensor_copy(out=ident_b[:], in_=ident_f[:]), writes=[B_const])
        P.op("dve", lambda v: v.tensor_scalar(out=ustrict[:], in0=iota_f[:], scalar1=pidx_f[:, 0:1], scalar2=None,
                                              op0=ALU.is_gt), writes=[B_const])
        P.op("dve", lambda v: v.memset(ones_b[:], 1.0), writes=[B_const])
        P.op("dve", lambda v: v.memset(ones256[:], 1.0 / 256.0), writes=[B_const])

        final_ops = []

        def emit_layer(LI, n_out, C, ext, x_ext, src_reads, dst, B_dst):
            n_in = n_out + 2 * HALO
            NTI = n_in // 128
            NTO = n_out // 128
            HT = HALO // 128
            NS = C // 128
            NSLOT = NE * C
            P.env["bc_vals"][LI] = NSLOT - 1
            if ext:
                FLAG_REG = ((0, HALO, 0), (HALO, 2 * HALO, 1), (n_in - 2 * HALO, n_in - HALO, 2), (n_in - HALO, n_in, 3))
                CP0, CP1 = HALO, n_out - HALO - 8
            else:
                FLAG_REG = ((0, HALO, 0), (n_in - HALO, n_in, 3))
                CP0, CP1 = 0, n_out - 8

            def din(name, shape, dt=F32):
                return nc.dram_tensor("%s_%d" % (name, LI), list(shape), dt, kind="ExternalInput").ap()

            w_in = din("w_in", [D, DIN])
            b_in_pc = din("b_in_pc", [128, 18])
            bv_bc = din("bv_bc", [128, 512])
            wpool_bd = din("wpool_bd", [2, 128, 128])
            pool_scale_pc = din("pool_scale_pc", [128, 2])
            poolcorr = din("poolcorr", [128, 2, 16])
            flags = din("flags", [128, 4])
            biasT = din("biasT", [8, 7, 128, 128])
            rowbias = din("rowbias", [128, NTO * 14])
            tokval = din("tokval", [128, 2, NTO])
            conv_dw_pc = din("conv_dw_pc", [128, 2, 31])
            conv_vec_pc = din("conv_vec_pc", [128, 4, 2])
            w_pw = din("w_pw", [256, 256])
            w_out = din("w_out", [D, D])
            vec_bc = din("vec_bc", [5, 128, D])
            w_router = din("w_router", [D, NE])
            b_router_bc = din("b_router_bc", [128, NE])
            ec_bc = din("ec_bc", [128, NE])
            b_gate_pc = din("b_gate_pc", [128, NE, 8])
            b_up_pc = din("b_up_pc", [128, NE, 8])
            if True:
                w_gate = din("w_gate", [NE, D, D])
                w_up = din("w_up", [NE, D, D])
                w_down = din("w_down", [NE, D, D])
                b_down = din("b_down", [NE, D])
            x1d = nc.dram_tensor("x1d_%d" % LI, [n_out, D], F32, kind="Internal").ap()
            xs = nc.dram_tensor("xs_%d" % LI, [NSLOT, D], BF16, kind="Internal").ap()
            ys = nc.dram_tensor("ys_%d" % LI, [NSLOT, D], F32, kind="Internal").ap()
            with contextlib.ExitStack() as top:
                top_bufs = []

                def Buf_(name):
                    return P.buf(name, top_bufs)

                def sb(name, shape, dt=F32, st=top):
                    return sb_global(name, shape, dt, st)

                B_par = Buf_("params")
                b_in_sb = sb("b_in_sb", [128, 18])
                flags_sb = sb("flags_sb", [128, 4])
                pscale_sb = sb("pscale_sb", [128, 2])
                pcorr_sb = sb("pcorr_sb", [128, 2, 16])
                rowb_sb = sb("rowb_sb", [128, NTO * 14])
                tokv_sb = sb("tokv_sb", [128, 2, NTO])
                cdw_sb = sb("cdw_sb", [128, 2, 31])
                cvec_sb = sb("cvec_sb", [128, 4, 2])
                brt_sb = sb("brt_sb", [128, NE])
                ec_sb = sb("ec_sb", [128, NE])
                bg_sb = sb("bg_sb", [128, NE, 8])
                bu_sb = sb("bu_sb", [128, NE, 8])
                for pdst, psrc in ((b_in_sb, b_in_pc), (flags_sb, flags), (pscale_sb, pool_scale_pc), (pcorr_sb, poolcorr),
                                 (rowb_sb, rowbias), (tokv_sb, tokval), (cdw_sb, conv_dw_pc), (cvec_sb, conv_vec_pc), (brt_sb, b_router_bc),
                                 (ec_sb, ec_bc), (bg_sb, b_gate_pc), (bu_sb, b_up_pc)):
                    P.dma("sync", (lambda d, s: (lambda q: q.dma_start(out=d[:], in_=s)))(pdst, psrc), writes=[B_par], indep=True)

                x1t = [sb("x1t%d" % i, [128, D]) for i in range(2)]
                B_x1t = [Buf_("x1t0"), Buf_("x1t1")]
                stats = sb("stats", [128, 2, 6])
                mv = sb("mv", [128, 2])
                rstd1 = sb("rstd1", [128, 1])
                B_st = Buf_("stats")

                x1b = [sb("x1b%d" % i, [128, D], BF16) for i in range(2)]
                B_x1b = [Buf_("x1b0"), Buf_("x1b1")]
                x1T = sb("x1T", [128, 8, 128])
                B_x1T = Buf_("x1T")
                wr_sb = sb("wr_sb", [128, 8, NE])
                B_wr = Buf_("wr")
                logit = sb("logit", [128, NE])
                top8 = sb("top8", [128, 8])
                negv0 = sb("negv0", [128, 1])
                ex4 = sb("ex4", [128, 4])
                ssum = sb("ssum", [128, 1])
                gates = sb("gates", [128, NTO, 4])
                mask_all = sb("mask_all", [128, NTO, NE], BF16)
                Atab = sb("Atab", [128, NE])
                ovf = sb("ovf", [128, NE])
                junk = sb("junk", [128, NE])
                slot_f = sb("slot_f", [128, 4])
                slots = sb("slots", [128, NTO, 4], I32)
                B_rt, B_gates, B_mask, B_slots = Buf_("rt"), Buf_("gates"), Buf_("mask"), Buf_("slots")
                B_x1d = Buf_("x1d")
                B_xs = Buf_("xs")
                tmp_t = [sb("tmp_t%d" % i, [128, D]) for i in range(2)]
                B_tmp = [Buf_("tmp0"), Buf_("tmp1")]

                with contextlib.ExitStack() as mx:
                    mx_bufs = []

                    def msb(name, shape, dt=F32):
                        return sb(name, shape, dt, st=mx)

                    ycatT = msb("ycatT", [128, 8, n_out], BF16)
                    B_ycat = [P.buf("ycat%d" % c, mx_bufs) for c in range(8)]
                    xin = [msb("xin%d" % i, [128, D]) for i in range(2)]
                    B_xin = [P.buf("xin%d" % i, mx_bufs) for i in range(2)]
                    B_xs0 = Buf_("xs_zero")
                    if stage == "full":
                        P.op("pool", lambda g: g.memset(ycatT[:], 0.0), writes=B_ycat)
                        zsrc = ycatT[:].rearrange("p c n -> p (c n)")
                        per = (8 * n_out) // D
                        for r0 in range(0, NSLOT // 128, per):
                            nr = min(per, NSLOT // 128 - r0)
                            P.dma("sync", (lambda r0, nr: (lambda q: q.dma_start(
                                out=xs[r0 * 128:(r0 + nr) * 128, :].rearrange("(s p) d -> p s d", p=128),
                                in_=zsrc[:, 0:nr * D].rearrange("p (s d) -> p s d", d=D))))(r0, nr),
                                  reads=B_ycat, writes=[B_xs0], indep=True)
                    w_in_v = w_in.rearrange("(c p) f -> p c f", p=128)
                    tblocks = [(s, min(512, n_in - s)) for s in range(0, n_in, 512)]

                    with contextlib.ExitStack() as sa:
                        sa_bufs = []
                        xT = sb("xT", [128, 8, n_in], BF16, st=sa)
                        B_xT = [P.buf("xT%d" % t, sa_bufs) for t in range(NTI)]

                        for t in range(NTI):
                            xi, bxi = xin[t % 2], B_xin[t % 2]
                            P.dma("sync", (lambda t, xi: (lambda q: q.dma_start(out=xi[:], in_=x_ext[t * 128:(t + 1) * 128, :])))(t, xi),
                                  reads=src_reads, writes=[bxi])
                            for hh in range(2):
                                bk = (t % 2) * 2 + hh
                                for c4 in range(4):
                                    c = hh * 4 + c4
                                    P.op("pe", (lambda xi, c, c4, bk: (lambda pe: pe.transpose(out=banks[bk][:, c4 * 128:(c4 + 1) * 128],
                                                                                                in_=xi[:, c * 128:(c + 1) * 128],
                                                                                                identity=ident_f[:])))(xi, c, c4, bk),
                                         reads=[bxi, B_const], writes=[bankb[bk]])
                                if hh == 0:
                                    P.op("act", (lambda t, hh, bk: (lambda a: a.copy(
                                        out=xT[:, hh * 4:(hh + 1) * 4, t * 128:(t + 1) * 128],
                                        in_=banks[bk][:].rearrange("p (c f) -> p c f", c=4))))(t, hh, bk),
                                         reads=[bankb[bk]], writes=[B_xT[t]])
                                else:
                                    P.op("dve", (lambda t, hh, bk: (lambda v: v.tensor_copy(
                                        out=xT[:, hh * 4:(hh + 1) * 4, t * 128:(t + 1) * 128],
                                        in_=banks[bk][:].rearrange("p (c f) -> p c f", c=4))))(t, hh, bk),
                                         reads=[bankb[bk]], writes=[B_xT[t]])

                        P.mark('phase0')

                        def xt_bufs(s, n):
                            return [B_xT[t] for t in range(s // 128, (s + n + 127) // 128)]

                        def inproj(wt, wcol, bw, s, n, bk):
                            for dch in range(8):
                                P.op("pe", (lambda dch: (lambda pe: pe.matmul(banks[bk][:, 0:n],
                                                                               lhsT=wt[:, dch, wcol:wcol + 128],
                                                                               rhs=xT[:, dch, s:s + n],
                                                                               start=(dch == 0), stop=(dch == 7))))(dch),
                                     reads=[bw] + xt_bufs(s, n), writes=[bankb[bk]])

                        with contextlib.ExitStack() as sp:
                            sp_bufs = []
                            wsl_p = sb("wsl_p", [128, 8, 256], BF16, st=sp)
                            B_wslp = P.buf("wsl_p", sp_bufs)
                            P.dma("pool", lambda g: g.dma_start(out=wsl_p[:], in_=w_in_v[:, :, 0:256]), writes=[B_wslp])
                            wpool_sb = sb("wpool_sb", [128, 2, 128], BF16, st=sp)
                            B_wp = P.buf("wpool", sp_bufs)
                            P.dma("pool", lambda g: g.dma_start(out=wpool_sb[:], in_=wpool_bd.rearrange("c p f -> p c f")), writes=[B_wp])
                            PADP = 16
                            uT = sb("uT", [128, n_in + PADP], st=sp)
                            aT = sb("aT", [128, n_in + PADP], st=sp)
                            a2T = sb("a2T", [128, n_in + PADP], st=sp)
                            mT = sb("mT", [128, n_out], st=sp)
                            dT = sb("dT", [128, n_out], BF16, st=sp)
                            B_u, B_a, B_a2, B_m, B_d = (P.buf(nm, sp_bufs) for nm in ("uT", "aT", "a2T", "mT", "dT"))
                            for pc in range(2):
                                P.op("dve", lambda v: v.memset(uT[:, n_in:n_in + PADP], 0.0), writes=[B_u])
                                for bi, (s, n) in enumerate(tblocks):
                                    bk = 4 + (bi % 2)
                                    inproj(wsl_p, pc * 128, B_wslp, s, n, bk)
                                    P.op("act", (lambda s, n, bk, pc: (lambda a: a.activation(out=uT[:, s:s + n], in_=banks[bk][:, 0:n],
                                                                                               func=AF.Identity, bias=b_in_sb[:, pc:pc + 1],
                                                                                               scale=1.0)))(s, n, bk, pc),
                                         reads=[bankb[bk], B_par], writes=[B_u])
                                for (fa, fb, fc) in FLAG_REG:
                                    P.op("dve", (lambda fa, fb, fc: (lambda v: v.tensor_scalar(out=uT[:, fa:fb], in0=uT[:, fa:fb],
                                                                                               scalar1=flags_sb[:, fc:fc + 1], scalar2=None,
                                                                                               op0=ALU.mult)))(fa, fb, fc),
                                         reads=[B_par], writes=[B_u])
                                L = n_in
                                P.op("dve", lambda v: v.tensor_tensor(out=aT[:, 0:L], in0=uT[:, 0:L], in1=uT[:, 1:L + 1], op=ALU.add),
                                     reads=[B_u], writes=[B_a])
                                P.op("dve", lambda v: v.memset(aT[:, L:L + PADP], 0.0), writes=[B_a])
                                P.op("dve", lambda v: v.tensor_tensor(out=a2T[:, 0:L], in0=aT[:, 0:L], in1=aT[:, 2:L + 2], op=ALU.add),
                                     reads=[B_a], writes=[B_a2])
                                P.op("dve", lambda v: v.memset(a2T[:, L:L + PADP], 0.0), writes=[B_a2])
                                o0 = HALO
                                if pc == 0:
                                    P.op("dve", lambda v: v.tensor_scalar(out=mT[0:64, :], in0=aT[0:64, o0 - 1:o0 - 1 + n_out], scalar1=0.5,
                                                                          scalar2=None, op0=ALU.mult), reads=[B_a], writes=[B_m])
                                    P.op("dve", lambda v: v.tensor_scalar(out=mT[64:128, :], in0=a2T[64:128, o0 - 2:o0 - 2 + n_out],
                                                                          scalar1=0.25, scalar2=None, op0=ALU.mult), reads=[B_a2], writes=[B_m])
                                else:
                                    P.op("dve", lambda v: v.tensor_tensor(out=aT[:, 0:L], in0=a2T[:, 0:L], in1=a2T[:, 4:L + 4], op=ALU.add),
                                         reads=[B_a2], writes=[B_a])
                                    P.op("dve", lambda v: v.tensor_tensor(out=a2T[:, 0:L], in0=aT[:, 0:L], in1=aT[:, 8:L + 8], op=ALU.add),
                                         reads=[B_a], writes=[B_a2])
                                    P.op("dve", lambda v: v.tensor_scalar(out=mT[0:64, :], in0=aT[0:64, o0 - 4:o0 - 4 + n_out], scalar1=0.125,
                                                                          scalar2=None, op0=ALU.mult), reads=[B_a], writes=[B_m])
                                    P.op("dve", lambda v: v.tensor_scalar(out=mT[64:128, :], in0=a2T[64:128, o0 - 8:o0 - 8 + n_out],
                                                                          scalar1=0.0625, scalar2=None, op0=ALU.mult), reads=[B_a2], writes=[B_m])
                                P.op("dve", (lambda pc: (lambda v: v.tensor_tensor(out=mT[:, CP0:CP0 + 8], in0=mT[:, CP0:CP0 + 8], in1=pcorr_sb[:, pc, 0:8],
                                                                                    op=ALU.mult)))(pc), reads=[B_par], writes=[B_m])
                                P.op("dve", (lambda pc: (lambda v: v.tensor_tensor(out=mT[:, CP1:CP1 + 8], in0=mT[:, CP1:CP1 + 8],
                                                                                    in1=pcorr_sb[:, pc, 8:16], op=ALU.mult)))(pc),
                                     reads=[B_par], writes=[B_m])
                                P.op("dve", lambda v: v.tensor_tensor(out=dT[:], in0=mT[:], in1=uT[:, o0:o0 + n_out], op=ALU.subtract),
                                     reads=[B_m, B_u], writes=[B_d])
                                for bi in range(n_out // 512):
                                    bk = 6 + (bi % 2)
                                    P.op("pe", (lambda bi, bk, pc: (lambda pe: pe.matmul(banks[bk][:], lhsT=wpool_sb[:, pc, :],
                                                                                          rhs=dT[:, bi * 512:(bi + 1) * 512],
                                                                                          start=True, stop=True)))(bi, bk, pc),
                                         reads=[B_wp, B_d], writes=[bankb[bk]])
                                    P.op("act", (lambda bi, bk, pc: (lambda a: a.activation(out=ycatT[:, pc, bi * 512:(bi + 1) * 512],
                                                                                             in_=banks[bk][:], func=AF.Copy,
                                                                                             scale=pscale_sb[:, pc:pc + 1])))(bi, bk, pc),
                                         reads=[bankb[bk], B_par], writes=[B_ycat[pc]])
                        P.close(sp_bufs)
                        P.mark('pool')

                        with contextlib.ExitStack() as sc:
                            sc_bufs = []
                            wsl_c = sb("wsl_c", [128, 8, 512], BF16, st=sc)
                            B_wslc = P.buf("wsl_c", sc_bufs)
                            P.dma("pool", lambda g: g.dma_start(out=wsl_c[:], in_=w_in_v[:, :, 1792:2304]), writes=[B_wslc])
                            wpw_sb = sb("wpw_sb", [128, 2, 256], BF16, st=sc)
                            B_wpw = P.buf("wpw", sc_bufs)
                            P.dma("pool", lambda g: g.dma_start(out=wpw_sb[:], in_=w_pw.rearrange("(c p) f -> p c f", p=128)), writes=[B_wpw])
                            hT = sb("hT", [128, 2, n_in], BF16, st=sc)
                            B_h = [P.buf("hT0", sc_bufs), P.buf("hT1", sc_bufs)]
                            sg = sb("sg", [128, 512], st=sc)
                            B_sg = P.buf("sg", sc_bufs)
                            for j in range(2):
                                for bi, (s, n) in enumerate(tblocks):
                                    inproj(wsl_c, 256 + j * 128, B_wslc, s, n, 4)
                                    P.op("act", (lambda s, n, j: (lambda a: a.activation(out=sg[:, 0:n], in_=banks[4][:, 0:n], func=AF.Sigmoid,
                                                                                         bias=b_in_sb[:, 16 + j:17 + j], scale=1.0)))(s, n, j),
                                         reads=[bankb[4], B_par], writes=[B_sg])
                                    inproj(wsl_c, j * 128, B_wslc, s, n, 5)
                                    P.op("dve", (lambda s, n, j: (lambda v: v.scalar_tensor_tensor(out=hT[:, j, s:s + n], in0=banks[5][:, 0:n],
                                                                                                    scalar=b_in_sb[:, 14 + j:15 + j],
                                                                                                    in1=sg[:, 0:n], op0=ALU.add,
                                                                                                    op1=ALU.mult)))(s, n, j),
                                         reads=[bankb[5], B_sg, B_par], writes=[B_h[j]])
                                for (fa, fb, fc) in FLAG_REG:
                                    P.op("dve", (lambda j, fa, fb, fc: (lambda v: v.tensor_scalar(out=hT[:, j, fa:fb], in0=hT[:, j, fa:fb],
                                                                                                  scalar1=flags_sb[:, fc:fc + 1], scalar2=None,
                                                                                                  op0=ALU.mult)))(j, fa, fb, fc),
                                         reads=[B_par], writes=[B_h[j]])
                            dg = sb("dg", [128, 2, 31, 128], BF16, st=sc)
                            B_dg = P.buf("dg", sc_bufs)
                            for j in range(2):
                                for k in range(31):
                                    P.op("dve", (lambda j, k: (lambda v: v.tensor_scalar(out=dg[:, j, k, :], in0=ident_f[:],
                                                                                         scalar1=cdw_sb[:, j, k:k + 1], scalar2=None,
                                                                                         op0=ALU.mult)))(j, k),
                                         reads=[B_par, B_const], writes=[B_dg])
                            cT = sb("cT", [128, 2, 512], st=sc)
                            cTb = sb("cTb", [128, 2, 512], BF16, st=sc)
                            sqb = sb("sqb", [128, 2, 512], BF16, st=sc)
                            mean_sb = sb("mean_sb", [128, 512], st=sc)
                            m2_sb = sb("m2_sb", [128, 512], st=sc)
                            rstd_sb = sb("rstd_sb", [128, 512], st=sc)
                            nrm = sb("nrm", [128, 2, 512], st=sc)
                            silu_b = sb("silu_b", [128, 2, 512], BF16, st=sc)
                            B_cT, B_cTb, B_sq, B_mean, B_m2, B_rstd, B_nrm, B_silu = (P.buf(nm, sc_bufs) for nm in
                                                                                      ("cT", "cTb", "sqb", "mean", "m2", "rstd", "nrm", "silu"))
                            for bi in range(n_out // 512):
                                t0 = HALO + bi * 512
                                for j in range(2):
                                    bk = 6 + j
                                    for k in range(31):
                                        P.op("pe", (lambda j, k, bk, t0: (lambda pe: pe.matmul(banks[bk][:], lhsT=dg[:, j, k, :],
                                                                                                rhs=hT[:, j, t0 + k - 15:t0 + k - 15 + 512],
                                                                                                start=(k == 0), stop=(k == 30))))(j, k, bk, t0),
                                             reads=[B_dg, B_h[j]], writes=[bankb[bk]])
                                    P.op("act", (lambda j, bk: (lambda a: a.activation(out=cT[:, j, :], in_=banks[bk][:], func=AF.Identity,
                                                                                       bias=cvec_sb[:, 0, j:j + 1], scale=1.0)))(j, bk),
                                         reads=[bankb[bk], B_par], writes=[B_cT])
                                P.op("dve", lambda v: v.tensor_copy(out=cTb[:], in_=cT[:]), reads=[B_cT], writes=[B_cTb])
                                P.op("act", lambda a: a.activation(out=sqb[:], in_=cT[:], func=AF.Square), reads=[B_cT], writes=[B_sq])
                                for j in range(2):
                                    P.op("pe", (lambda j: (lambda pe: pe.matmul(banks[4][:], lhsT=ones256[:], rhs=cTb[:, j, :],
                                                                                 start=(j == 0), stop=(j == 1))))(j),
                                         reads=[B_const, B_cTb], writes=[bankb[4]])
                                for j in range(2):
                                    P.op("pe", (lambda j: (lambda pe: pe.matmul(banks[5][:], lhsT=ones256[:], rhs=sqb[:, j, :],
                                                                                 start=(j == 0), stop=(j == 1))))(j),
                                         reads=[B_const, B_sq], writes=[bankb[5]])
                                P.op("act", lambda a: a.copy(out=mean_sb[:], in_=banks[4][:]), reads=[bankb[4]], writes=[B_mean])
                                P.op("dve", lambda v: v.tensor_tensor(out=m2_sb[:], in0=mean_sb[:], in1=mean_sb[:], op=ALU.mult),
                                     reads=[B_mean], writes=[B_m2])
                                P.op("dve", lambda v: v.scalar_tensor_tensor(out=m2_sb[:], in0=banks[5][:], scalar=EPS, in1=m2_sb[:],
                                                                             op0=ALU.add, op1=ALU.subtract), reads=[bankb[5]], writes=[B_m2])
                                P.op("act", lambda a: a.activation(out=m2_sb[:], in_=m2_sb[:], func=AF.Sqrt), reads=[B_m2], writes=[B_m2])
                                P.op("dve", lambda v: v.reciprocal(out=rstd_sb[:], in_=m2_sb[:]), reads=[B_m2], writes=[B_rstd])
                                for j in range(2):
                                    P.op("dve", (lambda j: (lambda v: v.tensor_tensor(out=nrm[:, j, :], in0=cT[:, j, :], in1=mean_sb[:],
                                                                                      op=ALU.subtract)))(j), reads=[B_cT, B_mean], writes=[B_nrm])
                                    P.op("dve", (lambda j: (lambda v: v.tensor_tensor(out=nrm[:, j, :], in0=nrm[:, j, :], in1=rstd_sb[:],
                                                                                      op=ALU.mult)))(j), reads=[B_rstd], writes=[B_nrm])
                                    P.op("act", (lambda j: (lambda a: a.activation(out=silu_b[:, j, :], in_=nrm[:, j, :], func=AF.Silu,
                                                                                   bias=cvec_sb[:, 2, j:j + 1], scale=cvec_sb[:, 1, j:j + 1])))(j),
                                         reads=[B_nrm, B_par], writes=[B_silu])
                                for cc in range(2):
                                    bk = 6 + cc
                                    for j in range(2):
                                        P.op("pe", (lambda j, cc, bk: (lambda pe: pe.matmul(banks[bk][:], lhsT=wpw_sb[:, j, cc * 128:(cc + 1) * 128],
                                                                                             rhs=silu_b[:, j, :], start=(j == 0),
                                                                                             stop=(j == 1))))(j, cc, bk),
                                             reads=[B_wpw, B_silu], writes=[bankb[bk]])
                                    P.op("act", (lambda cc, bk, bi: (lambda a: a.activation(out=ycatT[:, 6 + cc, bi * 512:(bi + 1) * 512],
                                                                                             in_=banks[bk][:], func=AF.Identity,
                                                                                             bias=cvec_sb[:, 3, cc:cc + 1], scale=1.0)))(cc, bk, bi),
                                         reads=[bankb[bk], B_par], writes=[B_ycat[6 + cc]])
                        P.close(sc_bufs)
                        P.mark('conv')

                        sidx_box = [0]

                        def attn_group(hg):
                            with contextlib.ExitStack() as sat:
                                at_bufs = []
                                wsl = sb("wsl_a", [128, 8, 3, 256], BF16, st=sat)
                                B_wsl = P.buf("wsl_a", at_bufs)
                                for qi, c0 in enumerate((256, 768, 1280)):
                                    P.dma("pool", (lambda qi, c0: (lambda g: g.dma_start(out=wsl[:, :, qi, :],
                                                                                         in_=w_in_v[:, :, c0 + hg * 256:c0 + (hg + 1) * 256])))(qi, c0),
                                          writes=[B_wsl], indep=True)
                                bv_sb = sb("bv_sb", [128, 256], st=sat)
                                B_bv = P.buf("bv", at_bufs)
                                P.dma("sync", lambda q: q.dma_start(out=bv_sb[:], in_=bv_bc[:, hg * 256:(hg + 1) * 256]), writes=[B_bv])
                                qT = sb("qT", [128, 4, n_out], BF16, st=sat)
                                kT = sb("kT", [128, 2, n_in], BF16, st=sat)
                                B_q, B_k = P.buf("qT", at_bufs), P.buf("kT", at_bufs)
                                P.op("pool", lambda g: g.memset(qT[:], 0.0), writes=[B_q])
                                wq = wsl[:, :, 0, :]
                                wk = wsl[:, :, 1, :]
                                for c in range(2):
                                    gch = 2 + hg * 2 + c
                                    for bi in range(n_out // 512):
                                        bk = 4 + (bi % 2)
                                        inproj(wq, c * 128, B_wsl, HALO + bi * 512, 512, bk)
                                        for hh2 in range(2):
                                            P.op("act", (lambda c, bi, bk, gch, hh2: (lambda a: a.activation(
                                                out=qT[hh2 * 64:(hh2 + 1) * 64, 2 * c + hh2, bi * 512:(bi + 1) * 512],
                                                in_=banks[bk][hh2 * 64:(hh2 + 1) * 64, :],
                                                func=AF.Identity, bias=b_in_sb[hh2 * 64:(hh2 + 1) * 64, gch:gch + 1],
                                                scale=1.0)))(c, bi, bk, gch, hh2),
                                                 reads=[bankb[bk], B_par], writes=[B_q])
                                    for bi, (s, n) in enumerate(tblocks):
                                        bk = 6 + (bi % 2)
                                        inproj(wk, c * 128, B_wsl, s, n, bk)
                                        P.op("dve", (lambda c, s, n, bk, gch: (lambda v: v.tensor_scalar(out=kT[:, c, s:s + n], in0=banks[bk][:, 0:n],
                                                                                                          scalar1=b_in_sb[:, gch + 4:gch + 5], scalar2=None,
                                                                                                          op0=ALU.add)))(c, s, n, bk, gch),
                                             reads=[bankb[bk], B_par], writes=[B_k])
                                P.mark('a_qk%d' % hg)
                                vaug = sb("vaug", [128, NTI, 4, 65], BF16, st=sat)
                                B_v = P.buf("vaug", at_bufs)
                                P.op("dve", lambda v: v.memset(vaug[:, :, :, 64:65], 1.0), writes=[B_v])
                                for t in range(NTI):
                                    bk = 4 + (t % 2)
                                    for dch in range(8):
                                        P.op("pe", (lambda t, dch, bk: (lambda pe: pe.matmul(banks[bk][:, 0:256], lhsT=xT[:, dch, t * 128:(t + 1) * 128],
                                                                                              rhs=wsl[:, dch, 2, :],
                                                                                              start=(dch == 0), stop=(dch == 7))))(t, dch, bk),
                                             reads=[B_wsl, B_xT[t]], writes=[bankb[bk]])
                                    P.op("dve", (lambda t, bk: (lambda v: v.tensor_tensor(out=vaug[:, t, :, 0:64],
                                                                                          in0=banks[bk][:, 0:256].rearrange("p (h d) -> p h d", h=4),
                                                                                          in1=bv_sb[:].rearrange("p (h d) -> p h d", h=4),
                                                                                          op=ALU.add)))(t, bk),
                                         reads=[bankb[bk], B_bv], writes=[B_v])
                                P.mark('a_v%d' % hg)
                                Etab = sb("Etab", [128, 4, 7, 128], BF16, st=sat)
                                B_E = P.buf("Etab", at_bufs)
                                bstage = [sb("bstage0", [128, 7, 128], st=sat)] * 2
                                B_bst = [P.buf("bst0", at_bufs)] * 2
                                for h4 in range(4):
                                    P.dma("sync", (lambda h4: (lambda q: q.dma_start(out=bstage[h4 % 2][:],
                                                                                     in_=biasT[hg * 4 + h4].rearrange("j k q -> k j q"))))(h4),
                                          writes=[B_bst[h4 % 2]])
                                    P.op("act", (lambda h4: (lambda a: a.activation(out=Etab[:, h4, :, :], in_=bstage[h4 % 2][:], func=AF.Exp)))(h4),
                                         reads=[B_bst[h4 % 2]], writes=[B_E])
                                P.mark('a_E%d' % hg)
                                pt = sb("pT", [128, 7, 512], BF16, st=sat)
                                bpt = P.buf("pT", at_bufs)
                                yat = [sb("yat%d" % i, [128, 256], BF16, st=sat) for i in range(2)]
                                B_yat = [P.buf("yat0", at_bufs), P.buf("yat1", at_bufs)]
                                rec = sb("rec", [128, 4], st=sat)
                                B_rec = P.buf("rec", at_bufs)
                                for p in range(NTO):
                                    qt = p + HT
                                    jt = [(j, qt - 3 + j) for j in range(7) if 0 <= qt - 3 + j < NTI]
                                    for (j, kt) in jt:
                                        bk = sidx_box[0] % 4
                                        sidx_box[0] += 1
                                        for h4 in range(4):
                                            ch, hp = h4 // 2, (h4 % 2) * 64
                                            P.op("pe", (lambda bk, h4, ch, hp, kt, p: (lambda pe: pe.matmul(
                                                banks[bk][:, h4 * 128:(h4 + 1) * 128],
                                                lhsT=kT[:, ch, kt * 128:(kt + 1) * 128],
                                                rhs=qT[:, h4, p * 128:(p + 1) * 128], start=True, stop=True)))(bk, h4, ch, hp, kt, p),
                                                 reads=[B_q, B_k], writes=[bankb[bk]])
                                        for rr in range(2):
                                            col = (p * 7 + j) * 2 + rr
                                            P.op("act", (lambda bk, j, rr, col: (lambda a: a.activation(
                                                out=pt[:, j, :].rearrange("p (h r c) -> p h r c", h=4, r=2)[:, :, rr, :],
                                                in_=banks[bk][:].rearrange("p (h r c) -> p h r c", h=4, r=2)[:, :, rr, :],
                                                func=AF.Exp, bias=rowb_sb[:, col:col + 1], scale=0.125)))(bk, j, rr, col),
                                                 reads=[bankb[bk], B_par], writes=[bpt])
                                        P.op("dve", (lambda j: (lambda v: v.tensor_tensor(
                                            out=pt[:, j, :].rearrange("p (h q) -> p h q", h=4),
                                            in0=pt[:, j, :].rearrange("p (h q) -> p h q", h=4),
                                            in1=Etab[:, :, j, :], op=ALU.mult)))(j),
                                             reads=[B_E], writes=[bpt])
                                    P.mark('a_S%d_%d' % (hg, p))
                                    ob = 4 + (p % 2)
                                    for h4 in range(4):
                                        for ji, (j, kt) in enumerate(jt):
                                            P.op("pe", (lambda ob, h4, j, kt, ji, nj: (lambda pe: pe.matmul(
                                                banks[ob][:, h4 * 65:(h4 + 1) * 65], lhsT=pt[:, j, h4 * 128:(h4 + 1) * 128],
                                                rhs=vaug[:, kt, h4, :], start=(ji == 0), stop=(ji == nj - 1))))(ob, h4, j, kt, ji, len(jt)),
                                                 reads=[bpt, B_v], writes=[bankb[ob]])
                                    P.mark('a_AV%d_%d' % (hg, p))
                                    ya, bya = yat[p % 2], B_yat[p % 2]
                                    P.op("dve", (lambda ob: (lambda v: v.reciprocal(
                                        out=rec[:], in_=banks[ob][:, 0:260].rearrange("p (h d) -> p h d", h=4)[:, :, 64])))(ob),
                                         reads=[bankb[ob]], writes=[B_rec])
                                    for h4 in range(4):
                                        P.op("dve", (lambda ob, h4, ya: (lambda v: v.tensor_scalar(
                                            out=ya[:, h4 * 64:(h4 + 1) * 64], in0=banks[ob][:, h4 * 65:h4 * 65 + 64],
                                            scalar1=rec[:, h4:h4 + 1], scalar2=None, op0=ALU.mult)))(ob, h4, ya),
                                             reads=[bankb[ob], B_rec], writes=[bya])
                                    P.mark('a_N%d_%d' % (hg, p))
                                    tb = 6 + (p % 2)
                                    tbv = banks[tb][:].bitcast(BF16)
                                    for c2 in range(2):
                                        P.op("pe", (lambda c2, ya, tbv: (lambda pe: pe.transpose(out=tbv[:, c2 * 128:(c2 + 1) * 128],
                                                                                                  in_=ya[:, c2 * 128:(c2 + 1) * 128],
                                                                                                  identity=ident_b[:])))(c2, ya, tbv),
                                             reads=[bya, B_const], writes=[bankb[tb]])
                                    P.op("act", (lambda p, tbv: (lambda a: a.copy(out=ycatT[:, 2 + hg * 2:4 + hg * 2, p * 128:(p + 1) * 128],
                                                                                  in_=tbv[:, 0:256].rearrange("p (c f) -> p c f", c=2))))(p, tbv),
                                         reads=[bankb[tb]], writes=[B_ycat[2 + hg]])
                            P.close(at_bufs)
                        for _hg in range(2):
                            attn_group(_hg)
                            P.mark('attn%d' % _hg)
                    P.close(sa_bufs)

                    w_out_sb = msb("w_out_sb", [128, 8, D], BF16)
                    B_wout = P.buf("w_out", mx_bufs)
                    P.dma("pool", lambda g: g.dma_start(out=w_out_sb[:], in_=w_out.rearrange("(c p) f -> p c f", p=128)),
                          writes=[B_wout])

                    vecs = msb("vecs", [128, 5, D])
                    B_vecs = P.buf("vecs", mx_bufs)
                    P.dma("sync", lambda q: q.dma_start(out=vecs[:], in_=vec_bc.rearrange("v p d -> p v d")), writes=[B_vecs])
                    def layer_norm(src, bsrc, ldst, bdst, gi, vecs, B_vecs):
                        for hh in range(2):
                            P.op("dve", (lambda hh: (lambda v: v.bn_stats(out=stats[:, hh, :], in_=src[:, hh * 512:(hh + 1) * 512])))(hh),
                                 reads=[bsrc], writes=[B_st])
                        P.op("dve", lambda v: v.bn_aggr(out=mv[:], in_=stats[:].rearrange("p a b -> p (a b)")), writes=[B_st])
                        P.op("dve", lambda v: v.tensor_scalar(out=rstd1[:], in0=mv[:, 1:2], scalar1=EPS, scalar2=None, op0=ALU.add),
                             writes=[B_st])
                        P.op("act", lambda a: a.activation(out=rstd1[:], in_=rstd1[:], func=AF.Sqrt), reads=[B_st], writes=[B_st])
                        P.op("dve", lambda v: v.reciprocal(out=rstd1[:], in_=rstd1[:]), reads=[B_st], writes=[B_st])
                        P.op("dve", lambda v: v.tensor_scalar(out=src[:], in0=src[:], scalar1=mv[:, 0:1], scalar2=rstd1[:, 0:1],
                                                              op0=ALU.subtract, op1=ALU.mult), reads=[B_st], writes=[bsrc])
                        P.op("dve", lambda v: v.tensor_tensor(out=src[:], in0=src[:], in1=vecs[:, gi, :], op=ALU.mult),
                             reads=[B_vecs], writes=[bsrc])
                        P.op("dve", lambda v: v.tensor_tensor(out=ldst[:], in0=src[:], in1=vecs[:, gi + 1, :], op=ALU.add),
                             reads=[B_vecs, bsrc], writes=[bdst])

                    P.dma("sync", lambda q: q.dma_start(out=wr_sb[:], in_=w_router.rearrange("(c p) e -> p c e", p=128)), writes=[B_wr])

                    for p in range(NTO):
                        xi, bxi = xin[p % 2], B_xin[p % 2]
                        tt, btt = tmp_t[p % 2], B_tmp[p % 2]
                        x1, bx1 = x1t[p % 2], B_x1t[p % 2]
                        P.dma("sync", (lambda p, xi: (lambda q: q.dma_start(out=xi[:], in_=x_ext[HALO + p * 128:HALO + (p + 1) * 128, :])))(p, xi),
                              reads=src_reads, writes=[bxi])
                        P.op("dve", (lambda xi: (lambda v: v.scalar_tensor_tensor(out=xi[:], in0=xi[:], scalar=ALPHA, in1=vecs[:, 0, :],
                                                                                   op0=ALU.mult, op1=ALU.add)))(xi),
                             reads=[B_vecs], writes=[bxi])
                        for hh in range(2):
                            bk = (p % 2) * 2 + hh
                            for fch in range(8):
                                P.op("pe", (lambda p, hh, fch, bk: (lambda pe: pe.matmul(banks[bk][:], lhsT=ycatT[:, fch, p * 128:(p + 1) * 128],
                                                                                          rhs=w_out_sb[:, fch, hh * 512:(hh + 1) * 512],
                                                                                          start=(fch == 0), stop=(fch == 7))))(p, hh, fch, bk),
                                     reads=[B_wout] + B_ycat, writes=[bankb[bk]])
                            P.op("dve", (lambda hh, bk, xi, tt: (lambda v: v.tensor_tensor(out=tt[:, hh * 512:(hh + 1) * 512],
                                                                                            in0=banks[bk][:], in1=xi[:, hh * 512:(hh + 1) * 512],
                                                                                            op=ALU.add)))(hh, bk, xi, tt),
                                 reads=[bankb[bk], bxi], writes=[btt])
                        layer_norm(tt, btt, x1, bx1, 1, vecs, B_vecs)
                        if stage == "mix":
                            final_ops.append(P.dma("sync", (lambda p, x1: (lambda q: q.dma_start(out=dst[p * 128:(p + 1) * 128, :], in_=x1[:])))(p, x1),
                                                   reads=[bx1], writes=[B_dst], sembuf=bx1, indep=True))
                            continue
                        P.dma("sync", (lambda p, x1: (lambda q: q.dma_start(out=x1d[p * 128:(p + 1) * 128, :], in_=x1[:])))(p, x1),
                              reads=[bx1], writes=[B_x1d], sembuf=bx1, indep=True)
                        xb, bxb = x1b[p % 2], B_x1b[p % 2]
                        P.op("act", (lambda x1, xb: (lambda a: a.copy(out=xb[:], in_=x1[:])))(x1, xb), reads=[bx1], writes=[bxb])
                        for hh in range(2):
                            bk = 4 + hh
                            for c4 in range(4):
                                c = hh * 4 + c4
                                P.op("pe", (lambda x1, c, c4, bk: (lambda pe: pe.transpose(out=banks[bk][:, c4 * 128:(c4 + 1) * 128],
                                                                                            in_=x1[:, c * 128:(c + 1) * 128],
                                                                                            identity=ident_f[:])))(x1, c, c4, bk),
                                     reads=[bx1, B_const], writes=[bankb[bk]])
                            P.op("act", (lambda hh, bk: (lambda a: a.copy(out=x1T[:, hh * 4:(hh + 1) * 4, :],
                                                                          in_=banks[bk][:].rearrange("p (c f) -> p c f", c=4))))(hh, bk),
                                 reads=[bankb[bk]], writes=[B_x1T])
                        for c in range(8):
                            P.op("pe", (lambda c: (lambda pe: pe.matmul(banks[6][:, 0:NE], lhsT=x1T[:, c, :], rhs=wr_sb[:, c, :],
                                                                         start=(c == 0), stop=(c == 7))))(c),
                                 reads=[B_x1T, B_wr], writes=[bankb[6]])
                        P.op("dve", lambda v: v.tensor_tensor(out=logit[:], in0=banks[6][:, 0:NE], in1=brt_sb[:], op=ALU.add),
                             reads=[bankb[6], B_par], writes=[B_rt])
                        P.op("dve", lambda v: v.max(out=top8[:], in_=logit[:]), writes=[B_rt])
                        P.op("dve", lambda v: v.tensor_scalar(out=negv0[:], in0=top8[:, 0:1], scalar1=-1.0, scalar2=None, op0=ALU.mult),
                             writes=[B_rt])
                        P.op("act", lambda a: a.activation(out=ex4[:], in_=top8[:, 0:4], func=AF.Exp, bias=negv0[:, 0:1], scale=1.0),
                             reads=[B_rt], writes=[B_rt])
                        P.op("dve", lambda v: v.tensor_reduce(out=ssum[:], in_=ex4[:], axis=mybir.AxisListType.X, op=ALU.add),
                             reads=[B_rt], writes=[B_rt])
                        P.op("dve", lambda v: v.reciprocal(out=ssum[:], in_=ssum[:]), writes=[B_rt])
                        P.op("dve", (lambda p: (lambda v: v.tensor_scalar(out=gates[:, p, :], in0=ex4[:], scalar1=ssum[:, 0:1], scalar2=None,
                                                                          op0=ALU.mult)))(p), reads=[B_rt], writes=[B_gates])
                        P.op("dve", (lambda p: (lambda v: v.tensor_scalar(out=mask_all[:, p, :], in0=logit[:], scalar1=top8[:, 3:4],
                                                                          scalar2=tokv_sb[:, 0, p:p + 1], op0=ALU.is_ge,
                                                                          op1=ALU.mult)))(p), reads=[B_rt, B_par], writes=[B_mask])
                        for pp in range(p + 1):
                            P.op("pe", (lambda pp, p: (lambda pe: pe.matmul(banks[7][:, 0:NE],
                                                                             lhsT=(ustrict[:] if pp == p else ones_b[:]),
                                                                             rhs=mask_all[:, pp, :], start=(pp == 0), stop=(pp == p))))(pp, p),
                                 reads=[B_mask, B_const], writes=[bankb[7]])
                        P.op("dve", lambda v: v.tensor_scalar(out=ovf[:], in0=banks[7][:, 0:NE], scalar1=float(C), scalar2=1.0e6,
                                                              op0=ALU.is_ge, op1=ALU.mult), reads=[bankb[7]], writes=[B_rt])
                        P.op("dve", lambda v: v.tensor_tensor(out=Atab[:], in0=banks[7][:, 0:NE], in1=ec_sb[:], op=ALU.add),
                             reads=[bankb[7], B_par], writes=[B_rt])
                        P.op("dve", (lambda p: (lambda v: v.scalar_tensor_tensor(out=Atab[:], in0=Atab[:], scalar=tokv_sb[:, 1, p:p + 1],
                                                                                 in1=ovf[:], op0=ALU.add, op1=ALU.add)))(p),
                             reads=[B_par], writes=[B_rt])
                        for k in range(4):
                            P.op("dve", (lambda k: (lambda v: v.scalar_tensor_tensor(out=junk[:], in0=logit[:], scalar=top8[:, k:k + 1],
                                                                                      in1=Atab[:], op0=ALU.is_equal, op1=ALU.mult,
                                                                                      accum_out=slot_f[:, k:k + 1])))(k), writes=[B_rt])
                        P.op("dve", (lambda p: (lambda v: v.tensor_copy(out=slots[:, p, :], in_=slot_f[:])))(p), reads=[B_rt], writes=[B_slots])
                        for k in range(4):
                            P.dma("pool", (lambda p, k, xb: (lambda g: g.indirect_dma_start(
                                out=xs[:, :], out_offset=bass.IndirectOffsetOnAxis(ap=slots[:, p, k:k + 1], axis=0),
                                in_=xb[:, :], in_offset=None, bounds_check=P.env["bc_reg%d" % LI], oob_is_err=False)))(p, k, xb),
                                  reads=[bxb, B_slots, B_xs0], writes=[B_xs], sembuf=bxb, indep=True)

                    P.close(mx_bufs)
                with contextlib.ExitStack() as me:
                    me_bufs = []

                    def esb(name, shape, dt=F32):
                        return sb(name, shape, dt, st=me)

                    wg = [esb("wg%d" % i, [128, 8, D], BF16) for i in range(2)]
                    wu = [esb("wu%d" % i, [128, 8, D], BF16) for i in range(2)]
                    wd = [esb("wd%d" % i, [128, 8, D], BF16) for i in range(2)]
                    B_wg = [P.buf("wg0", me_bufs), P.buf("wg1", me_bufs)]
                    B_wu = [P.buf("wu0", me_bufs), P.buf("wu1", me_bufs)]
                    B_wd = [P.buf("wd0", me_bufs), P.buf("wd1", me_bufs)]
                    xe = [esb("xe%d" % i, [128, NS, D], BF16) for i in range(2)]
                    B_xe = [P.buf("xe0", me_bufs), P.buf("xe1", me_bufs)]
                    xeT = esb("xeT", [128, 8, C], BF16)
                    B_xeT = P.buf("xeT", me_bufs)
                    bd = [esb("bd%d" % i, [128, D]) for i in range(2)]
                    B_bd = [P.buf("bd0", me_bufs), P.buf("bd1", me_bufs)]
                    gc = [esb("gc%d" % i, [128, C]) for i in range(2)]
                    sgm = [esb("sgm%d" % i, [128, C]) for i in range(2)]
                    ub = [esb("ub%d" % i, [128, C]) for i in range(2)]
                    B_gc = [P.buf("gc0", me_bufs), P.buf("gc1", me_bufs)]
                    B_sgm = [P.buf("sgm0", me_bufs), P.buf("sgm1", me_bufs)]
                    B_ub = [P.buf("ub0", me_bufs), P.buf("ub1", me_bufs)]
                    actT = esb("actT", [128, 8, C], BF16)
                    B_act = [P.buf("act%d" % f, me_bufs) for f in range(8)]
                    yo = [esb("yo%d" % i, [128, D]) for i in range(2)]
                    B_yo = [P.buf("yo0", me_bufs), P.buf("yo1", me_bufs)]
                    B_ys = P.buf("ys")

                    def load_w(e):
                        i = e % 2
                        for (wdst, bdst, wsrc) in ((wg[i], B_wg[i], w_gate), (wu[i], B_wu[i], w_up), (wd[i], B_wd[i], w_down)):
                            P.dma("pool", (lambda wdst, wsrc, e: (lambda g: g.dma_start(out=wdst[:], in_=wsrc[e].rearrange("(c p) f -> p c f", p=128))))(wdst, wsrc, e),
                                  writes=[bdst])

                    load_w(0)
                    for e in range(NE):
                        i = e % 2
                        if e + 1 < NE:
                            load_w(e + 1)
                        P.dma("sync", (lambda e, i: (lambda q: q.dma_start(out=xe[i][:], in_=xs[e * C:(e + 1) * C, :].rearrange("(s p) d -> p s d", p=128))))(e, i),
                              reads=[B_xs], writes=[B_xe[i]])
                        P.dma("sync", (lambda e, i: (lambda q: q.dma_start(out=bd[i][:], in_=b_down[e:e + 1, :].to_broadcast([128, D]))))(e, i),
                              writes=[B_bd[i]])
                        for s in range(NS):
                            for hh in range(2):
                                bk = hh
                                tbv = banks[bk][:].bitcast(BF16)
                                for c4 in range(4):
                                    c = hh * 4 + c4
                                    P.op("pe", (lambda i, s, c, c4, tbv: (lambda pe: pe.transpose(out=tbv[:, c4 * 128:(c4 + 1) * 128],
                                                                                                   in_=xe[i][:, s, c * 128:(c + 1) * 128],
                                                                                                   identity=ident_b[:])))(i, s, c, c4, tbv),
                                         reads=[B_xe[i], B_const], writes=[bankb[bk]])
                                if hh == 0:
                                    P.op("act", (lambda s, hh, tbv: (lambda a: a.copy(out=xeT[:, hh * 4:(hh + 1) * 4, s * 128:(s + 1) * 128],
                                                                                      in_=tbv[:, 0:512].rearrange("p (c f) -> p c f", c=4))))(s, hh, tbv),
                                         reads=[bankb[bk]], writes=[B_xeT])
                                else:
                                    P.op("dve", (lambda s, hh, tbv: (lambda v: v.tensor_copy(out=xeT[:, hh * 4:(hh + 1) * 4, s * 128:(s + 1) * 128],
                                                                                             in_=tbv[:, 0:512].rearrange("p (c f) -> p c f", c=4))))(s, hh, tbv),
                                         reads=[bankb[bk]], writes=[B_xeT])
                        for f in range(8):
                            bg, bu = 2 + (f % 2) * 2, 3 + (f % 2) * 2
                            k2 = f % 2
                            for dch in range(8):
                                P.op("pe", (lambda i, f, dch, bg: (lambda pe: pe.matmul(banks[bg][:, 0:C], lhsT=wg[i][:, dch, f * 128:(f + 1) * 128],
                                                                                         rhs=xeT[:, dch, :], start=(dch == 0), stop=(dch == 7))))(i, f, dch, bg),
                                     reads=[B_wg[i], B_xeT], writes=[bankb[bg]])
                            for dch in range(8):
                                P.op("pe", (lambda i, f, dch, bu: (lambda pe: pe.matmul(banks[bu][:, 0:C], lhsT=wu[i][:, dch, f * 128:(f + 1) * 128],
                                                                                         rhs=xeT[:, dch, :], start=(dch == 0), stop=(dch == 7))))(i, f, dch, bu),
                                     reads=[B_wu[i], B_xeT], writes=[bankb[bu]])
                            P.op("dve", (lambda e, f, bg, k2: (lambda v: v.tensor_scalar(out=gc[k2][:], in0=banks[bg][:, 0:C], scalar1=bg_sb[:, e, f:f + 1],
                                                                                          scalar2=7.0, op0=ALU.add, op1=ALU.min)))(e, f, bg, k2),
                                 reads=[bankb[bg], B_par], writes=[B_gc[k2]])
                            P.op("act", (lambda k2: (lambda a: a.activation(out=sgm[k2][:], in_=gc[k2][:], func=AF.Sigmoid, scale=1.702)))(k2),
                                 reads=[B_gc[k2]], writes=[B_sgm[k2]])
                            P.op("act", (lambda e, f, bu, k2: (lambda a: a.activation(out=ub[k2][:], in_=banks[bu][:, 0:C], func=AF.Identity,
                                                                                       bias=bu_sb[:, e, f:f + 1], scale=1.0)))(e, f, bu, k2),
                                 reads=[bankb[bu], B_par], writes=[B_ub[k2]])
                            P.op("dve", (lambda k2: (lambda v: v.tensor_scalar(out=ub[k2][:], in0=ub[k2][:], scalar1=7.0, scalar2=-7.0,
                                                                               op0=ALU.min, op1=ALU.max)))(k2), reads=[B_ub[k2]], writes=[B_ub[k2]])
                            P.op("dve", (lambda k2: (lambda v: v.tensor_tensor(out=gc[k2][:], in0=gc[k2][:], in1=sgm[k2][:], op=ALU.mult)))(k2),
                                 reads=[B_sgm[k2]], writes=[B_gc[k2]])
                            P.op("dve", (lambda f, k2: (lambda v: v.scalar_tensor_tensor(out=actT[:, f, :], in0=ub[k2][:], scalar=1.0, in1=gc[k2][:],
                                                                                          op0=ALU.add, op1=ALU.mult)))(f, k2),
                                 reads=[B_ub[k2], B_gc[k2]], writes=[B_act[f]])
                        for s in range(NS):
                            yk = (e * NS + s) % 2
                            for hh in range(2):
                                bk = 6 + hh
                                for f in range(8):
                                    P.op("pe", (lambda i, s, hh, f, bk: (lambda pe: pe.matmul(banks[bk][:], lhsT=actT[:, f, s * 128:(s + 1) * 128],
                                                                                               rhs=wd[i][:, f, hh * 512:(hh + 1) * 512],
                                                                                               start=(f == 0), stop=(f == 7))))(i, s, hh, f, bk),
                                         reads=[B_wd[i]] + B_act, writes=[bankb[bk]])
                                P.op("dve", (lambda i, yk, hh, bk: (lambda v: v.tensor_tensor(out=yo[yk][:, hh * 512:(hh + 1) * 512], in0=banks[bk][:],
                                                                                               in1=bd[i][:, hh * 512:(hh + 1) * 512], op=ALU.add)))(i, yk, hh, bk),
                                     reads=[bankb[bk], B_bd[i]], writes=[B_yo[yk]], indep=(hh == 1))
                            P.dma("sync", (lambda e, s, yk: (lambda q: q.dma_start(out=ys[e * C + s * 128:e * C + (s + 1) * 128, :], in_=yo[yk][:])))(e, s, yk),
                                  reads=[B_yo[yk]], writes=[B_ys], sembuf=B_yo[yk], indep=True)
                    P.close(me_bufs)

                with contextlib.ExitStack() as me:
                    cb_bufs = []

                    def esb(name, shape, dt=F32):
                        return sb(name, shape, dt, st=me)

                    vecs2 = esb("vecs2", [128, 5, D])
                    B_vecs2 = P.buf("vecs2", cb_bufs)
                    P.dma("sync", lambda q: q.dma_start(out=vecs2[:], in_=vec_bc.rearrange("v p d -> p v d")), writes=[B_vecs2])
                    gth = [[esb("gth%d_%d" % (i, k), [128, D]) for k in range(4)] for i in range(2)]
                    B_gth = [[P.buf("gth%d_%d" % (i, k), cb_bufs) for k in range(4)] for i in range(2)]
                    for i in range(2):
                        for k in range(4):
                            P.op("pool", (lambda i, k: (lambda g: g.memset(gth[i][k][:], 0.0)))(i, k), writes=[B_gth[i][k]])
                    for p in range(NTO):
                        i = p % 2
                        x1, bx1 = x1t[i], B_x1t[i]
                        tt, btt = tmp_t[i], B_tmp[i]
                        P.dma("sync", (lambda p, x1: (lambda q: q.dma_start(out=x1[:], in_=x1d[p * 128:(p + 1) * 128, :])))(p, x1),
                              reads=[B_x1d], writes=[bx1])
                        for k in range(4):
                            P.dma("pool", (lambda p, k, i: (lambda g: g.indirect_dma_start(
                                out=gth[i][k][:, :], out_offset=None, in_=ys[:, :],
                                in_offset=bass.IndirectOffsetOnAxis(ap=slots[:, p, k:k + 1], axis=0),
                                bounds_check=P.env["bc_reg%d" % LI], oob_is_err=False)))(p, k, i),
                                  reads=[B_ys, B_slots], writes=[B_gth[i][k]])
                        P.op("dve", (lambda x1, tt: (lambda v: v.tensor_scalar(out=tt[:], in0=x1[:], scalar1=ALPHA, scalar2=None, op0=ALU.mult)))(x1, tt),
                             reads=[bx1], writes=[btt])
                        for k in range(4):
                            P.op("dve", (lambda p, k, i, tt: (lambda v: v.scalar_tensor_tensor(out=tt[:], in0=gth[i][k][:], scalar=gates[:, p, k:k + 1],
                                                                                                in1=tt[:], op0=ALU.mult, op1=ALU.add)))(p, k, i, tt),
                                 reads=[B_gth[i][k], B_gates], writes=[btt])
                        layer_norm(tt, btt, x1, bx1, 3, vecs2, B_vecs2)
                        final_ops.append(P.dma("sync", (lambda p, x1: (lambda q: q.dma_start(out=dst[p * 128:(p + 1) * 128, :], in_=x1[:])))(p, x1),
                                               reads=[bx1], writes=[B_dst], sembuf=bx1, indep=True))
                    P.close(cb_bufs)
                P.close(top_bufs)

        x_ext0 = nc.dram_tensor("x_ext", [S_OWN + 4 * HALO, D], F32, kind="ExternalInput").ap()
        y_out = nc.dram_tensor("y_out", [S_OWN, D], F32, kind="ExternalOutput").ap()
        xmid = nc.dram_tensor("xmid", [S_OWN + 2 * HALO, D], F32, kind="Internal").ap()
        B_xmid = Buf("xmid")
        B_yout = Buf("y_out")
        emit_layer(0, S_OWN + 2 * HALO, 512, True, x_ext0, [], xmid, B_xmid)
        emit_layer(1, S_OWN, 384, False, xmid, [B_xmid], y_out, B_yout)
        P.emit(final_ops)
    return nc


def _static_tables(n_out, seg, ext, nseg_rows=128):
    NTO = n_out // 128
    t0 = seg * S_OWN - (HALO if ext else 0)
    r_base = t0 // GRID_W
    rows = nseg_rows
    S = rows * GRID_W
    rowbias = np.zeros((128, NTO, 7, 2), np.float32)
    for p in range(NTO):
        for j in range(7):
            for kr2 in range(2):
                kr = r_base + 2 * p - 6 + 2 * j + kr2
                for rr in range(2):
                    r = r_base + 2 * p + rr
                    sr = min(max(r - 4, 0), rows - 8)
                    ok = (0 <= kr < rows) and (sr <= kr < sr + 8)
                    if not ok:
                        rowbias[kr2 * 64:(kr2 + 1) * 64, p, j, rr] = NEG
    n_in = n_out + 2 * HALO
    x0 = t0 - HALO
    if ext:
        regs = ((0, HALO), (HALO, 2 * HALO), (n_in - 2 * HALO, n_in - HALO), (n_in - HALO, n_in))
    else:
        regs = ((0, HALO), (0, 0), (0, 0), (n_in - HALO, n_in))
    flags = np.ones((128, 4), np.float32)
    for i, (a, b) in enumerate(regs):
        if b > a and (x0 + a < 0 or x0 + b > S):
            flags[:, i] = 0.0
    poolcorr = np.ones((128, 2, 16), np.float32)
    wins = (2, 4, 8, 16)
    cp0, cp1 = (HALO, n_out - HALO - 8) if ext else (0, n_out - 8)
    for g, w in enumerate(wins):
        pc, half = g // 2, g % 2
        for i in range(8):
            for (pos, t) in ((i, t0 + cp0 + i), (8 + i, t0 + cp1 + i)):
                lo = min(max(t - w // 2, 0), S)
                hi = min(max(t + w // 2, 0), S)
                if hi > lo:
                    poolcorr[half * 64:(half + 1) * 64, pc, pos] = np.float32(w) / np.float32(hi - lo)
    tok = t0 + np.arange(NTO)[None, :] * 128 + np.arange(128)[:, None]
    ok = (tok >= 0) & (tok < S)
    tokval = np.stack([ok.astype(np.float32), np.where(ok, 0.0, 1.0e6).astype(np.float32)], 1)
    return rowbias.reshape(128, NTO * 14), flags, poolcorr, np.ascontiguousarray(tokval)


def _bias_index():
    j = np.arange(7)[:, None, None]
    key = np.arange(128)[None, :, None]
    q = np.arange(128)[None, None, :]
    kr2, kc = key // 64, key % 64
    rr, c = q // 64, q % 64
    dr = (2 * j + kr2 - 6) - rr
    dc = kc - c
    sc = np.clip(c - 8, 0, GRID_W - 16)
    valid = (kc >= sc) & (kc < sc + 16) & (np.abs(dr) <= 7) & (np.abs(dc) <= 15)
    ri = np.clip(dr + 7, 0, 14)
    ci = np.clip(dc + 15, 0, 30)
    ri, ci, valid = np.broadcast_arrays(ri, ci, valid)
    return ri, ci, valid


_PROG_CACHE = {}
LAYER_CFG = ((S_OWN + 2 * HALO, 512, True), (S_OWN, 384, False))


def _get_prog():
    if "full" not in _PROG_CACHE:
        _PROG_CACHE["full"] = build_program()
    return _PROG_CACHE["full"]


def _layer_common(l, P, C):
    f32 = np.float32
    ri, ci, valid = _bias_index()
    rpb = P["rpb"][l]
    biasT = np.where(valid[None], rpb[:, ri, ci], f32(NEG)).astype(f32)
    w_pool = P["w_pool"][l]
    wpool_bd = np.zeros((2, 128, 128), f32)
    for g in range(4):
        pc, half = g // 2, g % 2
        wpool_bd[pc, half * 64:(half + 1) * 64, half * 64:(half + 1) * 64] = w_pool[g]
    return {
        "w_in": np.ascontiguousarray(P["w_in"][l]),
        "b_in_pc": np.ascontiguousarray(P["b_in"][l].reshape(18, 128).T),
        "bv_bc": np.ascontiguousarray(np.broadcast_to(P["b_in"][l][1280:1792], (128, 512))),
        "wpool_bd": wpool_bd,
        "pool_scale_pc": np.ascontiguousarray(P["pool_scale"][l].reshape(2, 128).T),
        "biasT": biasT,
        "conv_dw_pc": np.ascontiguousarray(P["conv_dw"][l][:, 0, :].reshape(31, 2, 128).transpose(2, 1, 0)),
        "conv_vec_pc": np.ascontiguousarray(np.stack([P["conv_dw_b"][l], P["conv_ln_g"][l], P["conv_ln_b"][l],
                                                      P["b_conv_pw"][l]], 0).reshape(4, 2, 128).transpose(2, 0, 1)),
        "w_pw": np.ascontiguousarray(P["w_conv_pw"][l]),
        "w_out": np.ascontiguousarray(P["w_out"][l]),
        "vec_bc": np.ascontiguousarray(np.broadcast_to(
            np.stack([P["b_out"][l], P["ln1_g"][l], P["ln1_b"][l], P["ln2_g"][l], P["ln2_b"][l]], 0)[:, None, :], (5, 128, D))),
        "w_router": np.ascontiguousarray(P["w_router"][l]),
        "b_router_bc": np.ascontiguousarray(np.broadcast_to(P["b_router"][l], (128, NE))),
        "ec_bc": np.ascontiguousarray(np.broadcast_to((np.arange(NE) * C).astype(f32), (128, NE))),
        "b_gate_pc": np.ascontiguousarray(P["b_gate"][l].reshape(NE, 8, 128).transpose(2, 0, 1)),
        "b_up_pc": np.ascontiguousarray(P["b_up"][l].reshape(NE, 8, 128).transpose(2, 0, 1)),
        "w_gate": np.ascontiguousarray(P["w_gate"][l]),
        "w_up": np.ascontiguousarray(P["w_up"][l]),
        "w_down": np.ascontiguousarray(P["w_down"][l]),
        "b_down": np.ascontiguousarray(P["b_down"][l]),
    }


def _in_maps(P):
    f32 = np.float32
    x_full = P["x"]
    B, S, _ = x_full.shape
    nseg = S // S_OWN
    shared = {}
    for l, (n_out, C, ext) in enumerate(LAYER_CFG):
        for k, v in _layer_common(l, P, C).items():
            shared["%s_%d" % (k, l)] = v
    in_maps = []
    for core in range(8):
        b, seg = core // nseg, core % nseg
        t0 = seg * S_OWN
        m = dict(shared)
        xe = np.zeros((S_OWN + 4 * HALO, D), f32)
        lo, hi = max(t0 - 2 * HALO, 0), min(t0 + S_OWN + 2 * HALO, S)
        xe[lo - (t0 - 2 * HALO):hi - (t0 - 2 * HALO)] = x_full[b, lo:hi]
        m["x_ext"] = xe
        for l, (n_out, C, ext) in enumerate(LAYER_CFG):
            rowbias, flags, poolcorr, tokval = _static_tables(n_out, seg, ext)
            m["tokval_%d" % l] = tokval
            m["rowbias_%d" % l] = rowbias
            m["flags_%d" % l] = flags
            m["poolcorr_%d" % l] = poolcorr
        in_maps.append(m)
    return in_maps


def kernel(**inputs):
    P = {k: np.asarray(v, dtype=np.float32) for k, v in inputs.items()}
    x = P["x"]
    B, S, _ = x.shape
    nc = _get_prog()
    res = run_bass_kernel_spmd(nc, _in_maps(P), core_ids=list(range(8)))
    out = np.empty_like(x)
    nseg = S // S_OWN
    for core in range(8):
        b, seg = core // nseg, core % nseg
        out[b, seg * S_OWN:(seg + 1) * S_OWN] = res.results[core]["y_out"]
    return out
```

```python
import numpy as np
import concourse.bass as bass
import concourse.mybir as mybir
from concourse.bass_utils import run_bass_kernel_spmd

F32 = mybir.dt.float32
BF16 = mybir.dt.bfloat16
I32 = mybir.dt.int32
AF = mybir.ActivationFunctionType
ALU = mybir.AluOpType

D = 1024
NE = 32
S_OWN = 2048
HALO = 256
GRID_W = 64
ALPHA = (2.0 * 2) ** 0.25
EPS = 1e-5
NEG = -1e30
DIN = 2304


class Buf:
    __slots__ = ("name", "ws", "reads", "sem", "cum")

    def __init__(self, name):
        self.name = name
        self.ws = []
        self.reads = []
        self.sem = None
        self.cum = 0


class Op:
    __slots__ = ("eng", "fn", "deps", "is_dma", "sem", "val", "signal", "seq")

    def __init__(self, eng, fn, is_dma):
        self.seq = 0
        self.eng = eng
        self.fn = fn
        self.deps = []
        self.is_dma = is_dma
        self.sem = None
        self.val = 0
        self.signal = False


class Prog:
    ENG = ("sync", "act", "dve", "pe", "pool")

    def __init__(self, nc):
        self.nc = nc
        self.ops = {e: [] for e in self.ENG}
        self.dma_sems = []
        self.fence_k = {}
        self.seq = 0
        self.marks = {}
        self.limit = None
        self.env = {"bc_vals": {}}

    def mark(self, name):
        self.marks[name] = self.seq

    def buf(self, name, scope=None):
        b = Buf(name)
        b.reads = list(self.fence_k.values())
        if scope is not None:
            scope.append(b)
        return b

    def close(self, scope_bufs):
        for b in scope_bufs:
            for o in list(b.ws) + list(b.reads):
                if o.is_dma:
                    k = ("dma", id(o.sem))
                    if k not in self.fence_k or self.fence_k[k].val < o.val:
                        self.fence_k[k] = o
                else:
                    k = ("eng", o.eng)
                    if k not in self.fence_k or self.fence_k[k].seq < o.seq:
                        self.fence_k[k] = o

    def _deps(self, op, reads, writes, indep=False):
        deps = []
        for b in reads:
            deps.extend(b.ws)
        for b in writes:
            if not indep:
                deps.extend(b.ws)
            deps.extend(b.reads)
        seen = set()
        for d in deps:
            if d.is_dma or op.is_dma or d.eng != op.eng or op.eng != "pe":
                if id(d) not in seen:
                    seen.add(id(d))
                    op.deps.append(d)
                d.signal = True
        for b in writes:
            if indep:
                b.ws.append(op)
            else:
                b.ws = [op]
                b.reads = []
        for b in reads:
            if op not in b.ws:
                b.reads.append(op)

    def op(self, eng, fn, reads=(), writes=(), indep=False):
        o = Op(eng, fn, False)
        self.seq += 1
        o.seq = self.seq
        self._deps(o, reads, writes, indep)
        self.ops[eng].append(o)
        return o

    def dma(self, eng, fn, reads=(), writes=(), sembuf=None, indep=False):
        o = Op(eng, fn, True)
        self.seq += 1
        o.seq = self.seq
        sb = sembuf if sembuf is not None else writes[0]
        if sb.sem is None:
            sb.sem = ("dma", len(self.dma_sems))
            self.dma_sems.append(sb)
        sb.cum += 16
        o.sem = sb
        o.val = sb.cum
        o.signal = True
        self._deps(o, reads, writes, indep)
        self.ops[eng].append(o)
        return o

    def emit(self, final_waits):
        nc = self.nc
        if self.limit is not None:
            lim = self.marks.get(self.limit, self.limit)
            lim = int(lim)
            for e in self.ENG:
                self.ops[e] = [o for o in self.ops[e] if o.seq <= lim]
            final_waits = [o for e in self.ENG for o in self.ops[e] if o.is_dma]
            for e in self.ENG:
                if self.ops[e] and not self.ops[e][-1].is_dma:
                    self.ops[e][-1].signal = True
                    final_waits.append(self.ops[e][-1])
        for e in self.ENG:
            c = 0
            for o in self.ops[e]:
                if not o.is_dma and o.signal:
                    c += 1
                    o.val = c
        import contextlib
        with contextlib.ExitStack() as st:
            esem = {e: st.enter_context(nc.semaphore("es_" + e)) for e in self.ENG}
            dsem = [st.enter_context(nc.semaphore("ds_%d" % i)) for i in range(len(self.dma_sems))]
            block = st.enter_context(nc.Block())

            def semof(o):
                if o.is_dma:
                    return dsem[o.sem.sem[1]]
                return esem[o.eng]

            def run(e, eng):
                waited = {}
                if e == "pool":
                    for L, v in self.env["bc_vals"].items():
                        self.env["bc_reg%d" % L] = eng.alloc_register("bc_reg%d" % L)
                        eng.reg_mov(self.env["bc_reg%d" % L], int(v))
                for o in self.ops[e]:
                    need = {}
                    for d in o.deps:
                        s = semof(d)
                        k = id(s)
                        if k not in need or d.val > need[k][1]:
                            need[k] = (s, d.val)
                    for k, (s, v) in need.items():
                        if waited.get(k, 0) >= v:
                            continue
                        waited[k] = v
                        eng.wait_ge(s, v)
                    ins = o.fn(eng)
                    if o.is_dma:
                        ins.then_inc(semof(o), 16)
                    elif o.signal:
                        ins.then_inc(esem[e], 1)
                if e == "sync":
                    for o in final_waits:
                        eng.wait_ge(semof(o), o.val)

            block.sync(lambda eng: run("sync", eng))
            block.scalar(lambda eng: run("act", eng))
            block.vector(lambda eng: run("dve", eng))
            block.tensor(lambda eng: run("pe", eng))
            block.gpsimd(lambda eng: run("pool", eng))


def build_program(stage="full", limit=None):
    nc = bass.Bass("TRN2", target_bir_lowering=False)
    P = Prog(nc)
    P.limit = limit
    P.env["bc_vals"] = {}
    import contextlib
    with contextlib.ExitStack() as glob:
        _names = {}

        def sb_global(name, shape, dt=F32, st=glob):
            k = _names.get(name, 0)
            _names[name] = k + 1
            if k:
                name = "%s_%d" % (name, k)
            return st.enter_context(nc.sbuf_tensor(name, list(shape), dt))

        sb = sb_global
        banks = [glob.enter_context(nc.psum_tensor("bank%d" % i, [128, 512], F32)) for i in range(8)]
        bankb = [Buf("bank%d" % i) for i in range(8)]

        ident_f = sb("ident_f", [128, 128])
        ident_b = sb("ident_b", [128, 128], BF16)
        ones_b = sb("ones_b", [128, 128], BF16)
        ones256 = sb("ones256", [128, 128], BF16)
        ustrict = sb("ustrict", [128, 128], BF16)
        B_const = Buf("const")
        iota_i = sb("iota_i", [128, 128], I32)
        iota_f = sb("iota_f", [128, 128])
        pidx_i = sb("pidx_i", [128, 1], I32)
        pidx_f = sb("pidx_f", [128, 1])
        P.op("pool", lambda g: g.iota(iota_i[:], [[1, 128]], base=0, channel_multiplier=0), writes=[B_const])
        P.op("pool", lambda g: g.iota(pidx_i[:], [[0, 1]], base=0, channel_multiplier=1), writes=[B_const])
        P.op("dve", lambda v: v.tensor_copy(out=iota_f[:], in_=iota_i[:]), reads=[B_const], writes=[B_const])
        P.op("dve", lambda v: v.tensor_copy(out=pidx_f[:], in_=pidx_i[:]), reads=[B_const], writes=[B_const])
        P.op("dve", lambda v: v.tensor_scalar(out=ident_f[:], in0=iota_f[:], scalar1=pidx_f[:, 0:1], scalar2=None,
                                              op0=ALU.is_equal), reads=[B_const], writes=[B_const])
        P.op("dve", lambda v: v.tensor_copy(out=ident_b[:], in_=ident_f[:]), writes=[B_const])
        P.op("dve", lambda v: v.tensor_scalar(out=ustrict[:], in0=iota_f[:], scalar1=pidx_f[:, 0:1], scalar2=None,
                                              op0=ALU.is_gt), writes=[B_const])
        P.op("dve", lambda v: v.memset(ones_b[:], 1.0), writes=[B_const])
        P.op("dve", lambda v: v.memset(ones256[:], 1.0 / 256.0), writes=[B_const])

        final_ops = []

        def emit_layer(LI, n_out, C, ext, x_ext, src_reads, dst, B_dst):
            n_in = n_out + 2 * HALO
            NTI = n_in // 128
            NTO = n_out // 128
            HT = HALO // 128
            NS = C // 128
            NSLOT = NE * C
            P.env["bc_vals"][LI] = NSLOT - 1
            if ext:
                FLAG_REG = ((0, HALO, 0), (HALO, 2 * HALO, 1), (n_in - 2 * HALO, n_in - HALO, 2), (n_in - HALO, n_in, 3))
                CP0, CP1 = HALO, n_out - HALO - 8
            else:
                FLAG_REG = ((0, HALO, 0), (n_in - HALO, n_in, 3))
                CP0, CP1 = 0, n_out - 8

            def din(name, shape, dt=F32):
                return nc.dram_tensor("%s_%d" % (name, LI), list(shape), dt, kind="ExternalInput").ap()

            w_in = din("w_in", [D, DIN])
            b_in_pc = din("b_in_pc", [128, 18])
            bv_bc = din("bv_bc", [128, 512])
            wpool_bd = din("wpool_bd", [2, 128, 128])
            pool_scale_pc = din("pool_scale_pc", [128, 2])
            poolcorr = din("poolcorr", [128, 2, 16])
            flags = din("flags", [128, 4])
            biasT = din("biasT", [8, 7, 128, 128])
            rowbias = din("rowbias", [128, NTO * 14])
            tokval = din("tokval", [128, 2, NTO])
            conv_dw_pc = din("conv_dw_pc", [128, 2, 31])
            conv_vec_pc = din("conv_vec_pc", [128, 4, 2])
            w_pw = din("w_pw", [256, 256])
            w_out = din("w_out", [D, D])
            vec_bc = din("vec_bc", [5, 128, D])
            w_router = din("w_router", [D, NE])
            b_router_bc = din("b_router_bc", [128, NE])
            ec_bc = din("ec_bc", [128, NE])
            b_gate_pc = din("b_gate_pc", [128, NE, 8])
            b_up_pc = din("b_up_pc", [128, NE, 8])
            if True:
                w_gate = din("w_gate", [NE, D, D])
                w_up = din("w_up", [NE, D, D])
                w_down = din("w_down", [NE, D, D])
                b_down = din("b_down", [NE, D])
            x1d = nc.dram_tensor("x1d_%d" % LI, [n_out, D], F32, kind="Internal").ap()
            xs = nc.dram_tensor("xs_%d" % LI, [NSLOT, D], BF16, kind="Internal").ap()
            ys = nc.dram_tensor("ys_%d" % LI, [NSLOT, D], F32, kind="Internal").ap()
            with contextlib.ExitStack() as top:
                top_bufs = []

                def Buf_(name):
                    return P.buf(name, top_bufs)

                def sb(name, shape, dt=F32, st=top):
                    return sb_global(name, shape, dt, st)

                B_par = Buf_("params")
                b_in_sb = sb("b_in_sb", [128, 18])
                flags_sb = sb("flags_sb", [128, 4])
                pscale_sb = sb("pscale_sb", [128, 2])
                pcorr_sb = sb("pcorr_sb", [128, 2, 16])
                rowb_sb = sb("rowb_sb", [128, NTO * 14])
                tokv_sb = sb("tokv_sb", [128, 2, NTO])
                cdw_sb = sb("cdw_sb", [128, 2, 31])
                cvec_sb = sb("cvec_sb", [128, 4, 2])
                brt_sb = sb("brt_sb", [128, NE])
                ec_sb = sb("ec_sb", [128, NE])
                bg_sb = sb("bg_sb", [128, NE, 8])
                bu_sb = sb("bu_sb", [128, NE, 8])
                for pdst, psrc in ((b_in_sb, b_in_pc), (flags_sb, flags), (pscale_sb, pool_scale_pc), (pcorr_sb, poolcorr),
                                 (rowb_sb, rowbias), (tokv_sb, tokval), (cdw_sb, conv_dw_pc), (cvec_sb, conv_vec_pc), (brt_sb, b_router_bc),
                                 (ec_sb, ec_bc), (bg_sb, b_gate_pc), (bu_sb, b_up_pc)):
                    P.dma("sync", (lambda d, s: (lambda q: q.dma_start(out=d[:], in_=s)))(pdst, psrc), writes=[B_par], indep=True)

                x1t = [sb("x1t%d" % i, [128, D]) for i in range(2)]
                B_x1t = [Buf_("x1t0"), Buf_("x1t1")]
                stats = sb("stats", [128, 2, 6])
                mv = sb("mv", [128, 2])
                rstd1 = sb("rstd1", [128, 1])
                B_st = Buf_("stats")

                x1b = [sb("x1b%d" % i, [128, D], BF16) for i in range(2)]
                B_x1b = [Buf_("x1b0"), Buf_("x1b1")]
                x1T = sb("x1T", [128, 8, 128])
                B_x1T = Buf_("x1T")
                wr_sb = sb("wr_sb", [128, 8, NE])
                B_wr = Buf_("wr")
                logit = sb("logit", [128, NE])
                top8 = sb("top8", [128, 8])
                negv0 = sb("negv0", [128, 1])
                ex4 = sb("ex4", [128, 4])
                ssum = sb("ssum", [128, 1])
                gates = sb("gates", [128, NTO, 4])
                mask_all = sb("mask_all", [128, NTO, NE], BF16)
                Atab = sb("Atab", [128, NE])
                ovf = sb("ovf", [128, NE])
                junk = sb("junk", [128, NE])
                slot_f = sb("slot_f", [128, 4])
                slots = sb("slots", [128, NTO, 4], I32)
                B_rt, B_gates, B_mask, B_slots = Buf_("rt"), Buf_("gates"), Buf_("mask"), Buf_("slots")
                B_x1d = Buf_("x1d")
                B_xs = Buf_("xs")
                tmp_t = [sb("tmp_t%d" % i, [128, D]) for i in range(2)]
                B_tmp = [Buf_("tmp0"), Buf_("tmp1")]

                with contextlib.ExitStack() as mx:
                    mx_bufs = []

                    def msb(name, shape, dt=F32):
                        return sb(name, shape, dt, st=mx)

                    ycatT = msb("ycatT", [128, 8, n_out], BF16)
                    B_ycat = [P.buf("ycat%d" % c, mx_bufs) for c in range(8)]
                    xin = [msb("xin%d" % i, [128, D]) for i in range(2)]
                    B_xin = [P.buf("xin%d" % i, mx_bufs) for i in range(2)]
                    B_xs0 = Buf_("xs_zero")
                    if stage == "full":
                        P.op("pool", lambda g: g.memset(ycatT[:], 0.0), writes=B_ycat)
                        zsrc = ycatT[:].rearrange("p c n -> p (c n)")
                        per = (8 * n_out) // D
                        for r0 in range(0, NSLOT // 128, per):
                            nr = min(per, NSLOT // 128 - r0)
                            P.dma("sync", (lambda r0, nr: (lambda q: q.dma_start(
                                out=xs[r0 * 128:(r0 + nr) * 128, :].rearrange("(s p) d -> p s d", p=128),
                                in_=zsrc[:, 0:nr * D].rearrange("p (s d) -> p s d", d=D))))(r0, nr),
                                  reads=B_ycat, writes=[B_xs0], indep=True)
                    w_in_v = w_in.rearrange("(c p) f -> p c f", p=128)
                    tblocks = [(s, min(512, n_in - s)) for s in range(0, n_in, 512)]

                    with contextlib.ExitStack() as sa:
                        sa_bufs = []
                        xT = sb("xT", [128, 8, n_in], BF16, st=sa)
                        B_xT = [P.buf("xT%d" % t, sa_bufs) for t in range(NTI)]

                        for t in range(NTI):
                            xi, bxi = xin[t % 2], B_xin[t % 2]
                            P.dma("sync", (lambda t, xi: (lambda q: q.dma_start(out=xi[:], in_=x_ext[t * 128:(t + 1) * 128, :])))(t, xi),
                                  reads=src_reads, writes=[bxi])
                            for hh in range(2):
                                bk = (t % 2) * 2 + hh
                                for c4 in range(4):
                                    c = hh * 4 + c4
                                    P.op("pe", (lambda xi, c, c4, bk: (lambda pe: pe.transpose(out=banks[bk][:, c4 * 128:(c4 + 1) * 128],
                                                                                                in_=xi[:, c * 128:(c + 1) * 128],
                                                                                                identity=ident_f[:])))(xi, c, c4, bk),
                                         reads=[bxi, B_const], writes=[bankb[bk]])
                                if hh == 0:
                                    P.op("act", (lambda t, hh, bk: (lambda a: a.copy(
                                        out=xT[:, hh * 4:(hh + 1) * 4, t * 128:(t + 1) * 128],
                                        in_=banks[bk][:].rearrange("p (c f) -> p c f", c=4))))(t, hh, bk),
                                         reads=[bankb[bk]], writes=[B_xT[t]])
                                else:
                                    P.op("dve", (lambda t, hh, bk: (lambda v: v.tensor_copy(
                                        out=xT[:, hh * 4:(hh + 1) * 4, t * 128:(t + 1) * 128],
                                        in_=banks[bk][:].rearrange("p (c f) -> p c f", c=4))))(t, hh, bk),
                                         reads=[bankb[bk]], writes=[B_xT[t]])

                        P.mark('phase0')

                        def xt_bufs(s, n):
                            return [B_xT[t] for t in range(s // 128, (s + n + 127) // 128)]

                        def inproj(wt, wcol, bw, s, n, bk):
                            for dch in range(8):
                                P.op("pe", (lambda dch: (lambda pe: pe.matmul(banks[bk][:, 0:n],
                                                                               lhsT=wt[:, dch, wcol:wcol + 128],
                                                                               rhs=xT[:, dch, s:s + n],
                                                                               start=(dch == 0), stop=(dch == 7))))(dch),
                                     reads=[bw] + xt_bufs(s, n), writes=[bankb[bk]])

                        with contextlib.ExitStack() as sp:
                            sp_bufs = []
                            wsl_p = sb("wsl_p", [128, 8, 256], BF16, st=sp)
                            B_wslp = P.buf("wsl_p", sp_bufs)
                            P.dma("pool", lambda g: g.dma_start(out=wsl_p[:], in_=w_in_v[:, :, 0:256]), writes=[B_wslp])
                            wpool_sb = sb("wpool_sb", [128, 2, 128], BF16, st=sp)
                            B_wp = P.buf("wpool", sp_bufs)
                            P.dma("pool", lambda g: g.dma_start(out=wpool_sb[:], in_=wpool_bd.rearrange("c p f -> p c f")), writes=[B_wp])
                            PADP = 16
                            uT = sb("uT", [128, n_in + PADP], st=sp)
                            aT = sb("aT", [128, n_in + PADP], st=sp)
                            a2T = sb("a2T", [128, n_in + PADP], st=sp)
                            mT = sb("mT", [128, n_out], st=sp)
                            dT = sb("dT", [128, n_out], BF16, st=sp)
                            B_u, B_a, B_a2, B_m, B_d = (P.buf(nm, sp_bufs) for nm in ("uT", "aT", "a2T", "mT", "dT"))
                            for pc in range(2):
                                P.op("dve", lambda v: v.memset(uT[:, n_in:n_in + PADP], 0.0), writes=[B_u])
                                for bi, (s, n) in enumerate(tblocks):
                                    bk = 4 + (bi % 2)
                                    inproj(wsl_p, pc * 128, B_wslp, s, n, bk)
                                    P.op("act", (lambda s, n, bk, pc: (lambda a: a.activation(out=uT[:, s:s + n], in_=banks[bk][:, 0:n],
                                                                                               func=AF.Identity, bias=b_in_sb[:, pc:pc + 1],
                                                                                               scale=1.0)))(s, n, bk, pc),
                                         reads=[bankb[bk], B_par], writes=[B_u])
                                for (fa, fb, fc) in FLAG_REG:
                                    P.op("dve", (lambda fa, fb, fc: (lambda v: v.tensor_scalar(out=uT[:, fa:fb], in0=uT[:, fa:fb],
                                                                                               scalar1=flags_sb[:, fc:fc + 1], scalar2=None,
                                                                                               op0=ALU.mult)))(fa, fb, fc),
                                         reads=[B_par], writes=[B_u])
                                L = n_in
                                P.op("dve", lambda v: v.tensor_tensor(out=aT[:, 0:L], in0=uT[:, 0:L], in1=uT[:, 1:L + 1], op=ALU.add),
                                     reads=[B_u], writes=[B_a])
                                P.op("dve", lambda v: v.memset(aT[:, L:L + PADP], 0.0), writes=[B_a])
                                P.op("dve", lambda v: v.tensor_tensor(out=a2T[:, 0:L], in0=aT[:, 0:L], in1=aT[:, 2:L + 2], op=ALU.add),
                                     reads=[B_a], writes=[B_a2])
                                P.op("dve", lambda v: v.memset(a2T[:, L:L + PADP], 0.0), writes=[B_a2])
                                o0 = HALO
                                if pc == 0:
                                    P.op("dve", lambda v: v.tensor_scalar(out=mT[0:64, :], in0=aT[0:64, o0 - 1:o0 - 1 + n_out], scalar1=0.5,
                                                                          scalar2=None, op0=ALU.mult), reads=[B_a], writes=[B_m])
                                    P.op("dve", lambda v: v.tensor_scalar(out=mT[64:128, :], in0=a2T[64:128, o0 - 2:o0 - 2 + n_out],
                                                                          scalar1=0.25, scalar2=None, op0=ALU.mult), reads=[B_a2], writes=[B_m])
                                else:
                                    P.op("dve", lambda v: v.tensor_tensor(out=aT[:, 0:L], in0=a2T[:, 0:L], in1=a2T[:, 4:L + 4], op=ALU.add),
                                         reads=[B_a2], writes=[B_a])
                                    P.op("dve", lambda v: v.tensor_tensor(out=a2T[:, 0:L], in0=aT[:, 0:L], in1=aT[:, 8:L + 8], op=ALU.add),
                                         reads=[B_a], writes=[B_a2])
                                    P.op("dve", lambda v: v.tensor_scalar(out=mT[0:64, :], in0=aT[0:64, o0 - 4:o0 - 4 + n_out], scalar1=0.125,
                                                                          scalar2=None, op0=ALU.mult), reads=[B_a], writes=[B_m])
                                    P.op("dve", lambda v: v.tensor_scalar(out=mT[64:128, :], in0=a2T[64:128, o0 - 8:o0 - 8 + n_out],
                                                                          scalar1=0.0625, scalar2=None, op0=ALU.mult), reads=[B_a2], writes=[B_m])
                                P.op("dve", (lambda pc: (lambda v: v.tensor_tensor(out=mT[:, CP0:CP0 + 8], in0=mT[:, CP0:CP0 + 8], in1=pcorr_sb[:, pc, 0:8],
                                                                                    op=ALU.mult)))(pc), reads=[B_par], writes=[B_m])
                                P.op("dve", (lambda pc: (lambda v: v.tensor_tensor(out=mT[:, CP1:CP1 + 8], in0=mT[:, CP1:CP1 + 8],
                                                                                    in1=pcorr_sb[:, pc, 8:16], op=ALU.mult)))(pc),
                                     reads=[B_par], writes=[B_m])
                                P.op("dve", lambda v: v.tensor_tensor(out=dT[:], in0=mT[:], in1=uT[:, o0:o0 + n_out], op=ALU.subtract),
                                     reads=[B_m, B_u], writes=[B_d])
                                for bi in range(n_out // 512):
                                    bk = 6 + (bi % 2)
                                    P.op("pe", (lambda bi, bk, pc: (lambda pe: pe.matmul(banks[bk][:], lhsT=wpool_sb[:, pc, :],
                                                                                          rhs=dT[:, bi * 512:(bi + 1) * 512],
                                                                                          start=True, stop=True)))(bi, bk, pc),
                                         reads=[B_wp, B_d], writes=[bankb[bk]])
                                    P.op("act", (lambda bi, bk, pc: (lambda a: a.activation(out=ycatT[:, pc, bi * 512:(bi + 1) * 512],
                                                                                             in_=banks[bk][:], func=AF.Copy,
                                                                                             scale=pscale_sb[:, pc:pc + 1])))(bi, bk, pc),
                                         reads=[bankb[bk], B_par], writes=[B_ycat[pc]])
                        P.close(sp_bufs)
                        P.mark('pool')

                        with contextlib.ExitStack() as sc:
                            sc_bufs = []
                            wsl_c = sb("wsl_c", [128, 8, 512], BF16, st=sc)
                            B_wslc = P.buf("wsl_c", sc_bufs)
                            P.dma("pool", lambda g: g.dma_start(out=wsl_c[:], in_=w_in_v[:, :, 1792:2304]), writes=[B_wslc])
                            wpw_sb = sb("wpw_sb", [128, 2, 256], BF16, st=sc)
                            B_wpw = P.buf("wpw", sc_bufs)
                            P.dma("pool", lambda g: g.dma_start(out=wpw_sb[:], in_=w_pw.rearrange("(c p) f -> p c f", p=128)), writes=[B_wpw])
                            hT = sb("hT", [128, 2, n_in], BF16, st=sc)
                            B_h = [P.buf("hT0", sc_bufs), P.buf("hT1", sc_bufs)]
                            sg = sb("sg", [128, 512], st=sc)
                            B_sg = P.buf("sg", sc_bufs)
                            for j in range(2):
                                for bi, (s, n) in enumerate(tblocks):
                                    inproj(wsl_c, 256 + j * 128, B_wslc, s, n, 4)
                                    P.op("act", (lambda s, n, j: (lambda a: a.activation(out=sg[:, 0:n], in_=banks[4][:, 0:n], func=AF.Sigmoid,
                                                                                         bias=b_in_sb[:, 16 + j:17 + j], scale=1.0)))(s, n, j),
                                         reads=[bankb[4], B_par], writes=[B_sg])
                                    inproj(wsl_c, j * 128, B_wslc, s, n, 5)
                                    P.op("dve", (lambda s, n, j: (lambda v: v.scalar_tensor_tensor(out=hT[:, j, s:s + n], in0=banks[5][:, 0:n],
                                                                                                    scalar=b_in_sb[:, 14 + j:15 + j],
                                                                                                    in1=sg[:, 0:n], op0=ALU.add,
                                                                                                    op1=ALU.mult)))(s, n, j),
                                         reads=[bankb[5], B_sg, B_par], writes=[B_h[j]])
                                for (fa, fb, fc) in FLAG_REG:
                                    P.op("dve", (lambda j, fa, fb, fc: (lambda v: v.tensor_scalar(out=hT[:, j, fa:fb], in0=hT[:, j, fa:fb],
                                                                                                  scalar1=flags_sb[:, fc:fc + 1], scalar2=None,
                                                                                                  op0=ALU.mult)))(j, fa, fb, fc),
                                         reads=[B_par], writes=[B_h[j]])
                            dg = sb("dg", [128, 2, 31, 128], BF16, st=sc)
                            B_dg = P.buf("dg", sc_bufs)
                            for j in range(2):
                                for k in range(31):
                                    P.op("dve", (lambda j, k: (lambda v: v.tensor_scalar(out=dg[:, j, k, :], in0=ident_f[:],
                                                                                         scalar1=cdw_sb[:, j, k:k + 1], scalar2=None,
                                                                                         op0=ALU.mult)))(j, k),
                                         reads=[B_par, B_const], writes=[B_dg])
                            cT = sb("cT", [128, 2, 512], st=sc)
                            cTb = sb("cTb", [128, 2, 512], BF16, st=sc)
                            sqb = sb("sqb", [128, 2, 512], BF16, st=sc)
                            mean_sb = sb("mean_sb", [128, 512], st=sc)
                            m2_sb = sb("m2_sb", [128, 512], st=sc)
                            rstd_sb = sb("rstd_sb", [128, 512], st=sc)
                            nrm = sb("nrm", [128, 2, 512], st=sc)
                            silu_b = sb("silu_b", [128, 2, 512], BF16, st=sc)
                            B_cT, B_cTb, B_sq, B_mean, B_m2, B_rstd, B_nrm, B_silu = (P.buf(nm, sc_bufs) for nm in
                                                                                      ("cT", "cTb", "sqb", "mean", "m2", "rstd", "nrm", "silu"))
                            for bi in range(n_out // 512):
                                t0 = HALO + bi * 512
                                for j in range(2):
                                    bk = 6 + j
                                    for k in range(31):
                                        P.op("pe", (lambda j, k, bk, t0: (lambda pe: pe.matmul(banks[bk][:], lhsT=dg[:, j, k, :],
                                                                                                rhs=hT[:, j, t0 + k - 15:t0 + k - 15 + 512],
                                                                                                start=(k == 0), stop=(k == 30))))(j, k, bk, t0),
                                             reads=[B_dg, B_h[j]], writes=[bankb[bk]])
                                    P.op("act", (lambda j, bk: (lambda a: a.activation(out=cT[:, j, :], in_=banks[bk][:], func=AF.Identity,
                                                                                       bias=cvec_sb[:, 0, j:j + 1], scale=1.0)))(j, bk),
                                         reads=[bankb[bk], B_par], writes=[B_cT])
                                P.op("dve", lambda v: v.tensor_copy(out=cTb[:], in_=cT[:]), reads=[B_cT], writes=[B_cTb])
                                P.op("act", lambda a: a.activation(out=sqb[:], in_=cT[:], func=AF.Square), reads=[B_cT], writes=[B_sq])
                                for j in range(2):
                                    P.op("pe", (lambda j: (lambda pe: pe.matmul(banks[4][:], lhsT=ones256[:], rhs=cTb[:, j, :],
                                                                                 start=(j == 0), stop=(j == 1))))(j),
                                         reads=[B_const, B_cTb], writes=[bankb[4]])
                                for j in range(2):
                                    P.op("pe", (lambda j: (lambda pe: pe.matmul(banks[5][:], lhsT=ones256[:], rhs=sqb[:, j, :],
                                                                                 start=(j == 0), stop=(j == 1))))(j),
                                         reads=[B_const, B_sq], writes=[bankb[5]])
                                P.op("act", lambda a: a.copy(out=mean_sb[:], in_=banks[4][:]), reads=[bankb[4]], writes=[B_mean])
                                P.op("dve", lambda v: v.tensor_tensor(out=m2_sb[:], in0=mean_sb[:], in1=mean_sb[:], op=ALU.mult),
                                     reads=[B_mean], writes=[B_m2])
                                P.op("dve", lambda v: v.scalar_tensor_tensor(out=m2_sb[:], in0=banks[5][:], scalar=EPS, in1=m2_sb[:],
                                                                             op0=ALU.add, op1=ALU.subtract), reads=[bankb[5]], writes=[B_m2])
                                P.op("act", lambda a: a.activation(out=m2_sb[:], in_=m2_sb[:], func=AF.Sqrt), reads=[B_m2], writes=[B_m2])
                                P.op("dve", lambda v: v.reciprocal(out=rstd_sb[:], in_=m2_sb[:]), reads=[B_m2], writes=[B_rstd])
                                for j in range(2):
                                    P.op("dve", (lambda j: (lambda v: v.tensor_tensor(out=nrm[:, j, :], in0=cT[:, j, :], in1=mean_sb[:],
                                                                                      op=ALU.subtract)))(j), reads=[B_cT, B_mean], writes=[B_nrm])
                                    P.op("dve", (lambda j: (lambda v: v.tensor_tensor(out=nrm[:, j, :], in0=nrm[:, j, :], in1=rstd_sb[:],
                                                                                      op=ALU.mult)))(j), reads=[B_rstd], writes=[B_nrm])
                                    P.op("act", (lambda j: (lambda a: a.activation(out=silu_b[:, j, :], in_=nrm[:, j, :], func=AF.Silu,
                                                                                   bias=cvec_sb[:, 2, j:j + 1], scale=cvec_sb[:, 1, j:j + 1])))(j),
                                         reads=[B_nrm, B_par], writes=[B_silu])
                                for cc in range(2):
                                    bk = 6 + cc
                                    for j in range(2):
                                        P.op("pe", (lambda j, cc, bk: (lambda pe: pe.matmul(banks[bk][:], lhsT=wpw_sb[:, j, cc * 128:(cc + 1) * 128],
                                                                                             rhs=silu_b[:, j, :], start=(j == 0),
                                                                                             stop=(j == 1))))(j, cc, bk),
                                             reads=[B_wpw, B_silu], writes=[bankb[bk]])
                                    P.op("act", (lambda cc, bk, bi: (lambda a: a.activation(out=ycatT[:, 6 + cc, bi * 512:(bi + 1) * 512],
                                                                                             in_=banks[bk][:], func=AF.Identity,
                                                                                             bias=cvec_sb[:, 3, cc:cc + 1], scale=1.0)))(cc, bk, bi),
                                         reads=[bankb[bk], B_par], writes=[B_ycat[6 + cc]])
                        P.close(sc_bufs)
                        P.mark('conv')

                        sidx_box = [0]

                        def attn_group(hg):
                            with contextlib.ExitStack() as sat:
                                at_bufs = []
                                wsl = sb("wsl_a", [128, 8, 3, 256], BF16, st=sat)
                                B_wsl = P.buf("wsl_a", at_bufs)
                                for qi, c0 in enumerate((256, 768, 1280)):
                                    P.dma("pool", (lambda qi, c0: (lambda g: g.dma_start(out=wsl[:, :, qi, :],
                                                                                         in_=w_in_v[:, :, c0 + hg * 256:c0 + (hg + 1) * 256])))(qi, c0),
                                          writes=[B_wsl], indep=True)
                                bv_sb = sb("bv_sb", [128, 256], st=sat)
                                B_bv = P.buf("bv", at_bufs)
                                P.dma("sync", lambda q: q.dma_start(out=bv_sb[:], in_=bv_bc[:, hg * 256:(hg + 1) * 256]), writes=[B_bv])
                                qT = sb("qT", [128, 4, n_out], BF16, st=sat)
                                kT = sb("kT", [128, 2, n_in], BF16, st=sat)
                                B_q, B_k = P.buf("qT", at_bufs), P.buf("kT", at_bufs)
                                P.op("pool", lambda g: g.memset(qT[:], 0.0), writes=[B_q])
                                wq = wsl[:, :, 0, :]
                                wk = wsl[:, :, 1, :]
                                for c in range(2):
                                    gch = 2 + hg * 2 + c
                                    for bi in range(n_out // 512):
                                        bk = 4 + (bi % 2)
                                        inproj(wq, c * 128, B_wsl, HALO + bi * 512, 512, bk)
                                        for hh2 in range(2):
                                            P.op("act", (lambda c, bi, bk, gch, hh2: (lambda a: a.activation(
                                                out=qT[hh2 * 64:(hh2 + 1) * 64, 2 * c + hh2, bi * 512:(bi + 1) * 512],
                                                in_=banks[bk][hh2 * 64:(hh2 + 1) * 64, :],
                                                func=AF.Identity, bias=b_in_sb[hh2 * 64:(hh2 + 1) * 64, gch:gch + 1],
                                                scale=1.0)))(c, bi, bk, gch, hh2),
                                                 reads=[bankb[bk], B_par], writes=[B_q])
                                    for bi, (s, n) in enumerate(tblocks):
                                        bk = 6 + (bi % 2)
                                        inproj(wk, c * 128, B_wsl, s, n, bk)
                                        P.op("dve", (lambda c, s, n, bk, gch: (lambda v: v.tensor_scalar(out=kT[:, c, s:s + n], in0=banks[bk][:, 0:n],
                                                                                                          scalar1=b_in_sb[:, gch + 4:gch + 5], scalar2=None,
                                                                                                          op0=ALU.add)))(c, s, n, bk, gch),
                                             reads=[bankb[bk], B_par], writes=[B_k])
                                P.mark('a_qk%d' % hg)
                                vaug = sb("vaug", [128, NTI, 4, 65], BF16, st=sat)
                                B_v = P.buf("vaug", at_bufs)
                                P.op("dve", lambda v: v.memset(vaug[:, :, :, 64:65], 1.0), writes=[B_v])
                                for t in range(NTI):
                                    bk = 4 + (t % 2)
                                    for dch in range(8):
                                        P.op("pe", (lambda t, dch, bk: (lambda pe: pe.matmul(banks[bk][:, 0:256], lhsT=xT[:, dch, t * 128:(t + 1) * 128],
                                                                                              rhs=wsl[:, dch, 2, :],
                                                                                              start=(dch == 0), stop=(dch == 7))))(t, dch, bk),
                                             reads=[B_wsl, B_xT[t]], writes=[bankb[bk]])
                                    P.op("dve", (lambda t, bk: (lambda v: v.tensor_tensor(out=vaug[:, t, :, 0:64],
                                                                                          in0=banks[bk][:, 0:256].rearrange("p (h d) -> p h d", h=4),
                                                                                          in1=bv_sb[:].rearrange("p (h d) -> p h d", h=4),
                                                                                          op=ALU.add)))(t, bk),
                                         reads=[bankb[bk], B_bv], writes=[B_v])
                                P.mark('a_v%d' % hg)
                                Etab = sb("Etab", [128, 4, 7, 128], BF16, st=sat)
                                B_E = P.buf("Etab", at_bufs)
                                bstage = [sb("bstage0", [128, 7, 128], st=sat)] * 2
                                B_bst = [P.buf("bst0", at_bufs)] * 2
                                for h4 in range(4):
                                    P.dma("sync", (lambda h4: (lambda q: q.dma_start(out=bstage[h4 % 2][:],
                                                                                     in_=biasT[hg * 4 + h4].rearrange("j k q -> k j q"))))(h4),
                                          writes=[B_bst[h4 % 2]])
                                    P.op("act", (lambda h4: (lambda a: a.activation(out=Etab[:, h4, :, :], in_=bstage[h4 % 2][:], func=AF.Exp)))(h4),
                                         reads=[B_bst[h4 % 2]], writes=[B_E])
                                P.mark('a_E%d' % hg)
                                pt = sb("pT", [128, 7, 512], BF16, st=sat)
                                bpt = P.buf("pT", at_bufs)
                                yat = [sb("yat%d" % i, [128, 256], BF16, st=sat) for i in range(2)]
                                B_yat = [P.buf("yat0", at_bufs), P.buf("yat1", at_bufs)]
                                rec = sb("rec", [128, 4], st=sat)
                                B_rec = P.buf("rec", at_bufs)
                                for p in range(NTO):
                                    qt = p + HT
                                    jt = [(j, qt - 3 + j) for j in range(7) if 0 <= qt - 3 + j < NTI]
                                    for (j, kt) in jt:
                                        bk = sidx_box[0] % 4
                                        sidx_box[0] += 1
                                        for h4 in range(4):
                                            ch, hp = h4 // 2, (h4 % 2) * 64
                                            P.op("pe", (lambda bk, h4, ch, hp, kt, p: (lambda pe: pe.matmul(
                                                banks[bk][:, h4 * 128:(h4 + 1) * 128],
                                                lhsT=kT[:, ch, kt * 128:(kt + 1) * 128],
                                                rhs=qT[:, h4, p * 128:(p + 1) * 128], start=True, stop=True)))(bk, h4, ch, hp, kt, p),
                                                 reads=[B_q, B_k], writes=[bankb[bk]])
                                        for rr in range(2):
                                            col = (p * 7 + j) * 2 + rr
                                            P.op("act", (lambda bk, j, rr, col: (lambda a: a.activation(
                                                out=pt[:, j, :].rearrange("p (h r c) -> p h r c", h=4, r=2)[:, :, rr, :],
                                                in_=banks[bk][:].rearrange("p (h r c) -> p h r c", h=4, r=2)[:, :, rr, :],
                                                func=AF.Exp, bias=rowb_sb[:, col:col + 1], scale=0.125)))(bk, j, rr, col),
                                                 reads=[bankb[bk], B_par], writes=[bpt])
                                        P.op("dve", (lambda j: (lambda v: v.tensor_tensor(
                                            out=pt[:, j, :].rearrange("p (h q) -> p h q", h=4),
                                            in0=pt[:, j, :].rearrange("p (h q) -> p h q", h=4),
                                            in1=Etab[:, :, j, :], op=ALU.mult)))(j),
                                             reads=[B_E], writes=[bpt])
                                    P.mark('a_S%d_%d' % (hg, p))
                                    ob = 4 + (p % 2)
                                    for h4 in range(4):
                                        for ji, (j, kt) in enumerate(jt):
                                            P.op("pe", (lambda ob, h4, j, kt, ji, nj: (lambda pe: pe.matmul(
                                                banks[ob][:, h4 * 65:(h4 + 1) * 65], lhsT=pt[:, j, h4 * 128:(h4 + 1) * 128],
                                                rhs=vaug[:, kt, h4, :], start=(ji == 0), stop=(ji == nj - 1))))(ob, h4, j, kt, ji, len(jt)),
                                                 reads=[bpt, B_v], writes=[bankb[ob]])
                                    P.mark('a_AV%d_%d' % (hg, p))
                                    ya, bya = yat[p % 2], B_yat[p % 2]
                                    P.op("dve", (lambda ob: (lambda v: v.reciprocal(
                                        out=rec[:], in_=banks[ob][:, 0:260].rearrange("p (h d) -> p h d", h=4)[:, :, 64])))(ob),
                                         reads=[bankb[ob]], writes=[B_rec])
                                    for h4 in range(4):
                                        P.op("dve", (lambda ob, h4, ya: (lambda v: v.tensor_scalar(
                                            out=ya[:, h4 * 64:(h4 + 1) * 64], in0=banks[ob][:, h4 * 65:h4 * 65 + 64],
                                            scalar1=rec[:, h4:h4 + 1], scalar2=None, op0=ALU.mult)))(ob, h4, ya),
                                             reads=[bankb[ob], B_rec], writes=[bya])
                                    P.mark('a_N%d_%d' % (hg, p))
                                    tb = 6 + (p % 2)
                                    tbv = banks[tb][:].bitcast(BF16)
                                    for c2 in range(2):
                                        P.op("pe", (lambda c2, ya, tbv: (lambda pe: pe.transpose(out=tbv[:, c2 * 128:(c2 + 1) * 128],
                                                                                                  in_=ya[:, c2 * 128:(c2 + 1) * 128],
                                                                                                  identity=ident_b[:])))(c2, ya, tbv),
                                             reads=[bya, B_const], writes=[bankb[tb]])
                                    P.op("act", (lambda p, tbv: (lambda a: a.copy(out=ycatT[:, 2 + hg * 2:4 + hg * 2, p * 128:(p + 1) * 128],
                                                                                  in_=tbv[:, 0:256].rearrange("p (c f) -> p c f", c=2))))(p, tbv),
                                         reads=[bankb[tb]], writes=[B_ycat[2 + hg]])
                            P.close(at_bufs)
                        for _hg in range(2):
                            attn_group(_hg)
                            P.mark('attn%d' % _hg)
                    P.close(sa_bufs)

                    w_out_sb = msb("w_out_sb", [128, 8, D], BF16)
                    B_wout = P.buf("w_out", mx_bufs)
                    P.dma("pool", lambda g: g.dma_start(out=w_out_sb[:], in_=w_out.rearrange("(c p) f -> p c f", p=128)),
                          writes=[B_wout])

                    vecs = msb("vecs", [128, 5, D])
                    B_vecs = P.buf("vecs", mx_bufs)
                    P.dma("sync", lambda q: q.dma_start(out=vecs[:], in_=vec_bc.rearrange("v p d -> p v d")), writes=[B_vecs])
                    def layer_norm(src, bsrc, ldst, bdst, gi, vecs, B_vecs):
                        for hh in range(2):
                            P.op("dve", (lambda hh: (lambda v: v.bn_stats(out=stats[:, hh, :], in_=src[:, hh * 512:(hh + 1) * 512])))(hh),
                                 reads=[bsrc], writes=[B_st])
                        P.op("dve", lambda v: v.bn_aggr(out=mv[:], in_=stats[:].rearrange("p a b -> p (a b)")), writes=[B_st])
                        P.op("dve", lambda v: v.tensor_scalar(out=rstd1[:], in0=mv[:, 1:2], scalar1=EPS, scalar2=None, op0=ALU.add),
                             writes=[B_st])
                        P.op("act", lambda a: a.activation(out=rstd1[:], in_=rstd1[:], func=AF.Sqrt), reads=[B_st], writes=[B_st])
                        P.op("dve", lambda v: v.reciprocal(out=rstd1[:], in_=rstd1[:]), reads=[B_st], writes=[B_st])
                        P.op("dve", lambda v: v.tensor_scalar(out=src[:], in0=src[:], scalar1=mv[:, 0:1], scalar2=rstd1[:, 0:1],
                                                              op0=ALU.subtract, op1=ALU.mult), reads=[B_st], writes=[bsrc])
                        P.op("dve", lambda v: v.tensor_tensor(out=src[:], in0=src[:], in1=vecs[:, gi, :], op=ALU.mult),
                             reads=[B_vecs], writes=[bsrc])
                        P.op("dve", lambda v: v.tensor_tensor(out=ldst[:], in0=src[:], in1=vecs[:, gi + 1, :], op=ALU.add),
                             reads=[B_vecs, bsrc], writes=[bdst])

                    P.dma("sync", lambda q: q.dma_start(out=wr_sb[:], in_=w_router.rearrange("(c p) e -> p c e", p=128)), writes=[B_wr])

                    for p in range(NTO):
                        xi, bxi = xin[p % 2], B_xin[p % 2]
                        tt, btt = tmp_t[p % 2], B_tmp[p % 2]
                        x1, bx1 = x1t[p % 2], B_x1t[p % 2]
                        P.dma("sync", (lambda p, xi: (lambda q: q.dma_start(out=xi[:], in_=x_ext[HALO + p * 128:HALO + (p + 1) * 128, :])))(p, xi),
                              reads=src_reads, writes=[bxi])
                        P.op("dve", (lambda xi: (lambda v: v.scalar_tensor_tensor(out=xi[:], in0=xi[:], scalar=ALPHA, in1=vecs[:, 0, :],
                                                                                   op0=ALU.mult, op1=ALU.add)))(xi),
                             reads=[B_vecs], writes=[bxi])
                        for hh in range(2):
                            bk = (p % 2) * 2 + hh
                            for fch in range(8):
                                P.op("pe", (lambda p, hh, fch, bk: (lambda pe: pe.matmul(banks[bk][:], lhsT=ycatT[:, fch, p * 128:(p + 1) * 128],
                                                                                          rhs=w_out_sb[:, fch, hh * 512:(hh + 1) * 512],
                                                                                          start=(fch == 0), stop=(fch == 7))))(p, hh, fch, bk),
                                     reads=[B_wout] + B_ycat, writes=[bankb[bk]])
                            P.op("dve", (lambda hh, bk, xi, tt: (lambda v: v.tensor_tensor(out=tt[:, hh * 512:(hh + 1) * 512],
                                                                                            in0=banks[bk][:], in1=xi[:, hh * 512:(hh + 1) * 512],
                                                                                            op=ALU.add)))(hh, bk, xi, tt),
                                 reads=[bankb[bk], bxi], writes=[btt])
                        layer_norm(tt, btt, x1, bx1, 1, vecs, B_vecs)
                        if stage == "mix":
                            final_ops.append(P.dma("sync", (lambda p, x1: (lambda q: q.dma_start(out=dst[p * 128:(p + 1) * 128, :], in_=x1[:])))(p, x1),
                                                   reads=[bx1], writes=[B_dst], sembuf=bx1, indep=True))
                            continue
                        P.dma("sync", (lambda p, x1: (lambda q: q.dma_start(out=x1d[p * 128:(p + 1) * 128, :], in_=x1[:])))(p, x1),
                              reads=[bx1], writes=[B_x1d], sembuf=bx1, indep=True)
                        xb, bxb = x1b[p % 2], B_x1b[p % 2]
                        P.op("act", (lambda x1, xb: (lambda a: a.copy(out=xb[:], in_=x1[:])))(x1, xb), reads=[bx1], writes=[bxb])
                        for hh in range(2):
                            bk = 4 + hh
                            for c4 in range(4):
                                c = hh * 4 + c4
                                P.op("pe", (lambda x1, c, c4, bk: (lambda pe: pe.transpose(out=banks[bk][:, c4 * 128:(c4 + 1) * 128],
                                                                                            in_=x1[:, c * 128:(c + 1) * 128],
                                                                                            identity=ident_f[:])))(x1, c, c4, bk),
                                     reads=[bx1, B_const], writes=[bankb[bk]])
                            P.op("act", (lambda hh, bk: (lambda a: a.copy(out=x1T[:, hh * 4:(hh + 1) * 4, :],
                                                                          in_=banks[bk][:].rearrange("p (c f) -> p c f", c=4))))(hh, bk),
                                 reads=[bankb[bk]], writes=[B_x1T])
                        for c in range(8):
                            P.op("pe", (lambda c: (lambda pe: pe.matmul(banks[6][:, 0:NE], lhsT=x1T[:, c, :], rhs=wr_sb[:, c, :],
                                                                         start=(c == 0), stop=(c == 7))))(c),
                                 reads=[B_x1T, B_wr], writes=[bankb[6]])
                        P.op("dve", lambda v: v.tensor_tensor(out=logit[:], in0=banks[6][:, 0:NE], in1=brt_sb[:], op=ALU.add),
                             reads=[bankb[6], B_par], writes=[B_rt])
                        P.op("dve", lambda v: v.max(out=top8[:], in_=logit[:]), writes=[B_rt])
                        P.op("dve", lambda v: v.tensor_scalar(out=negv0[:], in0=top8[:, 0:1], scalar1=-1.0, scalar2=None, op0=ALU.mult),
                             writes=[B_rt])
                        P.op("act", lambda a: a.activation(out=ex4[:], in_=top8[:, 0:4], func=AF.Exp, bias=negv0[:, 0:1], scale=1.0),
                             reads=[B_rt], writes=[B_rt])
                        P.op("dve", lambda v: v.tensor_reduce(out=ssum[:], in_=ex4[:], axis=mybir.AxisListType.X, op=ALU.add),
                             reads=[B_rt], writes=[B_rt])
                        P.op("dve", lambda v: v.reciprocal(out=ssum[:], in_=ssum[:]), writes=[B_rt])
                        P.op("dve", (lambda p: (lambda v: v.tensor_scalar(out=gates[:, p, :], in0=ex4[:], scalar1=ssum[:, 0:1], scalar2=None,
                                                                          op0=ALU.mult)))(p), reads=[B_rt], writes=[B_gates])
                        P.op("dve", (lambda p: (lambda v: v.tensor_scalar(out=mask_all[:, p, :], in0=logit[:], scalar1=top8[:, 3:4],
                                                                          scalar2=tokv_sb[:, 0, p:p + 1], op0=ALU.is_ge,
                                                                          op1=ALU.mult)))(p), reads=[B_rt, B_par], writes=[B_mask])
                        for pp in range(p + 1):
                            P.op("pe", (lambda pp, p: (lambda pe: pe.matmul(banks[7][:, 0:NE],
                                                                             lhsT=(ustrict[:] if pp == p else ones_b[:]),
                                                                             rhs=mask_all[:, pp, :], start=(pp == 0), stop=(pp == p))))(pp, p),
                                 reads=[B_mask, B_const], writes=[bankb[7]])
                        P.op("dve", lambda v: v.tensor_scalar(out=ovf[:], in0=banks[7][:, 0:NE], scalar1=float(C), scalar2=1.0e6,
                                                              op0=ALU.is_ge, op1=ALU.mult), reads=[bankb[7]], writes=[B_rt])
                        P.op("dve", lambda v: v.tensor_tensor(out=Atab[:], in0=banks[7][:, 0:NE], in1=ec_sb[:], op=ALU.add),
                             reads=[bankb[7], B_par], writes=[B_rt])
                        P.op("dve", (lambda p: (lambda v: v.scalar_tensor_tensor(out=Atab[:], in0=Atab[:], scalar=tokv_sb[:, 1, p:p + 1],
                                                                                 in1=ovf[:], op0=ALU.add, op1=ALU.add)))(p),
                             reads=[B_par], writes=[B_rt])
                        for k in range(4):
                            P.op("dve", (lambda k: (lambda v: v.scalar_tensor_tensor(out=junk[:], in0=logit[:], scalar=top8[:, k:k + 1],
                                                                                      in1=Atab[:], op0=ALU.is_equal, op1=ALU.mult,
                                                                                      accum_out=slot_f[:, k:k + 1])))(k), writes=[B_rt])
                        P.op("dve", (lambda p: (lambda v: v.tensor_copy(out=slots[:, p, :], in_=slot_f[:])))(p), reads=[B_rt], writes=[B_slots])
                        for k in range(4):
                            P.dma("pool", (lambda p, k, xb: (lambda g: g.indirect_dma_start(
                                out=xs[:, :], out_offset=bass.IndirectOffsetOnAxis(ap=slots[:, p, k:k + 1], axis=0),
                                in_=xb[:, :], in_offset=None, bounds_check=P.env["bc_reg%d" % LI], oob_is_err=False)))(p, k, xb),
                                  reads=[bxb, B_slots, B_xs0], writes=[B_xs], sembuf=bxb, indep=True)

                    P.close(mx_bufs)
                with contextlib.ExitStack() as me:
                    me_bufs = []

                    def esb(name, shape, dt=F32):
                        return sb(name, shape, dt, st=me)

                    wg = [esb("wg%d" % i, [128, 8, D], BF16) for i in range(2)]
                    wu = [esb("wu%d" % i, [128, 8, D], BF16) for i in range(2)]
                    wd = [esb("wd%d" % i, [128, 8, D], BF16) for i in range(2)]
                    B_wg = [P.buf("wg0", me_bufs), P.buf("wg1", me_bufs)]
                    B_wu = [P.buf("wu0", me_bufs), P.buf("wu1", me_bufs)]
                    B_wd = [P.buf("wd0", me_bufs), P.buf("wd1", me_bufs)]
                    xe = [esb("xe%d" % i, [128, NS, D], BF16) for i in range(3)]
                    B_xe = [P.buf("xe%d" % i, me_bufs) for i in range(3)]
                    xeT = esb("xeT", [128, 8, C], BF16)
                    B_xeT = P.buf("xeT", me_bufs)
                    bd = [esb("bd%d" % i, [128, D]) for i in range(2)]
                    B_bd = [P.buf("bd0", me_bufs), P.buf("bd1", me_bufs)]
                    gc = [esb("gc%d" % i, [128, C]) for i in range(2)]
                    sgm = [esb("sgm%d" % i, [128, C]) for i in range(2)]
                    ub = [esb("ub%d" % i, [128, C]) for i in range(2)]
                    B_gc = [P.buf("gc0", me_bufs), P.buf("gc1", me_bufs)]
                    B_sgm = [P.buf("sgm0", me_bufs), P.buf("sgm1", me_bufs)]
                    B_ub = [P.buf("ub0", me_bufs), P.buf("ub1", me_bufs)]
                    actT = esb("actT", [128, 8, C], BF16)
                    B_act = [P.buf("act%d" % f, me_bufs) for f in range(8)]
                    yo = [esb("yo%d" % i, [128, D]) for i in range(2)]
                    B_yo = [P.buf("yo0", me_bufs), P.buf("yo1", me_bufs)]
                    B_ys = P.buf("ys")

                    def load_w(e):
                        i = e % 2
                        for (wdst, bdst, wsrc) in ((wg[i], B_wg[i], w_gate), (wu[i], B_wu[i], w_up), (wd[i], B_wd[i], w_down)):
                            P.dma("pool", (lambda wdst, wsrc, e: (lambda g: g.dma_start(out=wdst[:], in_=wsrc[e].rearrange("(c p) f -> p c f", p=128))))(wdst, wsrc, e),
                                  writes=[bdst])

                    def load_xe(e):
                        ix = e % 3
                        P.dma("sync", (lambda e, ix: (lambda q: q.dma_start(out=xe[ix][:], in_=xs[e * C:(e + 1) * C, :].rearrange("(s p) d -> p s d", p=128))))(e, ix),
                              reads=[B_xs], writes=[B_xe[ix]])

                    def load_bd(e):
                        i = e % 2
                        P.dma("sync", (lambda e, i: (lambda q: q.dma_start(out=bd[i][:], in_=b_down[e:e + 1, :].to_broadcast([128, D]))))(e, i),
                              writes=[B_bd[i]])

                    load_xe(0)
                    load_xe(1)
                    load_bd(0)
                    load_w(0)
                    for e in range(NE):
                        i = e % 2
                        if e + 1 < NE:
                            load_w(e + 1)
                            load_bd(e + 1)
                        if e + 2 < NE:
                            load_xe(e + 2)
                        ix = e % 3
                        for s in range(NS):
                            for hh in range(2):
                                bk = hh
                                tbv = banks[bk][:].bitcast(BF16)
                                for c4 in range(4):
                                    c = hh * 4 + c4
                                    P.op("pe", (lambda ix, s, c, c4, tbv: (lambda pe: pe.transpose(out=tbv[:, c4 * 128:(c4 + 1) * 128],
                                                                                                    in_=xe[ix][:, s, c * 128:(c + 1) * 128],
                                                                                                    identity=ident_b[:])))(ix, s, c, c4, tbv),
                                         reads=[B_xe[ix], B_const], writes=[bankb[bk]])
                                if hh == 0:
                                    P.op("act", (lambda s, hh, tbv: (lambda a: a.copy(out=xeT[:, hh * 4:(hh + 1) * 4, s * 128:(s + 1) * 128],
                                                                                      in_=tbv[:, 0:512].rearrange("p (c f) -> p c f", c=4))))(s, hh, tbv),
                                         reads=[bankb[bk]], writes=[B_xeT])
                                else:
                                    P.op("dve", (lambda s, hh, tbv: (lambda v: v.tensor_copy(out=xeT[:, hh * 4:(hh + 1) * 4, s * 128:(s + 1) * 128],
                                                                                             in_=tbv[:, 0:512].rearrange("p (c f) -> p c f", c=4))))(s, hh, tbv),
                                         reads=[bankb[bk]], writes=[B_xeT])
                        for f in range(8):
                            bg, bu = 2 + (f % 2) * 2, 3 + (f % 2) * 2
                            k2 = f % 2
                            for dch in range(8):
                                P.op("pe", (lambda i, f, dch, bg: (lambda pe: pe.matmul(banks[bg][:, 0:C], lhsT=wg[i][:, dch, f * 128:(f + 1) * 128],
                                                                                         rhs=xeT[:, dch, :], start=(dch == 0), stop=(dch == 7))))(i, f, dch, bg),
                                     reads=[B_wg[i], B_xeT], writes=[bankb[bg]])
                            for dch in range(8):
                                P.op("pe", (lambda i, f, dch, bu: (lambda pe: pe.matmul(banks[bu][:, 0:C], lhsT=wu[i][:, dch, f * 128:(f + 1) * 128],
                                                                                         rhs=xeT[:, dch, :], start=(dch == 0), stop=(dch == 7))))(i, f, dch, bu),
                                     reads=[B_wu[i], B_xeT], writes=[bankb[bu]])
                            P.op("dve", (lambda e, f, bg, k2: (lambda v: v.tensor_scalar(out=gc[k2][:], in0=banks[bg][:, 0:C], scalar1=bg_sb[:, e, f:f + 1],
                                                                                          scalar2=7.0, op0=ALU.add, op1=ALU.min)))(e, f, bg, k2),
                                 reads=[bankb[bg], B_par], writes=[B_gc[k2]])
                            P.op("act", (lambda k2: (lambda a: a.activation(out=sgm[k2][:], in_=gc[k2][:], func=AF.Sigmoid, scale=1.702)))(k2),
                                 reads=[B_gc[k2]], writes=[B_sgm[k2]])
                            P.op("act", (lambda e, f, bu, k2: (lambda a: a.activation(out=ub[k2][:], in_=banks[bu][:, 0:C], func=AF.Identity,
                                                                                       bias=bu_sb[:, e, f:f + 1], scale=1.0)))(e, f, bu, k2),
                                 reads=[bankb[bu], B_par], writes=[B_ub[k2]])
                            P.op("dve", (lambda k2: (lambda v: v.tensor_scalar(out=ub[k2][:], in0=ub[k2][:], scalar1=7.0, scalar2=-7.0,
                                                                               op0=ALU.min, op1=ALU.max)))(k2), reads=[B_ub[k2]], writes=[B_ub[k2]])
                            P.op("dve", (lambda k2: (lambda v: v.tensor_tensor(out=gc[k2][:], in0=gc[k2][:], in1=sgm[k2][:], op=ALU.mult)))(k2),
                                 reads=[B_sgm[k2]], writes=[B_gc[k2]])
                            P.op("dve", (lambda f, k2: (lambda v: v.scalar_tensor_tensor(out=actT[:, f, :], in0=ub[k2][:], scalar=1.0, in1=gc[k2][:],
                                                                                          op0=ALU.add, op1=ALU.mult)))(f, k2),
                                 reads=[B_ub[k2], B_gc[k2]], writes=[B_act[f]])
                        for s in range(NS):
                            yk = (e * NS + s) % 2
                            for hh in range(2):
                                bk = 6 + hh
                                for f in range(8):
                                    P.op("pe", (lambda i, s, hh, f, bk: (lambda pe: pe.matmul(banks[bk][:], lhsT=actT[:, f, s * 128:(s + 1) * 128],
                                                                                               rhs=wd[i][:, f, hh * 512:(hh + 1) * 512],
                                                                                               start=(f == 0), stop=(f == 7))))(i, s, hh, f, bk),
                                         reads=[B_wd[i]] + B_act, writes=[bankb[bk]])
                                P.op("dve", (lambda i, yk, hh, bk: (lambda v: v.tensor_tensor(out=yo[yk][:, hh * 512:(hh + 1) * 512], in0=banks[bk][:],
                                                                                               in1=bd[i][:, hh * 512:(hh + 1) * 512], op=ALU.add)))(i, yk, hh, bk),
                                     reads=[bankb[bk], B_bd[i]], writes=[B_yo[yk]], indep=(hh == 1))
                            P.dma("sync", (lambda e, s, yk: (lambda q: q.dma_start(out=ys[e * C + s * 128:e * C + (s + 1) * 128, :], in_=yo[yk][:])))(e, s, yk),
                                  reads=[B_yo[yk]], writes=[B_ys], sembuf=B_yo[yk], indep=True)
                    P.close(me_bufs)

                with contextlib.ExitStack() as me:
                    cb_bufs = []

                    def esb(name, shape, dt=F32):
                        return sb(name, shape, dt, st=me)

                    vecs2 = esb("vecs2", [128, 5, D])
                    B_vecs2 = P.buf("vecs2", cb_bufs)
                    P.dma("sync", lambda q: q.dma_start(out=vecs2[:], in_=vec_bc.rearrange("v p d -> p v d")), writes=[B_vecs2])
                    gth = [[esb("gth%d_%d" % (i, k), [128, D]) for k in range(4)] for i in range(2)]
                    B_gth = [[P.buf("gth%d_%d" % (i, k), cb_bufs) for k in range(4)] for i in range(2)]
                    for i in range(2):
                        for k in range(4):
                            P.op("pool", (lambda i, k: (lambda g: g.memset(gth[i][k][:], 0.0)))(i, k), writes=[B_gth[i][k]])
                    for p in range(NTO):
                        i = p % 2
                        x1, bx1 = x1t[i], B_x1t[i]
                        tt, btt = tmp_t[i], B_tmp[i]
                        P.dma("sync", (lambda p, x1: (lambda q: q.dma_start(out=x1[:], in_=x1d[p * 128:(p + 1) * 128, :])))(p, x1),
                              reads=[B_x1d], writes=[bx1])
                        for k in range(4):
                            P.dma("pool", (lambda p, k, i: (lambda g: g.indirect_dma_start(
                                out=gth[i][k][:, :], out_offset=None, in_=ys[:, :],
                                in_offset=bass.IndirectOffsetOnAxis(ap=slots[:, p, k:k + 1], axis=0),
                                bounds_check=P.env["bc_reg%d" % LI], oob_is_err=False)))(p, k, i),
                                  reads=[B_ys, B_slots], writes=[B_gth[i][k]])
                        P.op("dve", (lambda x1, tt: (lambda v: v.tensor_scalar(out=tt[:], in0=x1[:], scalar1=ALPHA, scalar2=None, op0=ALU.mult)))(x1, tt),
                             reads=[bx1], writes=[btt])
                        for k in range(4):
                            P.op("dve", (lambda p, k, i, tt: (lambda v: v.scalar_tensor_tensor(out=tt[:], in0=gth[i][k][:], scalar=gates[:, p, k:k + 1],
                                                                                                in1=tt[:], op0=ALU.mult, op1=ALU.add)))(p, k, i, tt),
                                 reads=[B_gth[i][k], B_gates], writes=[btt])
                        layer_norm(tt, btt, x1, bx1, 3, vecs2, B_vecs2)
                        final_ops.append(P.dma("sync", (lambda p, x1: (lambda q: q.dma_start(out=dst[p * 128:(p + 1) * 128, :], in_=x1[:])))(p, x1),
                                               reads=[bx1], writes=[B_dst], sembuf=bx1, indep=True))
                    P.close(cb_bufs)
                P.close(top_bufs)

        x_ext0 = nc.dram_tensor("x_ext", [S_OWN + 4 * HALO, D], F32, kind="ExternalInput").ap()
        y_out = nc.dram_tensor("y_out", [S_OWN, D], F32, kind="ExternalOutput").ap()
        xmid = nc.dram_tensor("xmid", [S_OWN + 2 * HALO, D], F32, kind="Internal").ap()
        B_xmid = Buf("xmid")
        B_yout = Buf("y_out")
        emit_layer(0, S_OWN + 2 * HALO, 512, True, x_ext0, [], xmid, B_xmid)
        emit_layer(1, S_OWN, 384, False, xmid, [B_xmid], y_out, B_yout)
        P.emit(final_ops)
    return nc


def _static_tables(n_out, seg, ext, nseg_rows=128):
    NTO = n_out // 128
    t0 = seg * S_OWN - (HALO if ext else 0)
    r_base = t0 // GRID_W
    rows = nseg_rows
    S = rows * GRID_W
    rowbias = np.zeros((128, NTO, 7, 2), np.float32)
    for p in range(NTO):
        for j in range(7):
            for kr2 in range(2):
                kr = r_base + 2 * p - 6 + 2 * j + kr2
                for rr in range(2):
                    r = r_base + 2 * p + rr
                    sr = min(max(r - 4, 0), rows - 8)
                    ok = (0 <= kr < rows) and (sr <= kr < sr + 8)
                    if not ok:
                        rowbias[kr2 * 64:(kr2 + 1) * 64, p, j, rr] = NEG
    n_in = n_out + 2 * HALO
    x0 = t0 - HALO
    if ext:
        regs = ((0, HALO), (HALO, 2 * HALO), (n_in - 2 * HALO, n_in - HALO), (n_in - HALO, n_in))
    else:
        regs = ((0, HALO), (0, 0), (0, 0), (n_in - HALO, n_in))
    flags = np.ones((128, 4), np.float32)
    for i, (a, b) in enumerate(regs):
        if b > a and (x0 + a < 0 or x0 + b > S):
            flags[:, i] = 0.0
    poolcorr = np.ones((128, 2, 16), np.float32)
    wins = (2, 4, 8, 16)
    cp0, cp1 = (HALO, n_out - HALO - 8) if ext else (0, n_out - 8)
    for g, w in enumerate(wins):
        pc, half = g // 2, g % 2
        for i in range(8):
            for (pos, t) in ((i, t0 + cp0 + i), (8 + i, t0 + cp1 + i)):
                lo = min(max(t - w // 2, 0), S)
                hi = min(max(t + w // 2, 0), S)
                if hi > lo:
                    poolcorr[half * 64:(half + 1) * 64, pc, pos] = np.float32(w) / np.float32(hi - lo)
    tok = t0 + np.arange(NTO)[None, :] * 128 + np.arange(128)[:, None]
    ok = (tok >= 0) & (tok < S)
    tokval = np.stack([ok.astype(np.float32), np.where(ok, 0.0, 1.0e6).astype(np.float32)], 1)
    return rowbias.reshape(128, NTO * 14), flags, poolcorr, np.ascontiguousarray(tokval)


def _bias_index():
    j = np.arange(7)[:, None, None]
    key = np.arange(128)[None, :, None]
    q = np.arange(128)[None, None, :]
    kr2, kc = key // 64, key % 64
    rr, c = q // 64, q % 64
    dr = (2 * j + kr2 - 6) - rr
    dc = kc - c
    sc = np.clip(c - 8, 0, GRID_W - 16)
    valid = (kc >= sc) & (kc < sc + 16) & (np.abs(dr) <= 7) & (np.abs(dc) <= 15)
    ri = np.clip(dr + 7, 0, 14)
    ci = np.clip(dc + 15, 0, 30)
    ri, ci, valid = np.broadcast_arrays(ri, ci, valid)
    return ri, ci, valid


_PROG_CACHE = {}
LAYER_CFG = ((S_OWN + 2 * HALO, 512, True), (S_OWN, 384, False))


def _get_prog():
    if "full" not in _PROG_CACHE:
        _PROG_CACHE["full"] = build_program()
    return _PROG_CACHE["full"]


def _layer_common(l, P, C):
    f32 = np.float32
    ri, ci, valid = _bias_index()
    rpb = P["rpb"][l]
    biasT = np.where(valid[None], rpb[:, ri, ci], f32(NEG)).astype(f32)
    w_pool = P["w_pool"][l]
    wpool_bd = np.zeros((2, 128, 128), f32)
    for g in range(4):
        pc, half = g // 2, g % 2
        wpool_bd[pc, half * 64:(half + 1) * 64, half * 64:(half + 1) * 64] = w_pool[g]
    return {
        "w_in": np.ascontiguousarray(P["w_in"][l]),
        "b_in_pc": np.ascontiguousarray(P["b_in"][l].reshape(18, 128).T),
        "bv_bc": np.ascontiguousarray(np.broadcast_to(P["b_in"][l][1280:1792], (128, 512))),
        "wpool_bd": wpool_bd,
        "pool_scale_pc": np.ascontiguousarray(P["pool_scale"][l].reshape(2, 128).T),
        "biasT": biasT,
        "conv_dw_pc": np.ascontiguousarray(P["conv_dw"][l][:, 0, :].reshape(31, 2, 128).transpose(2, 1, 0)),
        "conv_vec_pc": np.ascontiguousarray(np.stack([P["conv_dw_b"][l], P["conv_ln_g"][l], P["conv_ln_b"][l],
                                                      P["b_conv_pw"][l]], 0).reshape(4, 2, 128).transpose(2, 0, 1)),
        "w_pw": np.ascontiguousarray(P["w_conv_pw"][l]),
        "w_out": np.ascontiguousarray(P["w_out"][l]),
        "vec_bc": np.ascontiguousarray(np.broadcast_to(
            np.stack([P["b_out"][l], P["ln1_g"][l], P["ln1_b"][l], P["ln2_g"][l], P["ln2_b"][l]], 0)[:, None, :], (5, 128, D))),
        "w_router": np.ascontiguousarray(P["w_router"][l]),
        "b_router_bc": np.ascontiguousarray(np.broadcast_to(P["b_router"][l], (128, NE))),
        "ec_bc": np.ascontiguousarray(np.broadcast_to((np.arange(NE) * C).astype(f32), (128, NE))),
        "b_gate_pc": np.ascontiguousarray(P["b_gate"][l].reshape(NE, 8, 128).transpose(2, 0, 1)),
        "b_up_pc": np.ascontiguousarray(P["b_up"][l].reshape(NE, 8, 128).transpose(2, 0, 1)),
        "w_gate": np.ascontiguousarray(P["w_gate"][l]),
        "w_up": np.ascontiguousarray(P["w_up"][l]),
        "w_down": np.ascontiguousarray(P["w_down"][l]),
        "b_down": np.ascontiguousarray(P["b_down"][l]),
    }


def _in_maps(P):
    f32 = np.float32
    x_full = P["x"]
    B, S, _ = x_full.shape
    nseg = S // S_OWN
    shared = {}
    for l, (n_out, C, ext) in enumerate(LAYER_CFG):
        for k, v in _layer_common(l, P, C).items():
            shared["%s_%d" % (k, l)] = v
    in_maps = []
    for core in range(8):
        b, seg = core // nseg, core % nseg
        t0 = seg * S_OWN
        m = dict(shared)
        xe = np.zeros((S_OWN + 4 * HALO, D), f32)
        lo, hi = max(t0 - 2 * HALO, 0), min(t0 + S_OWN + 2 * HALO, S)
        xe[lo - (t0 - 2 * HALO):hi - (t0 - 2 * HALO)] = x_full[b, lo:hi]
        m["x_ext"] = xe
        for l, (n_out, C, ext) in enumerate(LAYER_CFG):
            rowbias, flags, poolcorr, tokval = _static_tables(n_out, seg, ext)
            m["tokval_%d" % l] = tokval
            m["rowbias_%d" % l] = rowbias
            m["flags_%d" % l] = flags
            m["poolcorr_%d" % l] = poolcorr
        in_maps.append(m)
    return in_maps


def kernel(**inputs):
    P = {k: np.asarray(v, dtype=np.float32) for k, v in inputs.items()}
    x = P["x"]
    B, S, _ = x.shape
    nc = _get_prog()
    res = run_bass_kernel_spmd(nc, _in_maps(P), core_ids=list(range(8)))
    out = np.empty_like(x)
    nseg = S // S_OWN
    for core in range(8):
        b, seg = core // nseg, core % nseg
        out[b, seg * S_OWN:(seg + 1) * S_OWN] = res.results[core]["y_out"]
    return out
```

```python
import numpy as np
import concourse.bass as bass
import concourse.mybir as mybir
from concourse.bass_utils import run_bass_kernel_spmd

F32 = mybir.dt.float32
BF16 = mybir.dt.bfloat16
I32 = mybir.dt.int32
AF = mybir.ActivationFunctionType
ALU = mybir.AluOpType

D = 1024
NE = 32
S_OWN = 2048
HALO = 256
GRID_W = 64
ALPHA = (2.0 * 2) ** 0.25
EPS = 1e-5
NEG = -1e30
DIN = 2304


class Buf:
    __slots__ = ("name", "ws", "reads", "sem", "cum")

    def __init__(self, name):
        self.name = name
        self.ws = []
        self.reads = []
        self.sem = None
        self.cum = 0


class Op:
    __slots__ = ("eng", "fn", "deps", "is_dma", "sem", "val", "signal", "seq")

    def __init__(self, eng, fn, is_dma):
        self.seq = 0
        self.eng = eng
        self.fn = fn
        self.deps = []
        self.is_dma = is_dma
        self.sem = None
        self.val = 0
        self.signal = False


class Prog:
    ENG = ("sync", "act", "dve", "pe", "pool")

    def __init__(self, nc):
        self.nc = nc
        self.ops = {e: [] for e in self.ENG}
        self.dma_sems = []
        self.fence_k = {}
        self.seq = 0
        self.marks = {}
        self.limit = None
        self.env = {"bc_vals": {}}

    def mark(self, name):
        self.marks[name] = self.seq

    def buf(self, name, scope=None):
        b = Buf(name)
        b.reads = list(self.fence_k.values())
        if scope is not None:
            scope.append(b)
        return b

    def close(self, scope_bufs):
        for b in scope_bufs:
            for o in list(b.ws) + list(b.reads):
                if o.is_dma:
                    k = ("dma", id(o.sem))
                    if k not in self.fence_k or self.fence_k[k].val < o.val:
                        self.fence_k[k] = o
                else:
                    k = ("eng", o.eng)
                    if k not in self.fence_k or self.fence_k[k].seq < o.seq:
                        self.fence_k[k] = o

    def _deps(self, op, reads, writes, indep=False):
        deps = []
        for b in reads:
            deps.extend(b.ws)
        for b in writes:
            if not indep:
                deps.extend(b.ws)
            deps.extend(b.reads)
        seen = set()
        for d in deps:
            if d.is_dma or op.is_dma or d.eng != op.eng or op.eng != "pe":
                if id(d) not in seen:
                    seen.add(id(d))
                    op.deps.append(d)
                d.signal = True
        for b in writes:
            if indep:
                b.ws.append(op)
            else:
                b.ws = [op]
                b.reads = []
        for b in reads:
            if op not in b.ws:
                b.reads.append(op)

    def op(self, eng, fn, reads=(), writes=(), indep=False):
        o = Op(eng, fn, False)
        self.seq += 1
        o.seq = self.seq
        self._deps(o, reads, writes, indep)
        self.ops[eng].append(o)
        return o

    def dma(self, eng, fn, reads=(), writes=(), sembuf=None, indep=False):
        o = Op(eng, fn, True)
        self.seq += 1
        o.seq = self.seq
        sb = sembuf if sembuf is not None else writes[0]
        if sb.sem is None:
            sb.sem = ("dma", len(self.dma_sems))
            self.dma_sems.append(sb)
        sb.cum += 16
        o.sem = sb
        o.val = sb.cum
        o.signal = True
        self._deps(o, reads, writes, indep)
        self.ops[eng].append(o)
        return o

    def emit(self, final_waits):
        nc = self.nc
        if self.limit is not None:
            lim = self.marks.get(self.limit, self.limit)
            lim = int(lim)
            for e in self.ENG:
                self.ops[e] = [o for o in self.ops[e] if o.seq <= lim]
            final_waits = [o for e in self.ENG for o in self.ops[e] if o.is_dma]
            for e in self.ENG:
                if self.ops[e] and not self.ops[e][-1].is_dma:
                    self.ops[e][-1].signal = True
                    final_waits.append(self.ops[e][-1])
        for e in self.ENG:
            c = 0
            for o in self.ops[e]:
                if not o.is_dma and o.signal:
                    c += 1
                    o.val = c
        import contextlib
        with contextlib.ExitStack() as st:
            esem = {e: st.enter_context(nc.semaphore("es_" + e)) for e in self.ENG}
            dsem = [st.enter_context(nc.semaphore("ds_%d" % i)) for i in range(len(self.dma_sems))]
            block = st.enter_context(nc.Block())

            def semof(o):
                if o.is_dma:
                    return dsem[o.sem.sem[1]]
                return esem[o.eng]

            def run(e, eng):
                waited = {}
                if e == "pool":
                    for L, v in self.env["bc_vals"].items():
                        self.env["bc_reg%d" % L] = eng.alloc_register("bc_reg%d" % L)
                        eng.reg_mov(self.env["bc_reg%d" % L], int(v))
                for o in self.ops[e]:
                    need = {}
                    for d in o.deps:
                        s = semof(d)
                        k = id(s)
                        if k not in need or d.val > need[k][1]:
                            need[k] = (s, d.val)
                    for k, (s, v) in need.items():
                        if waited.get(k, 0) >= v:
                            continue
                        waited[k] = v
                        eng.wait_ge(s, v)
                    ins = o.fn(eng)
                    if o.is_dma:
                        ins.then_inc(semof(o), 16)
                    elif o.signal:
                        ins.then_inc(esem[e], 1)
                if e == "sync":
                    for o in final_waits:
                        eng.wait_ge(semof(o), o.val)

            block.sync(lambda eng: run("sync", eng))
            block.scalar(lambda eng: run("act", eng))
            block.vector(lambda eng: run("dve", eng))
            block.tensor(lambda eng: run("pe", eng))
            block.gpsimd(lambda eng: run("pool", eng))


def build_program(stage="full", limit=None):
    nc = bass.Bass("TRN2", target_bir_lowering=False)
    P = Prog(nc)
    P.limit = limit
    P.env["bc_vals"] = {}
    import contextlib
    with contextlib.ExitStack() as glob:
        _names = {}

        def sb_global(name, shape, dt=F32, st=glob):
            k = _names.get(name, 0)
            _names[name] = k + 1
            if k:
                name = "%s_%d" % (name, k)
            return st.enter_context(nc.sbuf_tensor(name, list(shape), dt))

        sb = sb_global
        banks = [glob.enter_context(nc.psum_tensor("bank%d" % i, [128, 512], F32)) for i in range(8)]
        bankb = [Buf("bank%d" % i) for i in range(8)]

        ident_f = sb("ident_f", [128, 128])
        ident_b = sb("ident_b", [128, 128], BF16)
        ones_b = sb("ones_b", [128, 128], BF16)
        ones256 = sb("ones256", [128, 128], BF16)
        ustrict = sb("ustrict", [128, 128], BF16)
        B_const = Buf("const")
        iota_i = sb("iota_i", [128, 128], I32)
        iota_f = sb("iota_f", [128, 128])
        pidx_i = sb("pidx_i", [128, 1], I32)
        pidx_f = sb("pidx_f", [128, 1])
        P.op("pool", lambda g: g.iota(iota_i[:], [[1, 128]], base=0, channel_multiplier=0), writes=[B_const])
        P.op("pool", lambda g: g.iota(pidx_i[:], [[0, 1]], base=0, channel_multiplier=1), writes=[B_const])
        P.op("dve", lambda v: v.tensor_copy(out=iota_f[:], in_=iota_i[:]), reads=[B_const], writes=[B_const])
        P.op("dve", lambda v: v.tensor_copy(out=pidx_f[:], in_=pidx_i[:]), reads=[B_const], writes=[B_const])
        P.op("dve", lambda v: v.tensor_scalar(out=ident_f[:], in0=iota_f[:], scalar1=pidx_f[:, 0:1], scalar2=None,
                                              op0=ALU.is_equal), reads=[B_const], writes=[B_const])
        P.op("dve", lambda v: v.tensor_copy(out=ident_b[:], in_=ident_f[:]), writes=[B_const])
        P.op("dve", lambda v: v.tensor_scalar(out=ustrict[:], in0=iota_f[:], scalar1=pidx_f[:, 0:1], scalar2=None,
                                              op0=ALU.is_gt), writes=[B_const])
        P.op("dve", lambda v: v.memset(ones_b[:], 1.0), writes=[B_const])
        P.op("dve", lambda v: v.memset(ones256[:], 1.0 / 256.0), writes=[B_const])

        final_ops = []

        def emit_layer(LI, n_out, C, ext, x_ext, src_reads, dst, B_dst):
            n_in = n_out + 2 * HALO
            NTI = n_in // 128
            NTO = n_out // 128
            HT = HALO // 128
            NS = C // 128
            NSLOT = NE * C
            P.env["bc_vals"][LI] = NSLOT - 1
            if ext:
                FLAG_REG = ((0, HALO, 0), (HALO, 2 * HALO, 1), (n_in - 2 * HALO, n_in - HALO, 2), (n_in - HALO, n_in, 3))
                CP0, CP1 = HALO, n_out - HALO - 8
            else:
                FLAG_REG = ((0, HALO, 0), (n_in - HALO, n_in, 3))
                CP0, CP1 = 0, n_out - 8

            def din(name, shape, dt=F32):
                return nc.dram_tensor("%s_%d" % (name, LI), list(shape), dt, kind="ExternalInput").ap()

            w_in = din("w_in", [D, DIN])
            b_in_pc = din("b_in_pc", [128, 18])
            bv_bc = din("bv_bc", [128, 512])
            wpool_bd = din("wpool_bd", [2, 128, 128])
            pool_scale_pc = din("pool_scale_pc", [128, 2])
            poolcorr = din("poolcorr", [128, 2, 16])
            flags = din("flags", [128, 4])
            biasT = din("biasT", [8, 7, 128, 128])
            rowbias = din("rowbias", [128, NTO * 14])
            tokval = din("tokval", [128, 2, NTO])
            conv_dw_pc = din("conv_dw_pc", [128, 2, 31])
            conv_vec_pc = din("conv_vec_pc", [128, 4, 2])
            w_pw = din("w_pw", [256, 256])
            w_out = din("w_out", [D, D])
            vec_bc = din("vec_bc", [5, 128, D])
            w_router = din("w_router", [D, NE])
            b_router_bc = din("b_router_bc", [128, NE])
            ec_bc = din("ec_bc", [128, NE])
            b_gate_pc = din("b_gate_pc", [128, NE, 8])
            b_up_pc = din("b_up_pc", [128, NE, 8])
            if True:
                w_gate = din("w_gate", [NE, D, D])
                w_up = din("w_up", [NE, D, D])
                w_down = din("w_down", [NE, D, D])
                b_down = din("b_down", [NE, D])
            x1d = nc.dram_tensor("x1d_%d" % LI, [n_out, D], F32, kind="Internal").ap()
            xs = nc.dram_tensor("xs_%d" % LI, [NSLOT, D], BF16, kind="Internal").ap()
            ys = nc.dram_tensor("ys_%d" % LI, [NSLOT, D], F32, kind="Internal").ap()
            with contextlib.ExitStack() as top:
                top_bufs = []

                def Buf_(name):
                    return P.buf(name, top_bufs)

                def sb(name, shape, dt=F32, st=top):
                    return sb_global(name, shape, dt, st)

                B_par = Buf_("params")
                b_in_sb = sb("b_in_sb", [128, 18])
                flags_sb = sb("flags_sb", [128, 4])
                pscale_sb = sb("pscale_sb", [128, 2])
                pcorr_sb = sb("pcorr_sb", [128, 2, 16])
                rowb_sb = sb("rowb_sb", [128, NTO * 14])
                tokv_sb = sb("tokv_sb", [128, 2, NTO])
                cdw_sb = sb("cdw_sb", [128, 2, 31])
                cvec_sb = sb("cvec_sb", [128, 4, 2])
                brt_sb = sb("brt_sb", [128, NE])
                ec_sb = sb("ec_sb", [128, NE])
                bg_sb = sb("bg_sb", [128, NE, 8])
                bu_sb = sb("bu_sb", [128, NE, 8])
                bu1_sb = sb("bu1_sb", [128, NE, 8])
                B_bu1 = Buf_("bu1")
                for pdst, psrc in ((b_in_sb, b_in_pc), (flags_sb, flags), (pscale_sb, pool_scale_pc), (pcorr_sb, poolcorr),
                                 (rowb_sb, rowbias), (tokv_sb, tokval), (cdw_sb, conv_dw_pc), (cvec_sb, conv_vec_pc), (brt_sb, b_router_bc),
                                 (ec_sb, ec_bc), (bg_sb, b_gate_pc), (bu_sb, b_up_pc)):
                    P.dma("sync", (lambda d, s: (lambda q: q.dma_start(out=d[:], in_=s)))(pdst, psrc), writes=[B_par], indep=True)

                P.op("dve", lambda v: v.tensor_scalar(out=bu1_sb[:], in0=bu_sb[:], scalar1=1.0, scalar2=None, op0=ALU.add),
                     reads=[B_par], writes=[B_bu1])

                x1t = [sb("x1t%d" % i, [128, D]) for i in range(2)]
                B_x1t = [Buf_("x1t0"), Buf_("x1t1")]
                stats = sb("stats", [128, 2, 6])
                mv = sb("mv", [128, 2])
                rstd1 = sb("rstd1", [128, 1])
                B_st = Buf_("stats")

                x1b = [sb("x1b%d" % i, [128, D], BF16) for i in range(2)]
                B_x1b = [Buf_("x1b0"), Buf_("x1b1")]
                x1T = sb("x1T", [128, 8, 128])
                B_x1T = Buf_("x1T")
                wr_sb = sb("wr_sb", [128, 8, NE])
                B_wr = Buf_("wr")
                logit = sb("logit", [128, NE])
                top8 = sb("top8", [128, 8])
                negv0 = sb("negv0", [128, 1])
                ex4 = sb("ex4", [128, 4])
                ssum = sb("ssum", [128, 1])
                gates = sb("gates", [128, NTO, 4])
                mask_all = sb("mask_all", [128, NTO, NE], BF16)
                Atab = sb("Atab", [128, NE])
                ovf = sb("ovf", [128, NE])
                junk = sb("junk", [128, NE])
                slot_f = sb("slot_f", [128, 4])
                slots = sb("slots", [128, NTO, 4], I32)
                B_rt, B_gates, B_mask, B_slots = Buf_("rt"), Buf_("gates"), Buf_("mask"), Buf_("slots")
                B_x1d = Buf_("x1d")
                B_xs = Buf_("xs")
                tmp_t = [sb("tmp_t%d" % i, [128, D]) for i in range(2)]
                B_tmp = [Buf_("tmp0"), Buf_("tmp1")]

                with contextlib.ExitStack() as mx:
                    mx_bufs = []

                    def msb(name, shape, dt=F32):
                        return sb(name, shape, dt, st=mx)

                    ycatT = msb("ycatT", [128, 8, n_out], BF16)
                    B_ycat = [P.buf("ycat%d" % c, mx_bufs) for c in range(8)]
                    xin = [msb("xin%d" % i, [128, D]) for i in range(2)]
                    B_xin = [P.buf("xin%d" % i, mx_bufs) for i in range(2)]
                    B_xs0 = Buf_("xs_zero")
                    if stage == "full":
                        P.op("pool", lambda g: g.memset(ycatT[:], 0.0), writes=B_ycat)
                        zsrc = ycatT[:].rearrange("p c n -> p (c n)")
                        per = (8 * n_out) // D
                        for r0 in range(0, NSLOT // 128, per):
                            nr = min(per, NSLOT // 128 - r0)
                            P.dma("sync", (lambda r0, nr: (lambda q: q.dma_start(
                                out=xs[r0 * 128:(r0 + nr) * 128, :].rearrange("(s p) d -> p s d", p=128),
                                in_=zsrc[:, 0:nr * D].rearrange("p (s d) -> p s d", d=D))))(r0, nr),
                                  reads=B_ycat, writes=[B_xs0], indep=True)
                    w_in_v = w_in.rearrange("(c p) f -> p c f", p=128)
                    tblocks = [(s, min(512, n_in - s)) for s in range(0, n_in, 512)]

                    with contextlib.ExitStack() as sa:
                        sa_bufs = []
                        xT = sb("xT", [128, 8, n_in], BF16, st=sa)
                        B_xT = [P.buf("xT%d" % t, sa_bufs) for t in range(NTI)]

                        for t in range(NTI):
                            xi, bxi = xin[t % 2], B_xin[t % 2]
                            P.dma("sync", (lambda t, xi: (lambda q: q.dma_start(out=xi[:], in_=x_ext[t * 128:(t + 1) * 128, :])))(t, xi),
                                  reads=src_reads, writes=[bxi])
                            for hh in range(2):
                                bk = (t % 2) * 2 + hh
                                for c4 in range(4):
                                    c = hh * 4 + c4
                                    P.op("pe", (lambda xi, c, c4, bk: (lambda pe: pe.transpose(out=banks[bk][:, c4 * 128:(c4 + 1) * 128],
                                                                                                in_=xi[:, c * 128:(c + 1) * 128],
                                                                                                identity=ident_f[:])))(xi, c, c4, bk),
                                         reads=[bxi, B_const], writes=[bankb[bk]])
                                if hh == 0:
                                    P.op("act", (lambda t, hh, bk: (lambda a: a.copy(
                                        out=xT[:, hh * 4:(hh + 1) * 4, t * 128:(t + 1) * 128],
                                        in_=banks[bk][:].rearrange("p (c f) -> p c f", c=4))))(t, hh, bk),
                                         reads=[bankb[bk]], writes=[B_xT[t]])
                                else:
                                    P.op("dve", (lambda t, hh, bk: (lambda v: v.tensor_copy(
                                        out=xT[:, hh * 4:(hh + 1) * 4, t * 128:(t + 1) * 128],
                                        in_=banks[bk][:].rearrange("p (c f) -> p c f", c=4))))(t, hh, bk),
                                         reads=[bankb[bk]], writes=[B_xT[t]])

                        P.mark('phase0')

                        def xt_bufs(s, n):
                            return [B_xT[t] for t in range(s // 128, (s + n + 127) // 128)]

                        def inproj(wt, wcol, bw, s, n, bk):
                            for dch in range(8):
                                P.op("pe", (lambda dch: (lambda pe: pe.matmul(banks[bk][:, 0:n],
                                                                               lhsT=wt[:, dch, wcol:wcol + 128],
                                                                               rhs=xT[:, dch, s:s + n],
                                                                               start=(dch == 0), stop=(dch == 7))))(dch),
                                     reads=[bw] + xt_bufs(s, n), writes=[bankb[bk]])

                        with contextlib.ExitStack() as sp:
                            sp_bufs = []
                            wsl_p = sb("wsl_p", [128, 8, 256], BF16, st=sp)
                            B_wslp = P.buf("wsl_p", sp_bufs)
                            P.dma("pool", lambda g: g.dma_start(out=wsl_p[:], in_=w_in_v[:, :, 0:256]), writes=[B_wslp])
                            wpool_sb = sb("wpool_sb", [128, 2, 128], BF16, st=sp)
                            B_wp = P.buf("wpool", sp_bufs)
                            P.dma("pool", lambda g: g.dma_start(out=wpool_sb[:], in_=wpool_bd.rearrange("c p f -> p c f")), writes=[B_wp])
                            PADP = 16
                            uT = sb("uT", [128, n_in + PADP], st=sp)
                            aT = sb("aT", [128, n_in + PADP], st=sp)
                            a2T = sb("a2T", [128, n_in + PADP], st=sp)
                            mT = sb("mT", [128, n_out], st=sp)
                            dT = sb("dT", [128, n_out], BF16, st=sp)
                            B_u, B_a, B_a2, B_m, B_d = (P.buf(nm, sp_bufs) for nm in ("uT", "aT", "a2T", "mT", "dT"))
                            for pc in range(2):
                                P.op("dve", lambda v: v.memset(uT[:, n_in:n_in + PADP], 0.0), writes=[B_u])
                                for bi, (s, n) in enumerate(tblocks):
                                    bk = 4 + (bi % 2)
                                    inproj(wsl_p, pc * 128, B_wslp, s, n, bk)
                                    P.op("act", (lambda s, n, bk, pc: (lambda a: a.activation(out=uT[:, s:s + n], in_=banks[bk][:, 0:n],
                                                                                               func=AF.Identity, bias=b_in_sb[:, pc:pc + 1],
                                                                                               scale=1.0)))(s, n, bk, pc),
                                         reads=[bankb[bk], B_par], writes=[B_u])
                                for (fa, fb, fc) in FLAG_REG:
                                    P.op("dve", (lambda fa, fb, fc: (lambda v: v.tensor_scalar(out=uT[:, fa:fb], in0=uT[:, fa:fb],
                                                                                               scalar1=flags_sb[:, fc:fc + 1], scalar2=None,
                                                                                               op0=ALU.mult)))(fa, fb, fc),
                                         reads=[B_par], writes=[B_u])
                                L = n_in
                                P.op("dve", lambda v: v.tensor_tensor(out=aT[:, 0:L], in0=uT[:, 0:L], in1=uT[:, 1:L + 1], op=ALU.add),
                                     reads=[B_u], writes=[B_a])
                                P.op("dve", lambda v: v.memset(aT[:, L:L + PADP], 0.0), writes=[B_a])
                                P.op("dve", lambda v: v.tensor_tensor(out=a2T[:, 0:L], in0=aT[:, 0:L], in1=aT[:, 2:L + 2], op=ALU.add),
                                     reads=[B_a], writes=[B_a2])
                                P.op("dve", lambda v: v.memset(a2T[:, L:L + PADP], 0.0), writes=[B_a2])
                                o0 = HALO
                                if pc == 0:
                                    P.op("dve", lambda v: v.tensor_scalar(out=mT[0:64, :], in0=aT[0:64, o0 - 1:o0 - 1 + n_out], scalar1=0.5,
                                                                          scalar2=None, op0=ALU.mult), reads=[B_a], writes=[B_m])
                                    P.op("dve", lambda v: v.tensor_scalar(out=mT[64:128, :], in0=a2T[64:128, o0 - 2:o0 - 2 + n_out],
                                                                          scalar1=0.25, scalar2=None, op0=ALU.mult), reads=[B_a2], writes=[B_m])
                                else:
                                    P.op("dve", lambda v: v.tensor_tensor(out=aT[:, 0:L], in0=a2T[:, 0:L], in1=a2T[:, 4:L + 4], op=ALU.add),
                                         reads=[B_a2], writes=[B_a])
                                    P.op("dve", lambda v: v.tensor_tensor(out=a2T[:, 0:L], in0=aT[:, 0:L], in1=aT[:, 8:L + 8], op=ALU.add),
                                         reads=[B_a], writes=[B_a2])
                                    P.op("dve", lambda v: v.tensor_scalar(out=mT[0:64, :], in0=aT[0:64, o0 - 4:o0 - 4 + n_out], scalar1=0.125,
                                                                          scalar2=None, op0=ALU.mult), reads=[B_a], writes=[B_m])
                                    P.op("dve", lambda v: v.tensor_scalar(out=mT[64:128, :], in0=a2T[64:128, o0 - 8:o0 - 8 + n_out],
                                                                          scalar1=0.0625, scalar2=None, op0=ALU.mult), reads=[B_a2], writes=[B_m])
                                P.op("dve", (lambda pc: (lambda v: v.tensor_tensor(out=mT[:, CP0:CP0 + 8], in0=mT[:, CP0:CP0 + 8], in1=pcorr_sb[:, pc, 0:8],
                                                                                    op=ALU.mult)))(pc), reads=[B_par], writes=[B_m])
                                P.op("dve", (lambda pc: (lambda v: v.tensor_tensor(out=mT[:, CP1:CP1 + 8], in0=mT[:, CP1:CP1 + 8],
                                                                                    in1=pcorr_sb[:, pc, 8:16], op=ALU.mult)))(pc),
                                     reads=[B_par], writes=[B_m])
                                P.op("dve", lambda v: v.tensor_tensor(out=dT[:], in0=mT[:], in1=uT[:, o0:o0 + n_out], op=ALU.subtract),
                                     reads=[B_m, B_u], writes=[B_d])
                                for bi in range(n_out // 512):
                                    bk = 6 + (bi % 2)
                                    P.op("pe", (lambda bi, bk, pc: (lambda pe: pe.matmul(banks[bk][:], lhsT=wpool_sb[:, pc, :],
                                                                                          rhs=dT[:, bi * 512:(bi + 1) * 512],
                                                                                          start=True, stop=True)))(bi, bk, pc),
                                         reads=[B_wp, B_d], writes=[bankb[bk]])
                                    P.op("act", (lambda bi, bk, pc: (lambda a: a.activation(out=ycatT[:, pc, bi * 512:(bi + 1) * 512],
                                                                                             in_=banks[bk][:], func=AF.Copy,
                                                                                             scale=pscale_sb[:, pc:pc + 1])))(bi, bk, pc),
                                         reads=[bankb[bk], B_par], writes=[B_ycat[pc]])
                        P.close(sp_bufs)
                        P.mark('pool')

                        with contextlib.ExitStack() as sc:
                            sc_bufs = []
                            wsl_c = sb("wsl_c", [128, 8, 512], BF16, st=sc)
                            B_wslc = P.buf("wsl_c", sc_bufs)
                            P.dma("pool", lambda g: g.dma_start(out=wsl_c[:], in_=w_in_v[:, :, 1792:2304]), writes=[B_wslc])
                            wpw_sb = sb("wpw_sb", [128, 2, 256], BF16, st=sc)
                            B_wpw = P.buf("wpw", sc_bufs)
                            P.dma("pool", lambda g: g.dma_start(out=wpw_sb[:], in_=w_pw.rearrange("(c p) f -> p c f", p=128)), writes=[B_wpw])
                            hT = sb("hT", [128, 2, n_in], BF16, st=sc)
                            B_h = [P.buf("hT0", sc_bufs), P.buf("hT1", sc_bufs)]
                            sg = sb("sg", [128, 512], st=sc)
                            B_sg = P.buf("sg", sc_bufs)
                            for j in range(2):
                                for bi, (s, n) in enumerate(tblocks):
                                    inproj(wsl_c, 256 + j * 128, B_wslc, s, n, 4)
                                    P.op("act", (lambda s, n, j: (lambda a: a.activation(out=sg[:, 0:n], in_=banks[4][:, 0:n], func=AF.Sigmoid,
                                                                                         bias=b_in_sb[:, 16 + j:17 + j], scale=1.0)))(s, n, j),
                                         reads=[bankb[4], B_par], writes=[B_sg])
                                    inproj(wsl_c, j * 128, B_wslc, s, n, 5)
                                    P.op("dve", (lambda s, n, j: (lambda v: v.scalar_tensor_tensor(out=hT[:, j, s:s + n], in0=banks[5][:, 0:n],
                                                                                                    scalar=b_in_sb[:, 14 + j:15 + j],
                                                                                                    in1=sg[:, 0:n], op0=ALU.add,
                                                                                                    op1=ALU.mult)))(s, n, j),
                                         reads=[bankb[5], B_sg, B_par], writes=[B_h[j]])
                                for (fa, fb, fc) in FLAG_REG:
                                    P.op("dve", (lambda j, fa, fb, fc: (lambda v: v.tensor_scalar(out=hT[:, j, fa:fb], in0=hT[:, j, fa:fb],
                                                                                                  scalar1=flags_sb[:, fc:fc + 1], scalar2=None,
                                                                                                  op0=ALU.mult)))(j, fa, fb, fc),
                                         reads=[B_par], writes=[B_h[j]])
                            dg = sb("dg", [128, 2, 31, 128], BF16, st=sc)
                            B_dg = P.buf("dg", sc_bufs)
                            for j in range(2):
                                for k in range(31):
                                    P.op("dve", (lambda j, k: (lambda v: v.tensor_scalar(out=dg[:, j, k, :], in0=ident_f[:],
                                                                                         scalar1=cdw_sb[:, j, k:k + 1], scalar2=None,
                                                                                         op0=ALU.mult)))(j, k),
                                         reads=[B_par, B_const], writes=[B_dg])
                            cT = sb("cT", [128, 2, 512], st=sc)
                            cTb = sb("cTb", [128, 2, 512], BF16, st=sc)
                            sqb = sb("sqb", [128, 2, 512], BF16, st=sc)
                            mean_sb = sb("mean_sb", [128, 512], st=sc)
                            m2_sb = sb("m2_sb", [128, 512], st=sc)
                            rstd_sb = sb("rstd_sb", [128, 512], st=sc)
                            nrm = sb("nrm", [128, 2, 512], st=sc)
                            silu_b = sb("silu_b", [128, 2, 512], BF16, st=sc)
                            B_cT, B_cTb, B_sq, B_mean, B_m2, B_rstd, B_nrm, B_silu = (P.buf(nm, sc_bufs) for nm in
                                                                                      ("cT", "cTb", "sqb", "mean", "m2", "rstd", "nrm", "silu"))
                            for bi in range(n_out // 512):
                                t0 = HALO + bi * 512
                                for j in range(2):
                                    bk = 6 + j
                                    for k in range(31):
                                        P.op("pe", (lambda j, k, bk, t0: (lambda pe: pe.matmul(banks[bk][:], lhsT=dg[:, j, k, :],
                                                                                                rhs=hT[:, j, t0 + k - 15:t0 + k - 15 + 512],
                                                                                                start=(k == 0), stop=(k == 30))))(j, k, bk, t0),
                                             reads=[B_dg, B_h[j]], writes=[bankb[bk]])
                                    P.op("act", (lambda j, bk: (lambda a: a.activation(out=cT[:, j, :], in_=banks[bk][:], func=AF.Identity,
                                                                                       bias=cvec_sb[:, 0, j:j + 1], scale=1.0)))(j, bk),
                                         reads=[bankb[bk], B_par], writes=[B_cT])
                                P.op("dve", lambda v: v.tensor_copy(out=cTb[:], in_=cT[:]), reads=[B_cT], writes=[B_cTb])
                                P.op("act", lambda a: a.activation(out=sqb[:], in_=cT[:], func=AF.Square), reads=[B_cT], writes=[B_sq])
                                for j in range(2):
                                    P.op("pe", (lambda j: (lambda pe: pe.matmul(banks[4][:], lhsT=ones256[:], rhs=cTb[:, j, :],
                                                                                 start=(j == 0), stop=(j == 1))))(j),
                                         reads=[B_const, B_cTb], writes=[bankb[4]])
                                for j in range(2):
                                    P.op("pe", (lambda j: (lambda pe: pe.matmul(banks[5][:], lhsT=ones256[:], rhs=sqb[:, j, :],
                                                                                 start=(j == 0), stop=(j == 1))))(j),
                                         reads=[B_const, B_sq], writes=[bankb[5]])
                                P.op("act", lambda a: a.copy(out=mean_sb[:], in_=banks[4][:]), reads=[bankb[4]], writes=[B_mean])
                                P.op("dve", lambda v: v.tensor_tensor(out=m2_sb[:], in0=mean_sb[:], in1=mean_sb[:], op=ALU.mult),
                                     reads=[B_mean], writes=[B_m2])
                                P.op("dve", lambda v: v.scalar_tensor_tensor(out=m2_sb[:], in0=banks[5][:], scalar=EPS, in1=m2_sb[:],
                                                                             op0=ALU.add, op1=ALU.subtract), reads=[bankb[5]], writes=[B_m2])
                                P.op("act", lambda a: a.activation(out=m2_sb[:], in_=m2_sb[:], func=AF.Sqrt), reads=[B_m2], writes=[B_m2])
                                P.op("dve", lambda v: v.reciprocal(out=rstd_sb[:], in_=m2_sb[:]), reads=[B_m2], writes=[B_rstd])
                                for j in range(2):
                                    P.op("dve", (lambda j: (lambda v: v.tensor_tensor(out=nrm[:, j, :], in0=cT[:, j, :], in1=mean_sb[:],
                                                                                      op=ALU.subtract)))(j), reads=[B_cT, B_mean], writes=[B_nrm])
                                    P.op("dve", (lambda j: (lambda v: v.tensor_tensor(out=nrm[:, j, :], in0=nrm[:, j, :], in1=rstd_sb[:],
                                                                                      op=ALU.mult)))(j), reads=[B_rstd], writes=[B_nrm])
                                    P.op("act", (lambda j: (lambda a: a.activation(out=silu_b[:, j, :], in_=nrm[:, j, :], func=AF.Silu,
                                                                                   bias=cvec_sb[:, 2, j:j + 1], scale=cvec_sb[:, 1, j:j + 1])))(j),
                                         reads=[B_nrm, B_par], writes=[B_silu])
                                for cc in range(2):
                                    bk = 6 + cc
                                    for j in range(2):
                                        P.op("pe", (lambda j, cc, bk: (lambda pe: pe.matmul(banks[bk][:], lhsT=wpw_sb[:, j, cc * 128:(cc + 1) * 128],
                                                                                             rhs=silu_b[:, j, :], start=(j == 0),
                                                                                             stop=(j == 1))))(j, cc, bk),
                                             reads=[B_wpw, B_silu], writes=[bankb[bk]])
                                    P.op("act", (lambda cc, bk, bi: (lambda a: a.activation(out=ycatT[:, 6 + cc, bi * 512:(bi + 1) * 512],
                                                                                             in_=banks[bk][:], func=AF.Identity,
                                                                                             bias=cvec_sb[:, 3, cc:cc + 1], scale=1.0)))(cc, bk, bi),
                                         reads=[bankb[bk], B_par], writes=[B_ycat[6 + cc]])
                        P.close(sc_bufs)
                        P.mark('conv')

                        sidx_box = [0]

                        def attn_group(hg):
                            with contextlib.ExitStack() as sat:
                                at_bufs = []
                                wsl = sb("wsl_a", [128, 8, 3, 256], BF16, st=sat)
                                B_wsl = P.buf("wsl_a", at_bufs)
                                for qi, c0 in enumerate((256, 768, 1280)):
                                    P.dma("pool", (lambda qi, c0: (lambda g: g.dma_start(out=wsl[:, :, qi, :],
                                                                                         in_=w_in_v[:, :, c0 + hg * 256:c0 + (hg + 1) * 256])))(qi, c0),
                                          writes=[B_wsl], indep=True)
                                bv_sb = sb("bv_sb", [128, 256], st=sat)
                                B_bv = P.buf("bv", at_bufs)
                                P.dma("sync", lambda q: q.dma_start(out=bv_sb[:], in_=bv_bc[:, hg * 256:(hg + 1) * 256]), writes=[B_bv])
                                qT = sb("qT", [128, 4, n_out], BF16, st=sat)
                                kT = sb("kT", [128, 2, n_in], BF16, st=sat)
                                B_q, B_k = P.buf("qT", at_bufs), P.buf("kT", at_bufs)
                                P.op("pool", lambda g: g.memset(qT[:], 0.0), writes=[B_q])
                                wq = wsl[:, :, 0, :]
                                wk = wsl[:, :, 1, :]
                                for c in range(2):
                                    gch = 2 + hg * 2 + c
                                    for bi in range(n_out // 512):
                                        bk = 4 + (bi % 2)
                                        inproj(wq, c * 128, B_wsl, HALO + bi * 512, 512, bk)
                                        for hh2 in range(2):
                                            P.op("act", (lambda c, bi, bk, gch, hh2: (lambda a: a.activation(
                                                out=qT[hh2 * 64:(hh2 + 1) * 64, 2 * c + hh2, bi * 512:(bi + 1) * 512],
                                                in_=banks[bk][hh2 * 64:(hh2 + 1) * 64, :],
                                                func=AF.Identity, bias=b_in_sb[hh2 * 64:(hh2 + 1) * 64, gch:gch + 1],
                                                scale=1.0)))(c, bi, bk, gch, hh2),
                                                 reads=[bankb[bk], B_par], writes=[B_q])
                                    for bi, (s, n) in enumerate(tblocks):
                                        bk = 6 + (bi % 2)
                                        inproj(wk, c * 128, B_wsl, s, n, bk)
                                        P.op("dve", (lambda c, s, n, bk, gch: (lambda v: v.tensor_scalar(out=kT[:, c, s:s + n], in0=banks[bk][:, 0:n],
                                                                                                          scalar1=b_in_sb[:, gch + 4:gch + 5], scalar2=None,
                                                                                                          op0=ALU.add)))(c, s, n, bk, gch),
                                             reads=[bankb[bk], B_par], writes=[B_k])
                                P.mark('a_qk%d' % hg)
                                vaug = sb("vaug", [128, NTI, 4, 65], BF16, st=sat)
                                B_v = P.buf("vaug", at_bufs)
                                P.op("dve", lambda v: v.memset(vaug[:, :, :, 64:65], 1.0), writes=[B_v])
                                for t in range(NTI):
                                    bk = 4 + (t % 2)
                                    for dch in range(8):
                                        P.op("pe", (lambda t, dch, bk: (lambda pe: pe.matmul(banks[bk][:, 0:256], lhsT=xT[:, dch, t * 128:(t + 1) * 128],
                                                                                              rhs=wsl[:, dch, 2, :],
                                                                                              start=(dch == 0), stop=(dch == 7))))(t, dch, bk),
                                             reads=[B_wsl, B_xT[t]], writes=[bankb[bk]])
                                    P.op("dve", (lambda t, bk: (lambda v: v.tensor_tensor(out=vaug[:, t, :, 0:64],
                                                                                          in0=banks[bk][:, 0:256].rearrange("p (h d) -> p h d", h=4),
                                                                                          in1=bv_sb[:].rearrange("p (h d) -> p h d", h=4),
                                                                                          op=ALU.add)))(t, bk),
                                         reads=[bankb[bk], B_bv], writes=[B_v])
                                P.mark('a_v%d' % hg)
                                Etab = sb("Etab", [128, 4, 7, 128], BF16, st=sat)
                                B_E = P.buf("Etab", at_bufs)
                                bstage = [sb("bstage0", [128, 7, 128], st=sat)] * 2
                                B_bst = [P.buf("bst0", at_bufs)] * 2
                                for h4 in range(4):
                                    P.dma("sync", (lambda h4: (lambda q: q.dma_start(out=bstage[h4 % 2][:],
                                                                                     in_=biasT[hg * 4 + h4].rearrange("j k q -> k j q"))))(h4),
                                          writes=[B_bst[h4 % 2]])
                                    P.op("act", (lambda h4: (lambda a: a.activation(out=Etab[:, h4, :, :], in_=bstage[h4 % 2][:], func=AF.Exp)))(h4),
                                         reads=[B_bst[h4 % 2]], writes=[B_E])
                                P.mark('a_E%d' % hg)
                                pt = sb("pT", [128, 7, 512], BF16, st=sat)
                                bpt = P.buf("pT", at_bufs)
                                yat = [sb("yat%d" % i, [128, 256], BF16, st=sat) for i in range(2)]
                                B_yat = [P.buf("yat0", at_bufs), P.buf("yat1", at_bufs)]
                                rec = sb("rec", [128, 4], st=sat)
                                B_rec = P.buf("rec", at_bufs)
                                for p in range(NTO):
                                    qt = p + HT
                                    jt = [(j, qt - 3 + j) for j in range(7) if 0 <= qt - 3 + j < NTI]
                                    for (j, kt) in jt:
                                        bk = sidx_box[0] % 4
                                        sidx_box[0] += 1
                                        for h4 in range(4):
                                            ch, hp = h4 // 2, (h4 % 2) * 64
                                            P.op("pe", (lambda bk, h4, ch, hp, kt, p: (lambda pe: pe.matmul(
                                                banks[bk][:, h4 * 128:(h4 + 1) * 128],
                                                lhsT=kT[:, ch, kt * 128:(kt + 1) * 128],
                                                rhs=qT[:, h4, p * 128:(p + 1) * 128], start=True, stop=True)))(bk, h4, ch, hp, kt, p),
                                                 reads=[B_q, B_k], writes=[bankb[bk]])
                                        for rr in range(2):
                                            col = (p * 7 + j) * 2 + rr
                                            P.op("act", (lambda bk, j, rr, col: (lambda a: a.activation(
                                                out=pt[:, j, :].rearrange("p (h r c) -> p h r c", h=4, r=2)[:, :, rr, :],
                                                in_=banks[bk][:].rearrange("p (h r c) -> p h r c", h=4, r=2)[:, :, rr, :],
                                                func=AF.Exp, bias=rowb_sb[:, col:col + 1], scale=0.125)))(bk, j, rr, col),
                                                 reads=[bankb[bk], B_par], writes=[bpt])
                                        P.op("dve", (lambda j: (lambda v: v.tensor_tensor(
                                            out=pt[:, j, :].rearrange("p (h q) -> p h q", h=4),
                                            in0=pt[:, j, :].rearrange("p (h q) -> p h q", h=4),
                                            in1=Etab[:, :, j, :], op=ALU.mult)))(j),
                                             reads=[B_E], writes=[bpt])
                                    P.mark('a_S%d_%d' % (hg, p))
                                    ob = 4 + (p % 2)
                                    for h4 in range(4):
                                        for ji, (j, kt) in enumerate(jt):
                                            P.op("pe", (lambda ob, h4, j, kt, ji, nj: (lambda pe: pe.matmul(
                                                banks[ob][:, h4 * 65:(h4 + 1) * 65], lhsT=pt[:, j, h4 * 128:(h4 + 1) * 128],
                                                rhs=vaug[:, kt, h4, :], start=(ji == 0), stop=(ji == nj - 1))))(ob, h4, j, kt, ji, len(jt)),
                                                 reads=[bpt, B_v], writes=[bankb[ob]])
                                    P.mark('a_AV%d_%d' % (hg, p))
                                    ya, bya = yat[p % 2], B_yat[p % 2]
                                    P.op("dve", (lambda ob: (lambda v: v.reciprocal(
                                        out=rec[:], in_=banks[ob][:, 0:260].rearrange("p (h d) -> p h d", h=4)[:, :, 64])))(ob),
                                         reads=[bankb[ob]], writes=[B_rec])
                                    for h4 in range(4):
                                        P.op("dve", (lambda ob, h4, ya: (lambda v: v.tensor_scalar(
                                            out=ya[:, h4 * 64:(h4 + 1) * 64], in0=banks[ob][:, h4 * 65:h4 * 65 + 64],
                                            scalar1=rec[:, h4:h4 + 1], scalar2=None, op0=ALU.mult)))(ob, h4, ya),
                                             reads=[bankb[ob], B_rec], writes=[bya])
                                    P.mark('a_N%d_%d' % (hg, p))
                                    tb = 6 + (p % 2)
                                    tbv = banks[tb][:].bitcast(BF16)
                                    for c2 in range(2):
                                        P.op("pe", (lambda c2, ya, tbv: (lambda pe: pe.transpose(out=tbv[:, c2 * 128:(c2 + 1) * 128],
                                                                                                  in_=ya[:, c2 * 128:(c2 + 1) * 128],
                                                                                                  identity=ident_b[:])))(c2, ya, tbv),
                                             reads=[bya, B_const], writes=[bankb[tb]])
                                    P.op("act", (lambda p, tbv: (lambda a: a.copy(out=ycatT[:, 2 + hg * 2:4 + hg * 2, p * 128:(p + 1) * 128],
                                                                                  in_=tbv[:, 0:256].rearrange("p (c f) -> p c f", c=2))))(p, tbv),
                                         reads=[bankb[tb]], writes=[B_ycat[2 + hg]])
                            P.close(at_bufs)
                        for _hg in range(2):
                            attn_group(_hg)
                            P.mark('attn%d' % _hg)
                    P.close(sa_bufs)

                    w_out_sb = msb("w_out_sb", [128, 8, D], BF16)
                    B_wout = P.buf("w_out", mx_bufs)
                    P.dma("pool", lambda g: g.dma_start(out=w_out_sb[:], in_=w_out.rearrange("(c p) f -> p c f", p=128)),
                          writes=[B_wout])

                    vecs = msb("vecs", [128, 5, D])
                    B_vecs = P.buf("vecs", mx_bufs)
                    P.dma("sync", lambda q: q.dma_start(out=vecs[:], in_=vec_bc.rearrange("v p d -> p v d")), writes=[B_vecs])
                    def layer_norm(src, bsrc, ldst, bdst, gi, vecs, B_vecs):
                        for hh in range(2):
                            P.op("dve", (lambda hh: (lambda v: v.bn_stats(out=stats[:, hh, :], in_=src[:, hh * 512:(hh + 1) * 512])))(hh),
                                 reads=[bsrc], writes=[B_st])
                        P.op("dve", lambda v: v.bn_aggr(out=mv[:], in_=stats[:].rearrange("p a b -> p (a b)")), writes=[B_st])
                        P.op("dve", lambda v: v.tensor_scalar(out=rstd1[:], in0=mv[:, 1:2], scalar1=EPS, scalar2=None, op0=ALU.add),
                             writes=[B_st])
                        P.op("act", lambda a: a.activation(out=rstd1[:], in_=rstd1[:], func=AF.Sqrt), reads=[B_st], writes=[B_st])
                        P.op("dve", lambda v: v.reciprocal(out=rstd1[:], in_=rstd1[:]), reads=[B_st], writes=[B_st])
                        P.op("dve", lambda v: v.tensor_scalar(out=src[:], in0=src[:], scalar1=mv[:, 0:1], scalar2=rstd1[:, 0:1],
                                                              op0=ALU.subtract, op1=ALU.mult), reads=[B_st], writes=[bsrc])
                        P.op("dve", lambda v: v.tensor_tensor(out=src[:], in0=src[:], in1=vecs[:, gi, :], op=ALU.mult),
                             reads=[B_vecs], writes=[bsrc])
                        P.op("dve", lambda v: v.tensor_tensor(out=ldst[:], in0=src[:], in1=vecs[:, gi + 1, :], op=ALU.add),
                             reads=[B_vecs, bsrc], writes=[bdst])

                    P.dma("sync", lambda q: q.dma_start(out=wr_sb[:], in_=w_router.rearrange("(c p) e -> p c e", p=128)), writes=[B_wr])

                    for p in range(NTO):
                        xi, bxi = xin[p % 2], B_xin[p % 2]
                        tt, btt = tmp_t[p % 2], B_tmp[p % 2]
                        x1, bx1 = x1t[p % 2], B_x1t[p % 2]
                        P.dma("sync", (lambda p, xi: (lambda q: q.dma_start(out=xi[:], in_=x_ext[HALO + p * 128:HALO + (p + 1) * 128, :])))(p, xi),
                              reads=src_reads, writes=[bxi])
                        P.op("dve", (lambda xi: (lambda v: v.scalar_tensor_tensor(out=xi[:], in0=xi[:], scalar=ALPHA, in1=vecs[:, 0, :],
                                                                                   op0=ALU.mult, op1=ALU.add)))(xi),
                             reads=[B_vecs], writes=[bxi])
                        for hh in range(2):
                            bk = (p % 2) * 2 + hh
                            for fch in range(8):
                                P.op("pe", (lambda p, hh, fch, bk: (lambda pe: pe.matmul(banks[bk][:], lhsT=ycatT[:, fch, p * 128:(p + 1) * 128],
                                                                                          rhs=w_out_sb[:, fch, hh * 512:(hh + 1) * 512],
                                                                                          start=(fch == 0), stop=(fch == 7))))(p, hh, fch, bk),
                                     reads=[B_wout] + B_ycat, writes=[bankb[bk]])
                            P.op("dve", (lambda hh, bk, xi, tt: (lambda v: v.tensor_tensor(out=tt[:, hh * 512:(hh + 1) * 512],
                                                                                            in0=banks[bk][:], in1=xi[:, hh * 512:(hh + 1) * 512],
                                                                                            op=ALU.add)))(hh, bk, xi, tt),
                                 reads=[bankb[bk], bxi], writes=[btt])
                        layer_norm(tt, btt, x1, bx1, 1, vecs, B_vecs)
                        if stage == "mix":
                            final_ops.append(P.dma("sync", (lambda p, x1: (lambda q: q.dma_start(out=dst[p * 128:(p + 1) * 128, :], in_=x1[:])))(p, x1),
                                                   reads=[bx1], writes=[B_dst], sembuf=bx1, indep=True))
                            continue
                        P.dma("sync", (lambda p, x1: (lambda q: q.dma_start(out=x1d[p * 128:(p + 1) * 128, :], in_=x1[:])))(p, x1),
                              reads=[bx1], writes=[B_x1d], sembuf=bx1, indep=True)
                        xb, bxb = x1b[p % 2], B_x1b[p % 2]
                        P.op("act", (lambda x1, xb: (lambda a: a.copy(out=xb[:], in_=x1[:])))(x1, xb), reads=[bx1], writes=[bxb])
                        for hh in range(2):
                            bk = 4 + hh
                            for c4 in range(4):
                                c = hh * 4 + c4
                                P.op("pe", (lambda x1, c, c4, bk: (lambda pe: pe.transpose(out=banks[bk][:, c4 * 128:(c4 + 1) * 128],
                                                                                            in_=x1[:, c * 128:(c + 1) * 128],
                                                                                            identity=ident_f[:])))(x1, c, c4, bk),
                                     reads=[bx1, B_const], writes=[bankb[bk]])
                            P.op("act", (lambda hh, bk: (lambda a: a.copy(out=x1T[:, hh * 4:(hh + 1) * 4, :],
                                                                          in_=banks[bk][:].rearrange("p (c f) -> p c f", c=4))))(hh, bk),
                                 reads=[bankb[bk]], writes=[B_x1T])
                        for c in range(8):
                            P.op("pe", (lambda c: (lambda pe: pe.matmul(banks[6][:, 0:NE], lhsT=x1T[:, c, :], rhs=wr_sb[:, c, :],
                                                                         start=(c == 0), stop=(c == 7))))(c),
                                 reads=[B_x1T, B_wr], writes=[bankb[6]])
                        P.op("dve", lambda v: v.tensor_tensor(out=logit[:], in0=banks[6][:, 0:NE], in1=brt_sb[:], op=ALU.add),
                             reads=[bankb[6], B_par], writes=[B_rt])
                        P.op("dve", lambda v: v.max(out=top8[:], in_=logit[:]), writes=[B_rt])
                        P.op("dve", lambda v: v.tensor_scalar(out=negv0[:], in0=top8[:, 0:1], scalar1=-1.0, scalar2=None, op0=ALU.mult),
                             writes=[B_rt])
                        P.op("act", lambda a: a.activation(out=ex4[:], in_=top8[:, 0:4], func=AF.Exp, bias=negv0[:, 0:1], scale=1.0),
                             reads=[B_rt], writes=[B_rt])
                        P.op("dve", lambda v: v.tensor_reduce(out=ssum[:], in_=ex4[:], axis=mybir.AxisListType.X, op=ALU.add),
                             reads=[B_rt], writes=[B_rt])
                        P.op("dve", lambda v: v.reciprocal(out=ssum[:], in_=ssum[:]), writes=[B_rt])
                        P.op("dve", (lambda p: (lambda v: v.tensor_scalar(out=gates[:, p, :], in0=ex4[:], scalar1=ssum[:, 0:1], scalar2=None,
                                                                          op0=ALU.mult)))(p), reads=[B_rt], writes=[B_gates])
                        P.op("dve", (lambda p: (lambda v: v.tensor_scalar(out=mask_all[:, p, :], in0=logit[:], scalar1=top8[:, 3:4],
                                                                          scalar2=tokv_sb[:, 0, p:p + 1], op0=ALU.is_ge,
                                                                          op1=ALU.mult)))(p), reads=[B_rt, B_par], writes=[B_mask])
                        for pp in range(p + 1):
                            P.op("pe", (lambda pp, p: (lambda pe: pe.matmul(banks[7][:, 0:NE],
                                                                             lhsT=(ustrict[:] if pp == p else ones_b[:]),
                                                                             rhs=mask_all[:, pp, :], start=(pp == 0), stop=(pp == p))))(pp, p),
                                 reads=[B_mask, B_const], writes=[bankb[7]])
                        P.op("dve", lambda v: v.tensor_scalar(out=ovf[:], in0=banks[7][:, 0:NE], scalar1=float(C), scalar2=1.0e6,
                                                              op0=ALU.is_ge, op1=ALU.mult), reads=[bankb[7]], writes=[B_rt])
                        P.op("dve", lambda v: v.tensor_tensor(out=Atab[:], in0=banks[7][:, 0:NE], in1=ec_sb[:], op=ALU.add),
                             reads=[bankb[7], B_par], writes=[B_rt])
                        P.op("dve", (lambda p: (lambda v: v.scalar_tensor_tensor(out=Atab[:], in0=Atab[:], scalar=tokv_sb[:, 1, p:p + 1],
                                                                                 in1=ovf[:], op0=ALU.add, op1=ALU.add)))(p),
                             reads=[B_par], writes=[B_rt])
                        for k in range(4):
                            P.op("dve", (lambda k: (lambda v: v.scalar_tensor_tensor(out=junk[:], in0=logit[:], scalar=top8[:, k:k + 1],
                                                                                      in1=Atab[:], op0=ALU.is_equal, op1=ALU.mult,
                                                                                      accum_out=slot_f[:, k:k + 1])))(k), writes=[B_rt])
                        P.op("dve", (lambda p: (lambda v: v.tensor_copy(out=slots[:, p, :], in_=slot_f[:])))(p), reads=[B_rt], writes=[B_slots])
                        for k in range(4):
                            P.dma("pool", (lambda p, k, xb: (lambda g: g.indirect_dma_start(
                                out=xs[:, :], out_offset=bass.IndirectOffsetOnAxis(ap=slots[:, p, k:k + 1], axis=0),
                                in_=xb[:, :], in_offset=None, bounds_check=P.env["bc_reg%d" % LI], oob_is_err=False)))(p, k, xb),
                                  reads=[bxb, B_slots, B_xs0], writes=[B_xs], sembuf=bxb, indep=True)

                    P.close(mx_bufs)
                with contextlib.ExitStack() as me:
                    me_bufs = []

                    def esb(name, shape, dt=F32):
                        return sb(name, shape, dt, st=me)

                    wg = [esb("wg%d" % i, [128, 8, D], BF16) for i in range(2)]
                    wu = [esb("wu%d" % i, [128, 8, D], BF16) for i in range(2)]
                    wd = [esb("wd%d" % i, [128, 8, D], BF16) for i in range(2)]
                    B_wg = [P.buf("wg0", me_bufs), P.buf("wg1", me_bufs)]
                    B_wu = [P.buf("wu0", me_bufs), P.buf("wu1", me_bufs)]
                    B_wd = [P.buf("wd0", me_bufs), P.buf("wd1", me_bufs)]
                    xe = [esb("xe%d" % i, [128, NS, D], BF16) for i in range(3)]
                    B_xe = [P.buf("xe%d" % i, me_bufs) for i in range(3)]
                    xeT = esb("xeT", [128, 8, C], BF16)
                    B_xeT = P.buf("xeT", me_bufs)
                    bd = [esb("bd%d" % i, [128, D]) for i in range(2)]
                    B_bd = [P.buf("bd0", me_bufs), P.buf("bd1", me_bufs)]
                    gc = [esb("gc%d" % i, [128, C]) for i in range(2)]
                    sgm = [esb("sgm%d" % i, [128, C]) for i in range(2)]
                    ub = [esb("ub%d" % i, [128, C]) for i in range(2)]
                    B_gc = [P.buf("gc0", me_bufs), P.buf("gc1", me_bufs)]
                    B_sgm = [P.buf("sgm0", me_bufs), P.buf("sgm1", me_bufs)]
                    B_ub = [P.buf("ub0", me_bufs), P.buf("ub1", me_bufs)]
                    actT = esb("actT", [128, 8, C], BF16)
                    B_act = [P.buf("act%d" % f, me_bufs) for f in range(8)]
                    yo = [esb("yo%d" % i, [128, D]) for i in range(2)]
                    B_yo = [P.buf("yo0", me_bufs), P.buf("yo1", me_bufs)]
                    B_ys = P.buf("ys")

                    def load_w(e):
                        i = e % 2
                        for (wdst, bdst, wsrc) in ((wg[i], B_wg[i], w_gate), (wu[i], B_wu[i], w_up), (wd[i], B_wd[i], w_down)):
                            P.dma("pool", (lambda wdst, wsrc, e: (lambda g: g.dma_start(out=wdst[:], in_=wsrc[e].rearrange("(c p) f -> p c f", p=128))))(wdst, wsrc, e),
                                  writes=[bdst])

                    def load_xe(e):
                        ix = e % 3
                        P.dma("sync", (lambda e, ix: (lambda q: q.dma_start(out=xe[ix][:], in_=xs[e * C:(e + 1) * C, :].rearrange("(s p) d -> p s d", p=128))))(e, ix),
                              reads=[B_xs], writes=[B_xe[ix]])

                    def load_bd(e):
                        i = e % 2
                        P.dma("sync", (lambda e, i: (lambda q: q.dma_start(out=bd[i][:], in_=b_down[e:e + 1, :].to_broadcast([128, D]))))(e, i),
                              writes=[B_bd[i]])

                    load_xe(0)
                    load_xe(1)
                    load_bd(0)
                    load_w(0)
                    for e in range(NE):
                        i = e % 2
                        if e + 1 < NE:
                            load_w(e + 1)
                            load_bd(e + 1)
                        if e + 2 < NE:
                            load_xe(e + 2)
                        ix = e % 3
                        for s in range(NS):
                            for hh in range(2):
                                bk = hh
                                tbv = banks[bk][:].bitcast(BF16)
                                for c4 in range(4):
                                    c = hh * 4 + c4
                                    P.op("pe", (lambda ix, s, c, c4, tbv: (lambda pe: pe.transpose(out=tbv[:, c4 * 128:(c4 + 1) * 128],
                                                                                                    in_=xe[ix][:, s, c * 128:(c + 1) * 128],
                                                                                                    identity=ident_b[:])))(ix, s, c, c4, tbv),
                                         reads=[B_xe[ix], B_const], writes=[bankb[bk]])
                                if hh == 0:
                                    P.op("act", (lambda s, hh, tbv: (lambda a: a.copy(out=xeT[:, hh * 4:(hh + 1) * 4, s * 128:(s + 1) * 128],
                                                                                      in_=tbv[:, 0:512].rearrange("p (c f) -> p c f", c=4))))(s, hh, tbv),
                                         reads=[bankb[bk]], writes=[B_xeT])
                                else:
                                    P.op("dve", (lambda s, hh, tbv: (lambda v: v.tensor_copy(out=xeT[:, hh * 4:(hh + 1) * 4, s * 128:(s + 1) * 128],
                                                                                             in_=tbv[:, 0:512].rearrange("p (c f) -> p c f", c=4))))(s, hh, tbv),
                                         reads=[bankb[bk]], writes=[B_xeT])
                        for f in range(8):
                            bg, bu = 2 + (f % 2) * 2, 3 + (f % 2) * 2
                            k2 = f % 2
                            for dch in range(8):
                                P.op("pe", (lambda i, f, dch, bg: (lambda pe: pe.matmul(banks[bg][:, 0:C], lhsT=wg[i][:, dch, f * 128:(f + 1) * 128],
                                                                                         rhs=xeT[:, dch, :], start=(dch == 0), stop=(dch == 7))))(i, f, dch, bg),
                                     reads=[B_wg[i], B_xeT], writes=[bankb[bg]])
                            for dch in range(8):
                                P.op("pe", (lambda i, f, dch, bu: (lambda pe: pe.matmul(banks[bu][:, 0:C], lhsT=wu[i][:, dch, f * 128:(f + 1) * 128],
                                                                                         rhs=xeT[:, dch, :], start=(dch == 0), stop=(dch == 7))))(i, f, dch, bu),
                                     reads=[B_wu[i], B_xeT], writes=[bankb[bu]])
                            P.op("dve", (lambda e, f, bg, k2: (lambda v: v.tensor_scalar(out=gc[k2][:], in0=banks[bg][:, 0:C], scalar1=bg_sb[:, e, f:f + 1],
                                                                                          scalar2=7.0, op0=ALU.add, op1=ALU.min)))(e, f, bg, k2),
                                 reads=[bankb[bg], B_par], writes=[B_gc[k2]])
                            P.op("act", (lambda e, f, bu, k2: (lambda a: a.activation(out=ub[k2][:], in_=banks[bu][:, 0:C], func=AF.Identity,
                                                                                       bias=bu1_sb[:, e, f:f + 1], scale=1.0)))(e, f, bu, k2),
                                 reads=[bankb[bu], B_bu1], writes=[B_ub[k2]])
                            P.op("act", (lambda k2: (lambda a: a.activation(out=sgm[k2][:], in_=gc[k2][:], func=AF.Sigmoid, scale=1.702)))(k2),
                                 reads=[B_gc[k2]], writes=[B_sgm[k2]])
                            P.op("dve", (lambda k2: (lambda v: v.tensor_scalar(out=ub[k2][:], in0=ub[k2][:], scalar1=8.0, scalar2=-6.0,
                                                                               op0=ALU.min, op1=ALU.max)))(k2), reads=[B_ub[k2]], writes=[B_ub[k2]])
                            P.op("pool", (lambda k2: (lambda g: g.tensor_tensor(out=gc[k2][:], in0=gc[k2][:], in1=sgm[k2][:], op=ALU.mult)))(k2),
                                 reads=[B_sgm[k2]], writes=[B_gc[k2]])
                            P.op("pool", (lambda f, k2: (lambda g: g.tensor_tensor(out=actT[:, f, :], in0=ub[k2][:], in1=gc[k2][:],
                                                                                   op=ALU.mult)))(f, k2),
                                 reads=[B_ub[k2], B_gc[k2]], writes=[B_act[f]])
                        for s in range(NS):
                            yk = (e * NS + s) % 2
                            for hh in range(2):
                                bk = 6 + hh
                                for f in range(8):
                                    P.op("pe", (lambda i, s, hh, f, bk: (lambda pe: pe.matmul(banks[bk][:], lhsT=actT[:, f, s * 128:(s + 1) * 128],
                                                                                               rhs=wd[i][:, f, hh * 512:(hh + 1) * 512],
                                                                                               start=(f == 0), stop=(f == 7))))(i, s, hh, f, bk),
                                         reads=[B_wd[i]] + B_act, writes=[bankb[bk]])
                                P.op("dve", (lambda i, yk, hh, bk: (lambda v: v.tensor_tensor(out=yo[yk][:, hh * 512:(hh + 1) * 512], in0=banks[bk][:],
                                                                                               in1=bd[i][:, hh * 512:(hh + 1) * 512], op=ALU.add)))(i, yk, hh, bk),
                                     reads=[bankb[bk], B_bd[i]], writes=[B_yo[yk]], indep=(hh == 1))
                            P.dma("sync", (lambda e, s, yk: (lambda q: q.dma_start(out=ys[e * C + s * 128:e * C + (s + 1) * 128, :], in_=yo[yk][:])))(e, s, yk),
                                  reads=[B_yo[yk]], writes=[B_ys], sembuf=B_yo[yk], indep=True)
                    P.close(me_bufs)

                with contextlib.ExitStack() as me:
                    cb_bufs = []

                    def esb(name, shape, dt=F32):
                        return sb(name, shape, dt, st=me)

                    vecs2 = esb("vecs2", [128, 5, D])
                    B_vecs2 = P.buf("vecs2", cb_bufs)
                    P.dma("sync", lambda q: q.dma_start(out=vecs2[:], in_=vec_bc.rearrange("v p d -> p v d")), writes=[B_vecs2])
                    gth = [[esb("gth%d_%d" % (i, k), [128, D]) for k in range(4)] for i in range(2)]
                    B_gth = [[P.buf("gth%d_%d" % (i, k), cb_bufs) for k in range(4)] for i in range(2)]
                    for i in range(2):
                        for k in range(4):
                            P.op("pool", (lambda i, k: (lambda g: g.memset(gth[i][k][:], 0.0)))(i, k), writes=[B_gth[i][k]])
                    for p in range(NTO):
                        i = p % 2
                        x1, bx1 = x1t[i], B_x1t[i]
                        tt, btt = tmp_t[i], B_tmp[i]
                        P.dma("sync", (lambda p, x1: (lambda q: q.dma_start(out=x1[:], in_=x1d[p * 128:(p + 1) * 128, :])))(p, x1),
                              reads=[B_x1d], writes=[bx1])
                        for k in range(4):
                            P.dma("pool", (lambda p, k, i: (lambda g: g.indirect_dma_start(
                                out=gth[i][k][:, :], out_offset=None, in_=ys[:, :],
                                in_offset=bass.IndirectOffsetOnAxis(ap=slots[:, p, k:k + 1], axis=0),
                                bounds_check=P.env["bc_reg%d" % LI], oob_is_err=False)))(p, k, i),
                                  reads=[B_ys, B_slots], writes=[B_gth[i][k]])
                        P.op("dve", (lambda x1, tt: (lambda v: v.tensor_scalar(out=tt[:], in0=x1[:], scalar1=ALPHA, scalar2=None, op0=ALU.mult)))(x1, tt),
                             reads=[bx1], writes=[btt])
                        for k in range(4):
                            P.op("dve", (lambda p, k, i, tt: (lambda v: v.scalar_tensor_tensor(out=tt[:], in0=gth[i][k][:], scalar=gates[:, p, k:k + 1],
                                                                                                in1=tt[:], op0=ALU.mult, op1=ALU.add)))(p, k, i, tt),
                                 reads=[B_gth[i][k], B_gates], writes=[btt])
                        layer_norm(tt, btt, x1, bx1, 3, vecs2, B_vecs2)
                        final_ops.append(P.dma("sync", (lambda p, x1: (lambda q: q.dma_start(out=dst[p * 128:(p + 1) * 128, :], in_=x1[:])))(p, x1),
                                               reads=[bx1], writes=[B_dst], sembuf=bx1, indep=True))
                    P.close(cb_bufs)
                P.close(top_bufs)

        x_ext0 = nc.dram_tensor("x_ext", [S_OWN + 4 * HALO, D], F32, kind="ExternalInput").ap()
        y_out = nc.dram_tensor("y_out", [S_OWN, D], F32, kind="ExternalOutput").ap()
        xmid = nc.dram_tensor("xmid", [S_OWN + 2 * HALO, D], F32, kind="Internal").ap()
        B_xmid = Buf("xmid")
        B_yout = Buf("y_out")
        emit_layer(0, S_OWN + 2 * HALO, 512, True, x_ext0, [], xmid, B_xmid)
        emit_layer(1, S_OWN, 384, False, xmid, [B_xmid], y_out, B_yout)
        P.emit(final_ops)
    return nc


def _static_tables(n_out, seg, ext, nseg_rows=128):
    NTO = n_out // 128
    t0 = seg * S_OWN - (HALO if ext else 0)
    r_base = t0 // GRID_W
    rows = nseg_rows
    S = rows * GRID_W
    rowbias = np.zeros((128, NTO, 7, 2), np.float32)
    for p in range(NTO):
        for j in range(7):
            for kr2 in range(2):
                kr = r_base + 2 * p - 6 + 2 * j + kr2
                for rr in range(2):
                    r = r_base + 2 * p + rr
                    sr = min(max(r - 4, 0), rows - 8)
                    ok = (0 <= kr < rows) and (sr <= kr < sr + 8)
                    if not ok:
                        rowbias[kr2 * 64:(kr2 + 1) * 64, p, j, rr] = NEG
    n_in = n_out + 2 * HALO
    x0 = t0 - HALO
    if ext:
        regs = ((0, HALO), (HALO, 2 * HALO), (n_in - 2 * HALO, n_in - HALO), (n_in - HALO, n_in))
    else:
        regs = ((0, HALO), (0, 0), (0, 0), (n_in - HALO, n_in))
    flags = np.ones((128, 4), np.float32)
    for i, (a, b) in enumerate(regs):
        if b > a and (x0 + a < 0 or x0 + b > S):
            flags[:, i] = 0.0
    poolcorr = np.ones((128, 2, 16), np.float32)
    wins = (2, 4, 8, 16)
    cp0, cp1 = (HALO, n_out - HALO - 8) if ext else (0, n_out - 8)
    for g, w in enumerate(wins):
        pc, half = g // 2, g % 2
        for i in range(8):
            for (pos, t) in ((i, t0 + cp0 + i), (8 + i, t0 + cp1 + i)):
                lo = min(max(t - w // 2, 0), S)
                hi = min(max(t + w // 2, 0), S)
                if hi > lo:
                    poolcorr[half * 64:(half + 1) * 64, pc, pos] = np.float32(w) / np.float32(hi - lo)
    tok = t0 + np.arange(NTO)[None, :] * 128 + np.arange(128)[:, None]
    ok = (tok >= 0) & (tok < S)
    tokval = np.stack([ok.astype(np.float32), np.where(ok, 0.0, 1.0e6).astype(np.float32)], 1)
    return rowbias.reshape(128, NTO * 14), flags, poolcorr, np.ascontiguousarray(tokval)


def _bias_index():
    j = np.arange(7)[:, None, None]
    key = np.arange(128)[None, :, None]
    q = np.arange(128)[None, None, :]
    kr2, kc = key // 64, key % 64
    rr, c = q // 64, q % 64
    dr = (2 * j + kr2 - 6) - rr
    dc = kc - c
    sc = np.clip(c - 8, 0, GRID_W - 16)
    valid = (kc >= sc) & (kc < sc + 16) & (np.abs(dr) <= 7) & (np.abs(dc) <= 15)
    ri = np.clip(dr + 7, 0, 14)
    ci = np.clip(dc + 15, 0, 30)
    ri, ci, valid = np.broadcast_arrays(ri, ci, valid)
    return ri, ci, valid


_PROG_CACHE = {}
LAYER_CFG = ((S_OWN + 2 * HALO, 512, True), (S_OWN, 384, False))


def _get_prog():
    if "full" not in _PROG_CACHE:
        _PROG_CACHE["full"] = build_program()
    return _PROG_CACHE["full"]


def _layer_common(l, P, C):
    f32 = np.float32
    ri, ci, valid = _bias_index()
    rpb = P["rpb"][l]
    biasT = np.where(valid[None], rpb[:, ri, ci], f32(NEG)).astype(f32)
    w_pool = P["w_pool"][l]
    wpool_bd = np.zeros((2, 128, 128), f32)
    for g in range(4):
        pc, half = g // 2, g % 2
        wpool_bd[pc, half * 64:(half + 1) * 64, half * 64:(half + 1) * 64] = w_pool[g]
    return {
        "w_in": np.ascontiguousarray(P["w_in"][l]),
        "b_in_pc": np.ascontiguousarray(P["b_in"][l].reshape(18, 128).T),
        "bv_bc": np.ascontiguousarray(np.broadcast_to(P["b_in"][l][1280:1792], (128, 512))),
        "wpool_bd": wpool_bd,
        "pool_scale_pc": np.ascontiguousarray(P["pool_scale"][l].reshape(2, 128).T),
        "biasT": biasT,
        "conv_dw_pc": np.ascontiguousarray(P["conv_dw"][l][:, 0, :].reshape(31, 2, 128).transpose(2, 1, 0)),
        "conv_vec_pc": np.ascontiguousarray(np.stack([P["conv_dw_b"][l], P["conv_ln_g"][l], P["conv_ln_b"][l],
                                                      P["b_conv_pw"][l]], 0).reshape(4, 2, 128).transpose(2, 0, 1)),
        "w_pw": np.ascontiguousarray(P["w_conv_pw"][l]),
        "w_out": np.ascontiguousarray(P["w_out"][l]),
        "vec_bc": np.ascontiguousarray(np.broadcast_to(
            np.stack([P["b_out"][l], P["ln1_g"][l], P["ln1_b"][l], P["ln2_g"][l], P["ln2_b"][l]], 0)[:, None, :], (5, 128, D))),
        "w_router": np.ascontiguousarray(P["w_router"][l]),
        "b_router_bc": np.ascontiguousarray(np.broadcast_to(P["b_router"][l], (128, NE))),
        "ec_bc": np.ascontiguousarray(np.broadcast_to((np.arange(NE) * C).astype(f32), (128, NE))),
        "b_gate_pc": np.ascontiguousarray(P["b_gate"][l].reshape(NE, 8, 128).transpose(2, 0, 1)),
        "b_up_pc": np.ascontiguousarray(P["b_up"][l].reshape(NE, 8, 128).transpose(2, 0, 1)),
        "w_gate": np.ascontiguousarray(P["w_gate"][l]),
        "w_up": np.ascontiguousarray(P["w_up"][l]),
        "w_down": np.ascontiguousarray(P["w_down"][l]),
        "b_down": np.ascontiguousarray(P["b_down"][l]),
    }


def _in_maps(P):
    f32 = np.float32
    x_full = P["x"]
    B, S, _ = x_full.shape
    nseg = S // S_OWN
    shared = {}
    for l, (n_out, C, ext) in enumerate(LAYER_CFG):
        for k, v in _layer_common(l, P, C).items():
            shared["%s_%d" % (k, l)] = v
    in_maps = []
    for core in range(8):
        b, seg = core // nseg, core % nseg
        t0 = seg * S_OWN
        m = dict(shared)
        xe = np.zeros((S_OWN + 4 * HALO, D), f32)
        lo, hi = max(t0 - 2 * HALO, 0), min(t0 + S_OWN + 2 * HALO, S)
        xe[lo - (t0 - 2 * HALO):hi - (t0 - 2 * HALO)] = x_full[b, lo:hi]
        m["x_ext"] = xe
        for l, (n_out, C, ext) in enumerate(LAYER_CFG):
            rowbias, flags, poolcorr, tokval = _static_tables(n_out, seg, ext)
            m["tokval_%d" % l] = tokval
            m["rowbias_%d" % l] = rowbias
            m["flags_%d" % l] = flags
            m["poolcorr_%d" % l] = poolcorr
        in_maps.append(m)
    return in_maps


def kernel(**inputs):
    P = {k: np.asarray(v, dtype=np.float32) for k, v in inputs.items()}
    x = P["x"]
    B, S, _ = x.shape
    nc = _get_prog()
    res = run_bass_kernel_spmd(nc, _in_maps(P), core_ids=list(range(8)))
    out = np.empty_like(x)
    nseg = S // S_OWN
    for core in range(8):
        b, seg = core // nseg, core % nseg
        out[b, seg * S_OWN:(seg + 1) * S_OWN] = res.results[core]["y_out"]
    return out
```

```python
import numpy as np
import concourse.bass as bass
import concourse.mybir as mybir
from concourse.bass_utils import run_bass_kernel_spmd

F32 = mybir.dt.float32
BF16 = mybir.dt.bfloat16
I32 = mybir.dt.int32
AF = mybir.ActivationFunctionType
ALU = mybir.AluOpType

D = 1024
NE = 32
S_OWN = 2048
HALO = 256
GRID_W = 64
ALPHA = (2.0 * 2) ** 0.25
EPS = 1e-5
NEG = -1e30
DIN = 2304


class Buf:
    __slots__ = ("name", "ws", "reads", "sem", "cum")

    def __init__(self, name):
        self.name = name
        self.ws = []
        self.reads = []
        self.sem = None
        self.cum = 0


class Op:
    __slots__ = ("eng", "fn", "deps", "is_dma", "sem", "val", "signal", "seq")

    def __init__(self, eng, fn, is_dma):
        self.seq = 0
        self.eng = eng
        self.fn = fn
        self.deps = []
        self.is_dma = is_dma
        self.sem = None
        self.val = 0
        self.signal = False


class Prog:
    ENG = ("sync", "act", "dve", "pe", "pool")

    def __init__(self, nc):
        self.nc = nc
        self.ops = {e: [] for e in self.ENG}
        self.dma_sems = []
        self.fence_k = {}
        self.seq = 0
        self.marks = {}
        self.limit = None
        self.env = {"bc_vals": {}}

    def mark(self, name):
        self.marks[name] = self.seq

    def buf(self, name, scope=None):
        b = Buf(name)
        b.reads = list(self.fence_k.values())
        if scope is not None:
            scope.append(b)
        return b

    def close(self, scope_bufs):
        for b in scope_bufs:
            for o in list(b.ws) + list(b.reads):
                if o.is_dma:
                    k = ("dma", id(o.sem))
                    if k not in self.fence_k or self.fence_k[k].val < o.val:
                        self.fence_k[k] = o
                else:
                    k = ("eng", o.eng)
                    if k not in self.fence_k or self.fence_k[k].seq < o.seq:
                        self.fence_k[k] = o

    def _deps(self, op, reads, writes, indep=False):
        deps = []
        for b in reads:
            deps.extend(b.ws)
        for b in writes:
            if not indep:
                deps.extend(b.ws)
            deps.extend(b.reads)
        seen = set()
        for d in deps:
            if d.is_dma or op.is_dma or d.eng != op.eng or op.eng != "pe":
                if id(d) not in seen:
                    seen.add(id(d))
                    op.deps.append(d)
                d.signal = True
        for b in writes:
            if indep:
                b.ws.append(op)
            else:
                b.ws = [op]
                b.reads = []
        for b in reads:
            if op not in b.ws:
                b.reads.append(op)

    def op(self, eng, fn, reads=(), writes=(), indep=False):
        o = Op(eng, fn, False)
        self.seq += 1
        o.seq = self.seq
        self._deps(o, reads, writes, indep)
        self.ops[eng].append(o)
        return o

    def dma(self, eng, fn, reads=(), writes=(), sembuf=None, indep=False):
        o = Op(eng, fn, True)
        self.seq += 1
        o.seq = self.seq
        sb = sembuf if sembuf is not None else writes[0]
        if sb.sem is None:
            sb.sem = ("dma", len(self.dma_sems))
            self.dma_sems.append(sb)
        sb.cum += 16
        o.sem = sb
        o.val = sb.cum
        o.signal = True
        self._deps(o, reads, writes, indep)
        self.ops[eng].append(o)
        return o

    def emit(self, final_waits):
        nc = self.nc
        if self.limit is not None:
            lim = self.marks.get(self.limit, self.limit)
            lim = int(lim)
            for e in self.ENG:
                self.ops[e] = [o for o in self.ops[e] if o.seq <= lim]
            final_waits = [o for e in self.ENG for o in self.ops[e] if o.is_dma]
            for e in self.ENG:
                if self.ops[e] and not self.ops[e][-1].is_dma:
                    self.ops[e][-1].signal = True
                    final_waits.append(self.ops[e][-1])
        for e in self.ENG:
            c = 0
            for o in self.ops[e]:
                if not o.is_dma and o.signal:
                    c += 1
                    o.val = c
        import contextlib
        with contextlib.ExitStack() as st:
            esem = {e: st.enter_context(nc.semaphore("es_" + e)) for e in self.ENG}
            dsem = [st.enter_context(nc.semaphore("ds_%d" % i)) for i in range(len(self.dma_sems))]
            block = st.enter_context(nc.Block())

            def semof(o):
                if o.is_dma:
                    return dsem[o.sem.sem[1]]
                return esem[o.eng]

            def run(e, eng):
                waited = {}
                if e == "pool":
                    for L, v in self.env["bc_vals"].items():
                        self.env["bc_reg%d" % L] = eng.alloc_register("bc_reg%d" % L)
                        eng.reg_mov(self.env["bc_reg%d" % L], int(v))
                for o in self.ops[e]:
                    need = {}
                    for d in o.deps:
                        s = semof(d)
                        k = id(s)
                        if k not in need or d.val > need[k][1]:
                            need[k] = (s, d.val)
                    for k, (s, v) in need.items():
                        if waited.get(k, 0) >= v:
                            continue
                        waited[k] = v
                        eng.wait_ge(s, v)
                    ins = o.fn(eng)
                    if o.is_dma:
                        ins.then_inc(semof(o), 16)
                    elif o.signal:
                        ins.then_inc(esem[e], 1)
                if e == "sync":
                    for o in final_waits:
                        eng.wait_ge(semof(o), o.val)

            block.sync(lambda eng: run("sync", eng))
            block.scalar(lambda eng: run("act", eng))
            block.vector(lambda eng: run("dve", eng))
            block.tensor(lambda eng: run("pe", eng))
            block.gpsimd(lambda eng: run("pool", eng))


def build_program(stage="full", limit=None):
    nc = bass.Bass("TRN2", target_bir_lowering=False)
    P = Prog(nc)
    P.limit = limit
    P.env["bc_vals"] = {}
    import contextlib
    with contextlib.ExitStack() as glob:
        _names = {}

        def sb_global(name, shape, dt=F32, st=glob):
            k = _names.get(name, 0)
            _names[name] = k + 1
            if k:
                name = "%s_%d" % (name, k)
            return st.enter_context(nc.sbuf_tensor(name, list(shape), dt))

        sb = sb_global
        banks = [glob.enter_context(nc.psum_tensor("bank%d" % i, [128, 512], F32)) for i in range(8)]
        bankb = [Buf("bank%d" % i) for i in range(8)]

        ident_f = sb("ident_f", [128, 128])
        ident_b = sb("ident_b", [128, 128], BF16)
        ones_b = sb("ones_b", [128, 128], BF16)
        ones256 = sb("ones256", [128, 128], BF16)
        ustrict = sb("ustrict", [128, 128], BF16)
        B_const = Buf("const")
        iota_i = sb("iota_i", [128, 128], I32)
        iota_f = sb("iota_f", [128, 128])
        pidx_i = sb("pidx_i", [128, 1], I32)
        pidx_f = sb("pidx_f", [128, 1])
        P.op("pool", lambda g: g.iota(iota_i[:], [[1, 128]], base=0, channel_multiplier=0), writes=[B_const])
        P.op("pool", lambda g: g.iota(pidx_i[:], [[0, 1]], base=0, channel_multiplier=1), writes=[B_const])
        P.op("dve", lambda v: v.tensor_copy(out=iota_f[:], in_=iota_i[:]), reads=[B_const], writes=[B_const])
        P.op("dve", lambda v: v.tensor_copy(out=pidx_f[:], in_=pidx_i[:]), reads=[B_const], writes=[B_const])
        P.op("dve", lambda v: v.tensor_scalar(out=ident_f[:], in0=iota_f[:], scalar1=pidx_f[:, 0:1], scalar2=None,
                                              op0=ALU.is_equal), reads=[B_const], writes=[B_const])
        P.op("dve", lambda v: v.tensor_copy(out=ident_b[:], in_=ident_f[:]), writes=[B_const])
        P.op("dve", lambda v: v.tensor_scalar(out=ustrict[:], in0=iota_f[:], scalar1=pidx_f[:, 0:1], scalar2=None,
                                              op0=ALU.is_gt), writes=[B_const])
        P.op("dve", lambda v: v.memset(ones_b[:], 1.0), writes=[B_const])
        P.op("dve", lambda v: v.memset(ones256[:], 1.0 / 256.0), writes=[B_const])

        final_ops = []

        def emit_layer(LI, n_out, C, ext, x_ext, src_reads, dst, B_dst):
            n_in = n_out + 2 * HALO
            NTI = n_in // 128
            NTO = n_out // 128
            HT = HALO // 128
            NS = C // 128
            NSLOT = NE * C
            P.env["bc_vals"][LI] = NSLOT - 1
            if ext:
                FLAG_REG = ((0, HALO, 0), (HALO, 2 * HALO, 1), (n_in - 2 * HALO, n_in - HALO, 2), (n_in - HALO, n_in, 3))
                CP0, CP1 = HALO, n_out - HALO - 8
            else:
                FLAG_REG = ((0, HALO, 0), (n_in - HALO, n_in, 3))
                CP0, CP1 = 0, n_out - 8

            def din(name, shape, dt=F32):
                return nc.dram_tensor("%s_%d" % (name, LI), list(shape), dt, kind="ExternalInput").ap()

            w_in = din("w_in", [D, DIN])
            b_in_pc = din("b_in_pc", [128, 18])
            bv_bc = din("bv_bc", [128, 512])
            wpool_bd = din("wpool_bd", [2, 128, 128])
            pool_scale_pc = din("pool_scale_pc", [128, 2])
            poolcorr = din("poolcorr", [128, 2, 16])
            flags = din("flags", [128, 4])
            biasT = din("biasT", [8, 7, 128, 128])
            rowbias = din("rowbias", [128, NTO * 14])
            tokval = din("tokval", [128, 2, NTO])
            conv_dw_pc = din("conv_dw_pc", [128, 2, 31])
            conv_vec_pc = din("conv_vec_pc", [128, 4, 2])
            w_pw = din("w_pw", [256, 256])
            w_out = din("w_out", [D, D])
            vec_bc = din("vec_bc", [5, 128, D])
            w_router = din("w_router", [D, NE])
            b_router_bc = din("b_router_bc", [128, NE])
            ec_bc = din("ec_bc", [128, NE])
            b_gate_pc = din("b_gate_pc", [128, NE, 8])
            b_up_pc = din("b_up_pc", [128, NE, 8])
            if True:
                w_gate = din("w_gate", [NE, D, D])
                w_up = din("w_up", [NE, D, D])
                w_down = din("w_down", [NE, D, D])
                b_down = din("b_down", [NE, D])
            x1d = nc.dram_tensor("x1d_%d" % LI, [n_out, D], F32, kind="Internal").ap()
            xs = nc.dram_tensor("xs_%d" % LI, [NSLOT, D], BF16, kind="Internal").ap()
            ys = nc.dram_tensor("ys_%d" % LI, [NSLOT, D], F32, kind="Internal").ap()
            with contextlib.ExitStack() as top:
                top_bufs = []

                def Buf_(name):
                    return P.buf(name, top_bufs)

                def sb(name, shape, dt=F32, st=top):
                    return sb_global(name, shape, dt, st)

                B_par = Buf_("params")
                b_in_sb = sb("b_in_sb", [128, 18])
                flags_sb = sb("flags_sb", [128, 4])
                pscale_sb = sb("pscale_sb", [128, 2])
                pcorr_sb = sb("pcorr_sb", [128, 2, 16])
                rowb_sb = sb("rowb_sb", [128, NTO * 14])
                tokv_sb = sb("tokv_sb", [128, 2, NTO])
                cdw_sb = sb("cdw_sb", [128, 2, 31])
                cvec_sb = sb("cvec_sb", [128, 4, 2])
                brt_sb = sb("brt_sb", [128, NE])
                ec_sb = sb("ec_sb", [128, NE])
                bg_sb = sb("bg_sb", [128, NE, 8])
                bu_sb = sb("bu_sb", [128, NE, 8])
                bu1_sb = sb("bu1_sb", [128, NE, 8])
                B_bu1 = Buf_("bu1")
                for pdst, psrc in ((b_in_sb, b_in_pc), (flags_sb, flags), (pscale_sb, pool_scale_pc), (pcorr_sb, poolcorr),
                                 (rowb_sb, rowbias), (tokv_sb, tokval), (cdw_sb, conv_dw_pc), (cvec_sb, conv_vec_pc), (brt_sb, b_router_bc),
                                 (ec_sb, ec_bc), (bg_sb, b_gate_pc), (bu_sb, b_up_pc)):
                    P.dma("sync", (lambda d, s: (lambda q: q.dma_start(out=d[:], in_=s)))(pdst, psrc), writes=[B_par], indep=True)

                P.op("dve", lambda v: v.tensor_scalar(out=bu1_sb[:], in0=bu_sb[:], scalar1=1.0, scalar2=None, op0=ALU.add),
                     reads=[B_par], writes=[B_bu1])

                x1t = [sb("x1t%d" % i, [128, D]) for i in range(2)]
                B_x1t = [Buf_("x1t0"), Buf_("x1t1")]
                stats = sb("stats", [128, 2, 6])
                mv = sb("mv", [128, 2])
                rstd1 = sb("rstd1", [128, 1])
                B_st = Buf_("stats")

                x1b = [sb("x1b%d" % i, [128, D], BF16) for i in range(2)]
                B_x1b = [Buf_("x1b0"), Buf_("x1b1")]
                x1T = sb("x1T", [128, 8, 128])
                B_x1T = Buf_("x1T")
                wr_sb = sb("wr_sb", [128, 8, NE])
                B_wr = Buf_("wr")
                logit = sb("logit", [128, NE])
                top8 = sb("top8", [128, 8])
                negv0 = sb("negv0", [128, 1])
                ex4 = sb("ex4", [128, 4])
                ssum = sb("ssum", [128, 1])
                gates = sb("gates", [128, NTO, 4])
                mask_all = sb("mask_all", [128, NTO, NE], BF16)
                Atab = sb("Atab", [128, NE])
                ovf = sb("ovf", [128, NE])
                junk = sb("junk", [128, NE])
                slot_f = sb("slot_f", [128, 4])
                slots = sb("slots", [128, NTO, 4], I32)
                B_rt, B_gates, B_mask, B_slots = Buf_("rt"), Buf_("gates"), Buf_("mask"), Buf_("slots")
                B_x1d = Buf_("x1d")
                B_xs = Buf_("xs")
                tmp_t = [sb("tmp_t%d" % i, [128, D]) for i in range(2)]
                B_tmp = [Buf_("tmp0"), Buf_("tmp1")]

                with contextlib.ExitStack() as mx:
                    mx_bufs = []

                    def msb(name, shape, dt=F32):
                        return sb(name, shape, dt, st=mx)

                    ycatT = msb("ycatT", [128, 8, n_out], BF16)
                    B_ycat = [P.buf("ycat%d" % c, mx_bufs) for c in range(8)]
                    xin = [msb("xin%d" % i, [128, D]) for i in range(2)]
                    B_xin = [P.buf("xin%d" % i, mx_bufs) for i in range(2)]
                    B_xs0 = Buf_("xs_zero")
                    w_in_v = w_in.rearrange("(c p) f -> p c f", p=128)
                    tblocks = [(s, min(512, n_in - s)) for s in range(0, n_in, 512)]

                    with contextlib.ExitStack() as sa:
                        sa_bufs = []
                        xT = sb("xT", [128, 8, n_in], BF16, st=sa)
                        B_xT = [P.buf("xT%d" % t, sa_bufs) for t in range(NTI)]

                        for t in range(NTI):
                            xi, bxi = xin[t % 2], B_xin[t % 2]
                            P.dma("sync", (lambda t, xi: (lambda q: q.dma_start(out=xi[:], in_=x_ext[t * 128:(t + 1) * 128, :])))(t, xi),
                                  reads=src_reads, writes=[bxi])
                            for hh in range(2):
                                bk = (t % 2) * 2 + hh
                                for c4 in range(4):
                                    c = hh * 4 + c4
                                    P.op("pe", (lambda xi, c, c4, bk: (lambda pe: pe.transpose(out=banks[bk][:, c4 * 128:(c4 + 1) * 128],
                                                                                                in_=xi[:, c * 128:(c + 1) * 128],
                                                                                                identity=ident_f[:])))(xi, c, c4, bk),
                                         reads=[bxi, B_const], writes=[bankb[bk]])
                                if hh == 0:
                                    P.op("act", (lambda t, hh, bk: (lambda a: a.copy(
                                        out=xT[:, hh * 4:(hh + 1) * 4, t * 128:(t + 1) * 128],
                                        in_=banks[bk][:].rearrange("p (c f) -> p c f", c=4))))(t, hh, bk),
                                         reads=[bankb[bk]], writes=[B_xT[t]])
                                else:
                                    P.op("dve", (lambda t, hh, bk: (lambda v: v.tensor_copy(
                                        out=xT[:, hh * 4:(hh + 1) * 4, t * 128:(t + 1) * 128],
                                        in_=banks[bk][:].rearrange("p (c f) -> p c f", c=4))))(t, hh, bk),
                                         reads=[bankb[bk]], writes=[B_xT[t]])

                        if stage == "full":
                            P.op("pool", lambda g: g.memset(ycatT[:], 0.0), writes=B_ycat)
                            zsrc = ycatT[:].rearrange("p c n -> p (c n)")
                            per = (8 * n_out) // D
                            for r0 in range(0, NSLOT // 128, per):
                                nr = min(per, NSLOT // 128 - r0)
                                P.dma("sync", (lambda r0, nr: (lambda q: q.dma_start(
                                    out=xs[r0 * 128:(r0 + nr) * 128, :].rearrange("(s p) d -> p s d", p=128),
                                    in_=zsrc[:, 0:nr * D].rearrange("p (s d) -> p s d", d=D))))(r0, nr),
                                      reads=B_ycat, writes=[B_xs0], indep=True)
                        P.mark('phase0')

                        def xt_bufs(s, n):
                            return [B_xT[t] for t in range(s // 128, (s + n + 127) // 128)]

                        def inproj(wt, wcol, bw, s, n, bk):
                            for dch in range(8):
                                P.op("pe", (lambda dch: (lambda pe: pe.matmul(banks[bk][:, 0:n],
                                                                               lhsT=wt[:, dch, wcol:wcol + 128],
                                                                               rhs=xT[:, dch, s:s + n],
                                                                               start=(dch == 0), stop=(dch == 7))))(dch),
                                     reads=[bw] + xt_bufs(s, n), writes=[bankb[bk]])

                        with contextlib.ExitStack() as sp:
                            sp_bufs = []
                            wsl_p = sb("wsl_p", [128, 8, 256], BF16, st=sp)
                            B_wslp = P.buf("wsl_p", sp_bufs)
                            P.dma("pool", lambda g: g.dma_start(out=wsl_p[:], in_=w_in_v[:, :, 0:256]), writes=[B_wslp])
                            wpool_sb = sb("wpool_sb", [128, 2, 128], BF16, st=sp)
                            B_wp = P.buf("wpool", sp_bufs)
                            P.dma("pool", lambda g: g.dma_start(out=wpool_sb[:], in_=wpool_bd.rearrange("c p f -> p c f")), writes=[B_wp])
                            PADP = 16
                            uT = sb("uT", [128, n_in + PADP], st=sp)
                            aT = sb("aT", [128, n_in + PADP], st=sp)
                            a2T = sb("a2T", [128, n_in + PADP], st=sp)
                            mT = sb("mT", [128, n_out], st=sp)
                            dT = sb("dT", [128, n_out], BF16, st=sp)
                            B_u, B_a, B_a2, B_m, B_d = (P.buf(nm, sp_bufs) for nm in ("uT", "aT", "a2T", "mT", "dT"))
                            for pc in range(2):
                                P.op("dve", lambda v: v.memset(uT[:, n_in:n_in + PADP], 0.0), writes=[B_u])
                                for bi, (s, n) in enumerate(tblocks):
                                    bk = 4 + (bi % 2)
                                    inproj(wsl_p, pc * 128, B_wslp, s, n, bk)
                                    P.op("act", (lambda s, n, bk, pc: (lambda a: a.activation(out=uT[:, s:s + n], in_=banks[bk][:, 0:n],
                                                                                               func=AF.Identity, bias=b_in_sb[:, pc:pc + 1],
                                                                                               scale=1.0)))(s, n, bk, pc),
                                         reads=[bankb[bk], B_par], writes=[B_u])
                                for (fa, fb, fc) in FLAG_REG:
                                    P.op("dve", (lambda fa, fb, fc: (lambda v: v.tensor_scalar(out=uT[:, fa:fb], in0=uT[:, fa:fb],
                                                                                               scalar1=flags_sb[:, fc:fc + 1], scalar2=None,
                                                                                               op0=ALU.mult)))(fa, fb, fc),
                                         reads=[B_par], writes=[B_u])
                                L = n_in
                                P.op("dve", lambda v: v.tensor_tensor(out=aT[:, 0:L], in0=uT[:, 0:L], in1=uT[:, 1:L + 1], op=ALU.add),
                                     reads=[B_u], writes=[B_a])
                                P.op("dve", lambda v: v.memset(aT[:, L:L + PADP], 0.0), writes=[B_a])
                                P.op("dve", lambda v: v.tensor_tensor(out=a2T[:, 0:L], in0=aT[:, 0:L], in1=aT[:, 2:L + 2], op=ALU.add),
                                     reads=[B_a], writes=[B_a2])
                                P.op("dve", lambda v: v.memset(a2T[:, L:L + PADP], 0.0), writes=[B_a2])
                                o0 = HALO
                                if pc == 0:
                                    P.op("dve", lambda v: v.tensor_scalar(out=mT[0:64, :], in0=aT[0:64, o0 - 1:o0 - 1 + n_out], scalar1=0.5,
                                                                          scalar2=None, op0=ALU.mult), reads=[B_a], writes=[B_m])
                                    P.op("dve", lambda v: v.tensor_scalar(out=mT[64:128, :], in0=a2T[64:128, o0 - 2:o0 - 2 + n_out],
                                                                          scalar1=0.25, scalar2=None, op0=ALU.mult), reads=[B_a2], writes=[B_m])
                                else:
                                    P.op("dve", lambda v: v.tensor_tensor(out=aT[:, 0:L], in0=a2T[:, 0:L], in1=a2T[:, 4:L + 4], op=ALU.add),
                                         reads=[B_a2], writes=[B_a])
                                    P.op("dve", lambda v: v.tensor_tensor(out=a2T[:, 0:L], in0=aT[:, 0:L], in1=aT[:, 8:L + 8], op=ALU.add),
                                         reads=[B_a], writes=[B_a2])
                                    P.op("dve", lambda v: v.tensor_scalar(out=mT[0:64, :], in0=aT[0:64, o0 - 4:o0 - 4 + n_out], scalar1=0.125,
                                                                          scalar2=None, op0=ALU.mult), reads=[B_a], writes=[B_m])
                                    P.op("dve", lambda v: v.tensor_scalar(out=mT[64:128, :], in0=a2T[64:128, o0 - 8:o0 - 8 + n_out],
                                                                          scalar1=0.0625, scalar2=None, op0=ALU.mult), reads=[B_a2], writes=[B_m])
                                P.op("dve", (lambda pc: (lambda v: v.tensor_tensor(out=mT[:, CP0:CP0 + 8], in0=mT[:, CP0:CP0 + 8], in1=pcorr_sb[:, pc, 0:8],
                                                                                    op=ALU.mult)))(pc), reads=[B_par], writes=[B_m])
                                P.op("dve", (lambda pc: (lambda v: v.tensor_tensor(out=mT[:, CP1:CP1 + 8], in0=mT[:, CP1:CP1 + 8],
                                                                                    in1=pcorr_sb[:, pc, 8:16], op=ALU.mult)))(pc),
                                     reads=[B_par], writes=[B_m])
                                P.op("dve", lambda v: v.tensor_tensor(out=dT[:], in0=mT[:], in1=uT[:, o0:o0 + n_out], op=ALU.subtract),
                                     reads=[B_m, B_u], writes=[B_d])
                                for bi in range(n_out // 512):
                                    bk = 6 + (bi % 2)
                                    P.op("pe", (lambda bi, bk, pc: (lambda pe: pe.matmul(banks[bk][:], lhsT=wpool_sb[:, pc, :],
                                                                                          rhs=dT[:, bi * 512:(bi + 1) * 512],
                                                                                          start=True, stop=True)))(bi, bk, pc),
                                         reads=[B_wp, B_d], writes=[bankb[bk]])
                                    P.op("act", (lambda bi, bk, pc: (lambda a: a.activation(out=ycatT[:, pc, bi * 512:(bi + 1) * 512],
                                                                                             in_=banks[bk][:], func=AF.Copy,
                                                                                             scale=pscale_sb[:, pc:pc + 1])))(bi, bk, pc),
                                         reads=[bankb[bk], B_par], writes=[B_ycat[pc]])
                        P.close(sp_bufs)
                        P.mark('pool')

                        with contextlib.ExitStack() as sc:
                            sc_bufs = []
                            wsl_c = sb("wsl_c", [128, 8, 512], BF16, st=sc)
                            B_wslc = P.buf("wsl_c", sc_bufs)
                            P.dma("pool", lambda g: g.dma_start(out=wsl_c[:], in_=w_in_v[:, :, 1792:2304]), writes=[B_wslc])
                            wpw_sb = sb("wpw_sb", [128, 2, 256], BF16, st=sc)
                            B_wpw = P.buf("wpw", sc_bufs)
                            P.dma("pool", lambda g: g.dma_start(out=wpw_sb[:], in_=w_pw.rearrange("(c p) f -> p c f", p=128)), writes=[B_wpw])
                            hT = sb("hT", [128, 2, n_in], BF16, st=sc)
                            B_h = [P.buf("hT0", sc_bufs), P.buf("hT1", sc_bufs)]
                            sg = sb("sg", [128, 512], st=sc)
                            B_sg = P.buf("sg", sc_bufs)
                            for j in range(2):
                                for bi, (s, n) in enumerate(tblocks):
                                    inproj(wsl_c, 256 + j * 128, B_wslc, s, n, 4)
                                    P.op("act", (lambda s, n, j: (lambda a: a.activation(out=sg[:, 0:n], in_=banks[4][:, 0:n], func=AF.Sigmoid,
                                                                                         bias=b_in_sb[:, 16 + j:17 + j], scale=1.0)))(s, n, j),
                                         reads=[bankb[4], B_par], writes=[B_sg])
                                    inproj(wsl_c, j * 128, B_wslc, s, n, 5)
                                    P.op("dve", (lambda s, n, j: (lambda v: v.scalar_tensor_tensor(out=hT[:, j, s:s + n], in0=banks[5][:, 0:n],
                                                                                                    scalar=b_in_sb[:, 14 + j:15 + j],
                                                                                                    in1=sg[:, 0:n], op0=ALU.add,
                                                                                                    op1=ALU.mult)))(s, n, j),
                                         reads=[bankb[5], B_sg, B_par], writes=[B_h[j]])
                                for (fa, fb, fc) in FLAG_REG:
                                    P.op("dve", (lambda j, fa, fb, fc: (lambda v: v.tensor_scalar(out=hT[:, j, fa:fb], in0=hT[:, j, fa:fb],
                                                                                                  scalar1=flags_sb[:, fc:fc + 1], scalar2=None,
                                                                                                  op0=ALU.mult)))(j, fa, fb, fc),
                                         reads=[B_par], writes=[B_h[j]])
                            dg = sb("dg", [128, 2, 31, 128], BF16, st=sc)
                            B_dg = P.buf("dg", sc_bufs)
                            for j in range(2):
                                for k in range(31):
                                    P.op("dve", (lambda j, k: (lambda v: v.tensor_scalar(out=dg[:, j, k, :], in0=ident_f[:],
                                                                                         scalar1=cdw_sb[:, j, k:k + 1], scalar2=None,
                                                                                         op0=ALU.mult)))(j, k),
                                         reads=[B_par, B_const], writes=[B_dg])
                            cT = sb("cT", [128, 2, 512], st=sc)
                            cTb = sb("cTb", [128, 2, 512], BF16, st=sc)
                            sqb = sb("sqb", [128, 2, 512], BF16, st=sc)
                            mean_sb = sb("mean_sb", [128, 512], st=sc)
                            m2_sb = sb("m2_sb", [128, 512], st=sc)
                            rstd_sb = sb("rstd_sb", [128, 512], st=sc)
                            nrm = sb("nrm", [128, 2, 512], st=sc)
                            silu_b = sb("silu_b", [128, 2, 512], BF16, st=sc)
                            B_cT, B_cTb, B_sq, B_mean, B_m2, B_rstd, B_nrm, B_silu = (P.buf(nm, sc_bufs) for nm in
                                                                                      ("cT", "cTb", "sqb", "mean", "m2", "rstd", "nrm", "silu"))
                            for bi in range(n_out // 512):
                                t0 = HALO + bi * 512
                                for j in range(2):
                                    bk = 6 + j
                                    for k in range(31):
                                        P.op("pe", (lambda j, k, bk, t0: (lambda pe: pe.matmul(banks[bk][:], lhsT=dg[:, j, k, :],
                                                                                                rhs=hT[:, j, t0 + k - 15:t0 + k - 15 + 512],
                                                                                                start=(k == 0), stop=(k == 30))))(j, k, bk, t0),
                                             reads=[B_dg, B_h[j]], writes=[bankb[bk]])
                                    P.op("act", (lambda j, bk: (lambda a: a.activation(out=cT[:, j, :], in_=banks[bk][:], func=AF.Identity,
                                                                                       bias=cvec_sb[:, 0, j:j + 1], scale=1.0)))(j, bk),
                                         reads=[bankb[bk], B_par], writes=[B_cT])
                                P.op("dve", lambda v: v.tensor_copy(out=cTb[:], in_=cT[:]), reads=[B_cT], writes=[B_cTb])
                                P.op("act", lambda a: a.activation(out=sqb[:], in_=cT[:], func=AF.Square), reads=[B_cT], writes=[B_sq])
                                for j in range(2):
                                    P.op("pe", (lambda j: (lambda pe: pe.matmul(banks[4][:], lhsT=ones256[:], rhs=cTb[:, j, :],
                                                                                 start=(j == 0), stop=(j == 1))))(j),
                                         reads=[B_const, B_cTb], writes=[bankb[4]])
                                for j in range(2):
                                    P.op("pe", (lambda j: (lambda pe: pe.matmul(banks[5][:], lhsT=ones256[:], rhs=sqb[:, j, :],
                                                                                 start=(j == 0), stop=(j == 1))))(j),
                                         reads=[B_const, B_sq], writes=[bankb[5]])
                                P.op("act", lambda a: a.copy(out=mean_sb[:], in_=banks[4][:]), reads=[bankb[4]], writes=[B_mean])
                                P.op("dve", lambda v: v.tensor_tensor(out=m2_sb[:], in0=mean_sb[:], in1=mean_sb[:], op=ALU.mult),
                                     reads=[B_mean], writes=[B_m2])
                                P.op("dve", lambda v: v.scalar_tensor_tensor(out=m2_sb[:], in0=banks[5][:], scalar=EPS, in1=m2_sb[:],
                                                                             op0=ALU.add, op1=ALU.subtract), reads=[bankb[5]], writes=[B_m2])
                                P.op("act", lambda a: a.activation(out=m2_sb[:], in_=m2_sb[:], func=AF.Sqrt), reads=[B_m2], writes=[B_m2])
                                P.op("dve", lambda v: v.reciprocal(out=rstd_sb[:], in_=m2_sb[:]), reads=[B_m2], writes=[B_rstd])
                                for j in range(2):
                                    P.op("dve", (lambda j: (lambda v: v.tensor_tensor(out=nrm[:, j, :], in0=cT[:, j, :], in1=mean_sb[:],
                                                                                      op=ALU.subtract)))(j), reads=[B_cT, B_mean], writes=[B_nrm])
                                    P.op("dve", (lambda j: (lambda v: v.tensor_tensor(out=nrm[:, j, :], in0=nrm[:, j, :], in1=rstd_sb[:],
                                                                                      op=ALU.mult)))(j), reads=[B_rstd], writes=[B_nrm])
                                    P.op("act", (lambda j: (lambda a: a.activation(out=silu_b[:, j, :], in_=nrm[:, j, :], func=AF.Silu,
                                                                                   bias=cvec_sb[:, 2, j:j + 1], scale=cvec_sb[:, 1, j:j + 1])))(j),
                                         reads=[B_nrm, B_par], writes=[B_silu])
                                for cc in range(2):
                                    bk = 6 + cc
                                    for j in range(2):
                                        P.op("pe", (lambda j, cc, bk: (lambda pe: pe.matmul(banks[bk][:], lhsT=wpw_sb[:, j, cc * 128:(cc + 1) * 128],
                                                                                             rhs=silu_b[:, j, :], start=(j == 0),
                                                                                             stop=(j == 1))))(j, cc, bk),
                                             reads=[B_wpw, B_silu], writes=[bankb[bk]])
                                    P.op("act", (lambda cc, bk, bi: (lambda a: a.activation(out=ycatT[:, 6 + cc, bi * 512:(bi + 1) * 512],
                                                                                             in_=banks[bk][:], func=AF.Identity,
                                                                                             bias=cvec_sb[:, 3, cc:cc + 1], scale=1.0)))(cc, bk, bi),
                                         reads=[bankb[bk], B_par], writes=[B_ycat[6 + cc]])
                        P.close(sc_bufs)
                        P.mark('conv')

                        sidx_box = [0]

                        def attn_group(hg):
                            with contextlib.ExitStack() as sat:
                                at_bufs = []
                                wsl = sb("wsl_a", [128, 8, 3, 256], BF16, st=sat)
                                B_wsl = P.buf("wsl_a", at_bufs)
                                for qi, c0 in enumerate((256, 768, 1280)):
                                    P.dma("pool", (lambda qi, c0: (lambda g: g.dma_start(out=wsl[:, :, qi, :],
                                                                                         in_=w_in_v[:, :, c0 + hg * 256:c0 + (hg + 1) * 256])))(qi, c0),
                                          writes=[B_wsl], indep=True)
                                bv_sb = sb("bv_sb", [128, 256], st=sat)
                                B_bv = P.buf("bv", at_bufs)
                                P.dma("sync", lambda q: q.dma_start(out=bv_sb[:], in_=bv_bc[:, hg * 256:(hg + 1) * 256]), writes=[B_bv])
                                qT = sb("qT", [128, 4, n_out], BF16, st=sat)
                                kT = sb("kT", [128, 2, n_in], BF16, st=sat)
                                B_q, B_k = P.buf("qT", at_bufs), P.buf("kT", at_bufs)
                                P.op("pool", lambda g: g.memset(qT[:], 0.0), writes=[B_q])
                                wq = wsl[:, :, 0, :]
                                wk = wsl[:, :, 1, :]
                                for c in range(2):
                                    gch = 2 + hg * 2 + c
                                    for bi in range(n_out // 512):
                                        bk = 4 + (bi % 2)
                                        inproj(wq, c * 128, B_wsl, HALO + bi * 512, 512, bk)
                                        for hh2 in range(2):
                                            P.op("act", (lambda c, bi, bk, gch, hh2: (lambda a: a.activation(
                                                out=qT[hh2 * 64:(hh2 + 1) * 64, 2 * c + hh2, bi * 512:(bi + 1) * 512],
                                                in_=banks[bk][hh2 * 64:(hh2 + 1) * 64, :],
                                                func=AF.Identity, bias=b_in_sb[hh2 * 64:(hh2 + 1) * 64, gch:gch + 1],
                                                scale=1.0)))(c, bi, bk, gch, hh2),
                                                 reads=[bankb[bk], B_par], writes=[B_q])
                                    for bi, (s, n) in enumerate(tblocks):
                                        bk = 6 + (bi % 2)
                                        inproj(wk, c * 128, B_wsl, s, n, bk)
                                        P.op("dve", (lambda c, s, n, bk, gch: (lambda v: v.tensor_scalar(out=kT[:, c, s:s + n], in0=banks[bk][:, 0:n],
                                                                                                          scalar1=b_in_sb[:, gch + 4:gch + 5], scalar2=None,
                                                                                                          op0=ALU.add)))(c, s, n, bk, gch),
                                             reads=[bankb[bk], B_par], writes=[B_k])
                                P.mark('a_qk%d' % hg)
                                vaug = sb("vaug", [128, NTI, 4, 65], BF16, st=sat)
                                B_v = P.buf("vaug", at_bufs)
                                P.op("dve", lambda v: v.memset(vaug[:, :, :, 64:65], 1.0), writes=[B_v])
                                for t in range(NTI):
                                    bk = 4 + (t % 2)
                                    for dch in range(8):
                                        P.op("pe", (lambda t, dch, bk: (lambda pe: pe.matmul(banks[bk][:, 0:256], lhsT=xT[:, dch, t * 128:(t + 1) * 128],
                                                                                              rhs=wsl[:, dch, 2, :],
                                                                                              start=(dch == 0), stop=(dch == 7))))(t, dch, bk),
                                             reads=[B_wsl, B_xT[t]], writes=[bankb[bk]])
                                    P.op("dve", (lambda t, bk: (lambda v: v.tensor_tensor(out=vaug[:, t, :, 0:64],
                                                                                          in0=banks[bk][:, 0:256].rearrange("p (h d) -> p h d", h=4),
                                                                                          in1=bv_sb[:].rearrange("p (h d) -> p h d", h=4),
                                                                                          op=ALU.add)))(t, bk),
                                         reads=[bankb[bk], B_bv], writes=[B_v])
                                P.mark('a_v%d' % hg)
                                Etab = sb("Etab", [128, 4, 7, 128], BF16, st=sat)
                                B_E = P.buf("Etab", at_bufs)
                                bstage = [sb("bstage0", [128, 7, 128], st=sat)] * 2
                                B_bst = [P.buf("bst0", at_bufs)] * 2
                                for h4 in range(4):
                                    P.dma("sync", (lambda h4: (lambda q: q.dma_start(out=bstage[h4 % 2][:],
                                                                                     in_=biasT[hg * 4 + h4].rearrange("j k q -> k j q"))))(h4),
                                          writes=[B_bst[h4 % 2]])
                                    P.op("act", (lambda h4: (lambda a: a.activation(out=Etab[:, h4, :, :], in_=bstage[h4 % 2][:], func=AF.Exp)))(h4),
                                         reads=[B_bst[h4 % 2]], writes=[B_E])
                                P.mark('a_E%d' % hg)
                                pt = sb("pT", [128, 7, 512], BF16, st=sat)
                                bpt = [P.buf("pT%d" % j, at_bufs) for j in range(7)]
                                yat = [sb("yat%d" % i, [128, 256], BF16, st=sat) for i in range(2)]
                                B_yat = [P.buf("yat0", at_bufs), P.buf("yat1", at_bufs)]
                                rec = sb("rec", [128, 4], st=sat)
                                B_rec = P.buf("rec", at_bufs)
                                for p in range(NTO):
                                    qt = p + HT
                                    jt = [(j, qt - 3 + j) for j in range(7) if 0 <= qt - 3 + j < NTI]
                                    for (j, kt) in jt:
                                        bk = sidx_box[0] % 4
                                        sidx_box[0] += 1
                                        for h4 in range(4):
                                            ch, hp = h4 // 2, (h4 % 2) * 64
                                            P.op("pe", (lambda bk, h4, ch, hp, kt, p: (lambda pe: pe.matmul(
                                                banks[bk][:, h4 * 128:(h4 + 1) * 128],
                                                lhsT=kT[:, ch, kt * 128:(kt + 1) * 128],
                                                rhs=qT[:, h4, p * 128:(p + 1) * 128], start=True, stop=True)))(bk, h4, ch, hp, kt, p),
                                                 reads=[B_q, B_k], writes=[bankb[bk]])
                                        for rr in range(2):
                                            col = (p * 7 + j) * 2 + rr
                                            P.op("act", (lambda bk, j, rr, col: (lambda a: a.activation(
                                                out=pt[:, j, :].rearrange("p (h r c) -> p h r c", h=4, r=2)[:, :, rr, :],
                                                in_=banks[bk][:].rearrange("p (h r c) -> p h r c", h=4, r=2)[:, :, rr, :],
                                                func=AF.Exp, bias=rowb_sb[:, col:col + 1], scale=0.125)))(bk, j, rr, col),
                                                 reads=[bankb[bk], B_par], writes=[bpt[j]], indep=(rr == 1))
                                        P.op("dve", (lambda j: (lambda v: v.tensor_tensor(
                                            out=pt[:, j, :].rearrange("p (h q) -> p h q", h=4),
                                            in0=pt[:, j, :].rearrange("p (h q) -> p h q", h=4),
                                            in1=Etab[:, :, j, :], op=ALU.mult)))(j),
                                             reads=[B_E], writes=[bpt[j]])
                                    P.mark('a_S%d_%d' % (hg, p))
                                    ob = 4 + (p % 2)
                                    for h4 in range(4):
                                        for ji, (j, kt) in enumerate(jt):
                                            P.op("pe", (lambda ob, h4, j, kt, ji, nj: (lambda pe: pe.matmul(
                                                banks[ob][:, h4 * 65:(h4 + 1) * 65], lhsT=pt[:, j, h4 * 128:(h4 + 1) * 128],
                                                rhs=vaug[:, kt, h4, :], start=(ji == 0), stop=(ji == nj - 1))))(ob, h4, j, kt, ji, len(jt)),
                                                 reads=[bpt[j], B_v], writes=[bankb[ob]])
                                    P.mark('a_AV%d_%d' % (hg, p))
                                    ya, bya = yat[p % 2], B_yat[p % 2]
                                    P.op("dve", (lambda ob: (lambda v: v.reciprocal(
                                        out=rec[:], in_=banks[ob][:, 0:260].rearrange("p (h d) -> p h d", h=4)[:, :, 64])))(ob),
                                         reads=[bankb[ob]], writes=[B_rec])
                                    for h4 in range(4):
                                        P.op("dve", (lambda ob, h4, ya: (lambda v: v.tensor_scalar(
                                            out=ya[:, h4 * 64:(h4 + 1) * 64], in0=banks[ob][:, h4 * 65:h4 * 65 + 64],
                                            scalar1=rec[:, h4:h4 + 1], scalar2=None, op0=ALU.mult)))(ob, h4, ya),
                                             reads=[bankb[ob], B_rec], writes=[bya])
                                    P.mark('a_N%d_%d' % (hg, p))
                                    tb = 6 + (p % 2)
                                    tbv = banks[tb][:].bitcast(BF16)
                                    for c2 in range(2):
                                        P.op("pe", (lambda c2, ya, tbv: (lambda pe: pe.transpose(out=tbv[:, c2 * 128:(c2 + 1) * 128],
                                                                                                  in_=ya[:, c2 * 128:(c2 + 1) * 128],
                                                                                                  identity=ident_b[:])))(c2, ya, tbv),
                                             reads=[bya, B_const], writes=[bankb[tb]])
                                    P.op("act", (lambda p, tbv: (lambda a: a.copy(out=ycatT[:, 2 + hg * 2:4 + hg * 2, p * 128:(p + 1) * 128],
                                                                                  in_=tbv[:, 0:256].rearrange("p (c f) -> p c f", c=2))))(p, tbv),
                                         reads=[bankb[tb]], writes=[B_ycat[2 + hg]])
                            P.close(at_bufs)
                        for _hg in range(2):
                            attn_group(_hg)
                            P.mark('attn%d' % _hg)
                    P.close(sa_bufs)

                    w_out_sb = msb("w_out_sb", [128, 8, D], BF16)
                    B_wout = P.buf("w_out", mx_bufs)
                    P.dma("pool", lambda g: g.dma_start(out=w_out_sb[:], in_=w_out.rearrange("(c p) f -> p c f", p=128)),
                          writes=[B_wout])

                    vecs = msb("vecs", [128, 5, D])
                    B_vecs = P.buf("vecs", mx_bufs)
                    P.dma("sync", lambda q: q.dma_start(out=vecs[:], in_=vec_bc.rearrange("v p d -> p v d")), writes=[B_vecs])
                    def layer_norm(src, bsrc, ldst, bdst, gi, vecs, B_vecs):
                        for hh in range(2):
                            P.op("dve", (lambda hh: (lambda v: v.bn_stats(out=stats[:, hh, :], in_=src[:, hh * 512:(hh + 1) * 512])))(hh),
                                 reads=[bsrc], writes=[B_st])
                        P.op("dve", lambda v: v.bn_aggr(out=mv[:], in_=stats[:].rearrange("p a b -> p (a b)")), writes=[B_st])
                        P.op("dve", lambda v: v.tensor_scalar(out=rstd1[:], in0=mv[:, 1:2], scalar1=EPS, scalar2=None, op0=ALU.add),
                             writes=[B_st])
                        P.op("act", lambda a: a.activation(out=rstd1[:], in_=rstd1[:], func=AF.Sqrt), reads=[B_st], writes=[B_st])
                        P.op("dve", lambda v: v.reciprocal(out=rstd1[:], in_=rstd1[:]), reads=[B_st], writes=[B_st])
                        P.op("dve", lambda v: v.tensor_scalar(out=src[:], in0=src[:], scalar1=mv[:, 0:1], scalar2=rstd1[:, 0:1],
                                                              op0=ALU.subtract, op1=ALU.mult), reads=[B_st], writes=[bsrc])
                        P.op("dve", lambda v: v.tensor_tensor(out=src[:], in0=src[:], in1=vecs[:, gi, :], op=ALU.mult),
                             reads=[B_vecs], writes=[bsrc])
                        P.op("dve", lambda v: v.tensor_tensor(out=ldst[:], in0=src[:], in1=vecs[:, gi + 1, :], op=ALU.add),
                             reads=[B_vecs, bsrc], writes=[bdst])

                    P.dma("sync", lambda q: q.dma_start(out=wr_sb[:], in_=w_router.rearrange("(c p) e -> p c e", p=128)), writes=[B_wr])

                    for p in range(NTO):
                        xi, bxi = xin[p % 2], B_xin[p % 2]
                        tt, btt = tmp_t[p % 2], B_tmp[p % 2]
                        x1, bx1 = x1t[p % 2], B_x1t[p % 2]
                        P.dma("sync", (lambda p, xi: (lambda q: q.dma_start(out=xi[:], in_=x_ext[HALO + p * 128:HALO + (p + 1) * 128, :])))(p, xi),
                              reads=src_reads, writes=[bxi])
                        P.op("dve", (lambda xi: (lambda v: v.scalar_tensor_tensor(out=xi[:], in0=xi[:], scalar=ALPHA, in1=vecs[:, 0, :],
                                                                                   op0=ALU.mult, op1=ALU.add)))(xi),
                             reads=[B_vecs], writes=[bxi])
                        for hh in range(2):
                            bk = (p % 2) * 2 + hh
                            for fch in range(8):
                                P.op("pe", (lambda p, hh, fch, bk: (lambda pe: pe.matmul(banks[bk][:], lhsT=ycatT[:, fch, p * 128:(p + 1) * 128],
                                                                                          rhs=w_out_sb[:, fch, hh * 512:(hh + 1) * 512],
                                                                                          start=(fch == 0), stop=(fch == 7))))(p, hh, fch, bk),
                                     reads=[B_wout] + B_ycat, writes=[bankb[bk]])
                            P.op("dve", (lambda hh, bk, xi, tt: (lambda v: v.tensor_tensor(out=tt[:, hh * 512:(hh + 1) * 512],
                                                                                            in0=banks[bk][:], in1=xi[:, hh * 512:(hh + 1) * 512],
                                                                                            op=ALU.add)))(hh, bk, xi, tt),
                                 reads=[bankb[bk], bxi], writes=[btt])
                        layer_norm(tt, btt, x1, bx1, 1, vecs, B_vecs)
                        if stage == "mix":
                            final_ops.append(P.dma("sync", (lambda p, x1: (lambda q: q.dma_start(out=dst[p * 128:(p + 1) * 128, :], in_=x1[:])))(p, x1),
                                                   reads=[bx1], writes=[B_dst], sembuf=bx1, indep=True))
                            continue
                        P.dma("sync", (lambda p, x1: (lambda q: q.dma_start(out=x1d[p * 128:(p + 1) * 128, :], in_=x1[:])))(p, x1),
                              reads=[bx1], writes=[B_x1d], sembuf=bx1, indep=True)
                        xb, bxb = x1b[p % 2], B_x1b[p % 2]
                        P.op("act", (lambda x1, xb: (lambda a: a.copy(out=xb[:], in_=x1[:])))(x1, xb), reads=[bx1], writes=[bxb])
                        for hh in range(2):
                            bk = 4 + hh
                            for c4 in range(4):
                                c = hh * 4 + c4
                                P.op("pe", (lambda x1, c, c4, bk: (lambda pe: pe.transpose(out=banks[bk][:, c4 * 128:(c4 + 1) * 128],
                                                                                            in_=x1[:, c * 128:(c + 1) * 128],
                                                                                            identity=ident_f[:])))(x1, c, c4, bk),
                                     reads=[bx1, B_const], writes=[bankb[bk]])
                            P.op("act", (lambda hh, bk: (lambda a: a.copy(out=x1T[:, hh * 4:(hh + 1) * 4, :],
                                                                          in_=banks[bk][:].rearrange("p (c f) -> p c f", c=4))))(hh, bk),
                                 reads=[bankb[bk]], writes=[B_x1T])
                        for c in range(8):
                            P.op("pe", (lambda c: (lambda pe: pe.matmul(banks[6][:, 0:NE], lhsT=x1T[:, c, :], rhs=wr_sb[:, c, :],
                                                                         start=(c == 0), stop=(c == 7))))(c),
                                 reads=[B_x1T, B_wr], writes=[bankb[6]])
                        P.op("dve", lambda v: v.tensor_tensor(out=logit[:], in0=banks[6][:, 0:NE], in1=brt_sb[:], op=ALU.add),
                             reads=[bankb[6], B_par], writes=[B_rt])
                        P.op("dve", lambda v: v.max(out=top8[:], in_=logit[:]), writes=[B_rt])
                        P.op("dve", lambda v: v.tensor_scalar(out=negv0[:], in0=top8[:, 0:1], scalar1=-1.0, scalar2=None, op0=ALU.mult),
                             writes=[B_rt])
                        P.op("act", lambda a: a.activation(out=ex4[:], in_=top8[:, 0:4], func=AF.Exp, bias=negv0[:, 0:1], scale=1.0),
                             reads=[B_rt], writes=[B_rt])
                        P.op("dve", lambda v: v.tensor_reduce(out=ssum[:], in_=ex4[:], axis=mybir.AxisListType.X, op=ALU.add),
                             reads=[B_rt], writes=[B_rt])
                        P.op("dve", lambda v: v.reciprocal(out=ssum[:], in_=ssum[:]), writes=[B_rt])
                        P.op("dve", (lambda p: (lambda v: v.tensor_scalar(out=gates[:, p, :], in0=ex4[:], scalar1=ssum[:, 0:1], scalar2=None,
                                                                          op0=ALU.mult)))(p), reads=[B_rt], writes=[B_gates])
                        P.op("dve", (lambda p: (lambda v: v.tensor_scalar(out=mask_all[:, p, :], in0=logit[:], scalar1=top8[:, 3:4],
                                                                          scalar2=tokv_sb[:, 0, p:p + 1], op0=ALU.is_ge,
                                                                          op1=ALU.mult)))(p), reads=[B_rt, B_par], writes=[B_mask])
                        for pp in range(p + 1):
                            P.op("pe", (lambda pp, p: (lambda pe: pe.matmul(banks[7][:, 0:NE],
                                                                             lhsT=(ustrict[:] if pp == p else ones_b[:]),
                                                                             rhs=mask_all[:, pp, :], start=(pp == 0), stop=(pp == p))))(pp, p),
                                 reads=[B_mask, B_const], writes=[bankb[7]])
                        P.op("dve", lambda v: v.tensor_scalar(out=ovf[:], in0=banks[7][:, 0:NE], scalar1=float(C), scalar2=1.0e6,
                                                              op0=ALU.is_ge, op1=ALU.mult), reads=[bankb[7]], writes=[B_rt])
                        P.op("dve", lambda v: v.tensor_tensor(out=Atab[:], in0=banks[7][:, 0:NE], in1=ec_sb[:], op=ALU.add),
                             reads=[bankb[7], B_par], writes=[B_rt])
                        P.op("dve", (lambda p: (lambda v: v.scalar_tensor_tensor(out=Atab[:], in0=Atab[:], scalar=tokv_sb[:, 1, p:p + 1],
                                                                                 in1=ovf[:], op0=ALU.add, op1=ALU.add)))(p),
                             reads=[B_par], writes=[B_rt])
                        for k in range(4):
                            P.op("dve", (lambda k: (lambda v: v.scalar_tensor_tensor(out=junk[:], in0=logit[:], scalar=top8[:, k:k + 1],
                                                                                      in1=Atab[:], op0=ALU.is_equal, op1=ALU.mult,
                                                                                      accum_out=slot_f[:, k:k + 1])))(k), writes=[B_rt])
                        P.op("dve", (lambda p: (lambda v: v.tensor_copy(out=slots[:, p, :], in_=slot_f[:])))(p), reads=[B_rt], writes=[B_slots])
                        for k in range(4):
                            P.dma("pool", (lambda p, k, xb: (lambda g: g.indirect_dma_start(
                                out=xs[:, :], out_offset=bass.IndirectOffsetOnAxis(ap=slots[:, p, k:k + 1], axis=0),
                                in_=xb[:, :], in_offset=None, bounds_check=P.env["bc_reg%d" % LI], oob_is_err=False)))(p, k, xb),
                                  reads=[bxb, B_slots, B_xs0], writes=[B_xs], sembuf=bxb, indep=True)

                    P.close(mx_bufs)
                with contextlib.ExitStack() as me:
                    me_bufs = []

                    def esb(name, shape, dt=F32):
                        return sb(name, shape, dt, st=me)

                    wg = [esb("wg%d" % i, [128, 8, D], BF16) for i in range(2)]
                    wu = [esb("wu%d" % i, [128, 8, D], BF16) for i in range(2)]
                    wd = [esb("wd%d" % i, [128, 8, D], BF16) for i in range(2)]
                    B_wg = [P.buf("wg0", me_bufs), P.buf("wg1", me_bufs)]
                    B_wu = [P.buf("wu0", me_bufs), P.buf("wu1", me_bufs)]
                    B_wd = [P.buf("wd0", me_bufs), P.buf("wd1", me_bufs)]
                    xe = [esb("xe%d" % i, [128, NS, D], BF16) for i in range(3)]
                    B_xe = [P.buf("xe%d" % i, me_bufs) for i in range(3)]
                    xeT = esb("xeT", [128, 8, C], BF16)
                    B_xeT = P.buf("xeT", me_bufs)
                    bd = [esb("bd%d" % i, [128, D]) for i in range(2)]
                    B_bd = [P.buf("bd0", me_bufs), P.buf("bd1", me_bufs)]
                    gc = [esb("gc%d" % i, [128, C]) for i in range(2)]
                    sgm = [esb("sgm%d" % i, [128, C]) for i in range(2)]
                    ub = [esb("ub%d" % i, [128, C]) for i in range(2)]
                    B_gc = [P.buf("gc0", me_bufs), P.buf("gc1", me_bufs)]
                    B_sgm = [P.buf("sgm0", me_bufs), P.buf("sgm1", me_bufs)]
                    B_ub = [P.buf("ub0", me_bufs), P.buf("ub1", me_bufs)]
                    actT = esb("actT", [128, 8, C], BF16)
                    B_act = [P.buf("act%d" % f, me_bufs) for f in range(8)]
                    yo = [esb("yo%d" % i, [128, D]) for i in range(2)]
                    B_yo = [P.buf("yo0", me_bufs), P.buf("yo1", me_bufs)]
                    B_ys = P.buf("ys")

                    def load_w(e):
                        i = e % 2
                        for (wdst, bdst, wsrc) in ((wg[i], B_wg[i], w_gate), (wu[i], B_wu[i], w_up), (wd[i], B_wd[i], w_down)):
                            P.dma("pool", (lambda wdst, wsrc, e: (lambda g: g.dma_start(out=wdst[:], in_=wsrc[e].rearrange("(c p) f -> p c f", p=128))))(wdst, wsrc, e),
                                  writes=[bdst])

                    def load_xe(e):
                        ix = e % 3
                        P.dma("sync", (lambda e, ix: (lambda q: q.dma_start(out=xe[ix][:], in_=xs[e * C:(e + 1) * C, :].rearrange("(s p) d -> p s d", p=128))))(e, ix),
                              reads=[B_xs], writes=[B_xe[ix]])

                    def load_bd(e):
                        i = e % 2
                        P.dma("sync", (lambda e, i: (lambda q: q.dma_start(out=bd[i][:], in_=b_down[e:e + 1, :].to_broadcast([128, D]))))(e, i),
                              writes=[B_bd[i]])

                    load_xe(0)
                    load_xe(1)
                    load_bd(0)
                    load_w(0)
                    for e in range(NE):
                        i = e % 2
                        if e + 1 < NE:
                            load_w(e + 1)
                            load_bd(e + 1)
                        if e + 2 < NE:
                            load_xe(e + 2)
                        ix = e % 3
                        for s in range(NS):
                            for hh in range(2):
                                bk = hh
                                tbv = banks[bk][:].bitcast(BF16)
                                for c4 in range(4):
                                    c = hh * 4 + c4
                                    P.op("pe", (lambda ix, s, c, c4, tbv: (lambda pe: pe.transpose(out=tbv[:, c4 * 128:(c4 + 1) * 128],
                                                                                                    in_=xe[ix][:, s, c * 128:(c + 1) * 128],
                                                                                                    identity=ident_b[:])))(ix, s, c, c4, tbv),
                                         reads=[B_xe[ix], B_const], writes=[bankb[bk]])
                                if hh == 0:
                                    P.op("act", (lambda s, hh, tbv: (lambda a: a.copy(out=xeT[:, hh * 4:(hh + 1) * 4, s * 128:(s + 1) * 128],
                                                                                      in_=tbv[:, 0:512].rearrange("p (c f) -> p c f", c=4))))(s, hh, tbv),
                                         reads=[bankb[bk]], writes=[B_xeT])
                                else:
                                    P.op("dve", (lambda s, hh, tbv: (lambda v: v.tensor_copy(out=xeT[:, hh * 4:(hh + 1) * 4, s * 128:(s + 1) * 128],
                                                                                             in_=tbv[:, 0:512].rearrange("p (c f) -> p c f", c=4))))(s, hh, tbv),
                                         reads=[bankb[bk]], writes=[B_xeT])
                        for f in range(8):
                            bg, bu = 2 + (f % 2) * 2, 3 + (f % 2) * 2
                            k2 = f % 2
                            for dch in range(8):
                                P.op("pe", (lambda i, f, dch, bg: (lambda pe: pe.matmul(banks[bg][:, 0:C], lhsT=wg[i][:, dch, f * 128:(f + 1) * 128],
                                                                                         rhs=xeT[:, dch, :], start=(dch == 0), stop=(dch == 7))))(i, f, dch, bg),
                                     reads=[B_wg[i], B_xeT], writes=[bankb[bg]])
                            for dch in range(8):
                                P.op("pe", (lambda i, f, dch, bu: (lambda pe: pe.matmul(banks[bu][:, 0:C], lhsT=wu[i][:, dch, f * 128:(f + 1) * 128],
                                                                                         rhs=xeT[:, dch, :], start=(dch == 0), stop=(dch == 7))))(i, f, dch, bu),
                                     reads=[B_wu[i], B_xeT], writes=[bankb[bu]])
                            P.op("dve", (lambda e, f, bg, k2: (lambda v: v.tensor_scalar(out=gc[k2][:], in0=banks[bg][:, 0:C], scalar1=bg_sb[:, e, f:f + 1],
                                                                                          scalar2=7.0, op0=ALU.add, op1=ALU.min)))(e, f, bg, k2),
                                 reads=[bankb[bg], B_par], writes=[B_gc[k2]])
                            P.op("act", (lambda e, f, bu, k2: (lambda a: a.activation(out=ub[k2][:], in_=banks[bu][:, 0:C], func=AF.Identity,
                                                                                       bias=bu1_sb[:, e, f:f + 1], scale=1.0)))(e, f, bu, k2),
                                 reads=[bankb[bu], B_bu1], writes=[B_ub[k2]])
                            P.op("act", (lambda k2: (lambda a: a.activation(out=sgm[k2][:], in_=gc[k2][:], func=AF.Sigmoid, scale=1.702)))(k2),
                                 reads=[B_gc[k2]], writes=[B_sgm[k2]])
                            P.op("dve", (lambda k2: (lambda v: v.tensor_scalar(out=ub[k2][:], in0=ub[k2][:], scalar1=8.0, scalar2=-6.0,
                                                                               op0=ALU.min, op1=ALU.max)))(k2), reads=[B_ub[k2]], writes=[B_ub[k2]])
                            P.op("dve", (lambda k2: (lambda v: v.tensor_tensor(out=gc[k2][:], in0=gc[k2][:], in1=sgm[k2][:], op=ALU.mult)))(k2),
                                 reads=[B_sgm[k2]], writes=[B_gc[k2]])
                            P.op("pool", (lambda f, k2: (lambda g: g.tensor_tensor(out=actT[:, f, :], in0=ub[k2][:], in1=gc[k2][:],
                                                                                   op=ALU.mult)))(f, k2),
                                 reads=[B_ub[k2], B_gc[k2]], writes=[B_act[f]])
                        for s in range(NS):
                            yk = (e * NS + s) % 2
                            for hh in range(2):
                                bk = 6 + hh
                                for f in range(8):
                                    P.op("pe", (lambda i, s, hh, f, bk: (lambda pe: pe.matmul(banks[bk][:], lhsT=actT[:, f, s * 128:(s + 1) * 128],
                                                                                               rhs=wd[i][:, f, hh * 512:(hh + 1) * 512],
                                                                                               start=(f == 0), stop=(f == 7))))(i, s, hh, f, bk),
                                         reads=[B_wd[i]] + B_act, writes=[bankb[bk]])
                                P.op("dve", (lambda i, yk, hh, bk: (lambda v: v.tensor_tensor(out=yo[yk][:, hh * 512:(hh + 1) * 512], in0=banks[bk][:],
                                                                                               in1=bd[i][:, hh * 512:(hh + 1) * 512], op=ALU.add)))(i, yk, hh, bk),
                                     reads=[bankb[bk], B_bd[i]], writes=[B_yo[yk]], indep=(hh == 1))
                            P.dma("sync", (lambda e, s, yk: (lambda q: q.dma_start(out=ys[e * C + s * 128:e * C + (s + 1) * 128, :], in_=yo[yk][:])))(e, s, yk),
                                  reads=[B_yo[yk]], writes=[B_ys], sembuf=B_yo[yk], indep=True)
                    P.close(me_bufs)

                with contextlib.ExitStack() as me:
                    cb_bufs = []

                    def esb(name, shape, dt=F32):
                        return sb(name, shape, dt, st=me)

                    vecs2 = esb("vecs2", [128, 5, D])
                    B_vecs2 = P.buf("vecs2", cb_bufs)
                    P.dma("sync", lambda q: q.dma_start(out=vecs2[:], in_=vec_bc.rearrange("v p d -> p v d")), writes=[B_vecs2])
                    gth = [[esb("gth%d_%d" % (i, k), [128, D]) for k in range(4)] for i in range(2)]
                    B_gth = [[P.buf("gth%d_%d" % (i, k), cb_bufs) for k in range(4)] for i in range(2)]
                    for i in range(2):
                        for k in range(4):
                            P.op("pool", (lambda i, k: (lambda g: g.memset(gth[i][k][:], 0.0)))(i, k), writes=[B_gth[i][k]])
                    for p in range(NTO):
                        i = p % 2
                        x1, bx1 = x1t[i], B_x1t[i]
                        tt, btt = tmp_t[i], B_tmp[i]
                        P.dma("sync", (lambda p, x1: (lambda q: q.dma_start(out=x1[:], in_=x1d[p * 128:(p + 1) * 128, :])))(p, x1),
                              reads=[B_x1d], writes=[bx1])
                        for k in range(4):
                            P.dma("pool", (lambda p, k, i: (lambda g: g.indirect_dma_start(
                                out=gth[i][k][:, :], out_offset=None, in_=ys[:, :],
                                in_offset=bass.IndirectOffsetOnAxis(ap=slots[:, p, k:k + 1], axis=0),
                                bounds_check=P.env["bc_reg%d" % LI], oob_is_err=False)))(p, k, i),
                                  reads=[B_ys, B_slots], writes=[B_gth[i][k]])
                        P.op("dve", (lambda x1, tt: (lambda v: v.tensor_scalar(out=tt[:], in0=x1[:], scalar1=ALPHA, scalar2=None, op0=ALU.mult)))(x1, tt),
                             reads=[bx1], writes=[btt])
                        for k in range(4):
                            P.op("dve", (lambda p, k, i, tt: (lambda v: v.scalar_tensor_tensor(out=tt[:], in0=gth[i][k][:], scalar=gates[:, p, k:k + 1],
                                                                                                in1=tt[:], op0=ALU.mult, op1=ALU.add)))(p, k, i, tt),
                                 reads=[B_gth[i][k], B_gates], writes=[btt])
                        layer_norm(tt, btt, x1, bx1, 3, vecs2, B_vecs2)
                        final_ops.append(P.dma("sync", (lambda p, x1: (lambda q: q.dma_start(out=dst[p * 128:(p + 1) * 128, :], in_=x1[:])))(p, x1),
                                               reads=[bx1], writes=[B_dst], sembuf=bx1, indep=True))
                    P.close(cb_bufs)
                P.close(top_bufs)

        x_ext0 = nc.dram_tensor("x_ext", [S_OWN + 4 * HALO, D], F32, kind="ExternalInput").ap()
        y_out = nc.dram_tensor("y_out", [S_OWN, D], F32, kind="ExternalOutput").ap()
        xmid = nc.dram_tensor("xmid", [S_OWN + 2 * HALO, D], F32, kind="Internal").ap()
        B_xmid = Buf("xmid")
        B_yout = Buf("y_out")
        emit_layer(0, S_OWN + 2 * HALO, 512, True, x_ext0, [], xmid, B_xmid)
        emit_layer(1, S_OWN, 384, False, xmid, [B_xmid], y_out, B_yout)
        P.emit(final_ops)
    return nc


def _static_tables(n_out, seg, ext, nseg_rows=128):
    NTO = n_out // 128
    t0 = seg * S_OWN - (HALO if ext else 0)
    r_base = t0 // GRID_W
    rows = nseg_rows
    S = rows * GRID_W
    rowbias = np.zeros((128, NTO, 7, 2), np.float32)
    for p in range(NTO):
        for j in range(7):
            for kr2 in range(2):
                kr = r_base + 2 * p - 6 + 2 * j + kr2
                for rr in range(2):
                    r = r_base + 2 * p + rr
                    sr = min(max(r - 4, 0), rows - 8)
                    ok = (0 <= kr < rows) and (sr <= kr < sr + 8)
                    if not ok:
                        rowbias[kr2 * 64:(kr2 + 1) * 64, p, j, rr] = NEG
    n_in = n_out + 2 * HALO
    x0 = t0 - HALO
    if ext:
        regs = ((0, HALO), (HALO, 2 * HALO), (n_in - 2 * HALO, n_in - HALO), (n_in - HALO, n_in))
    else:
        regs = ((0, HALO), (0, 0), (0, 0), (n_in - HALO, n_in))
    flags = np.ones((128, 4), np.float32)
    for i, (a, b) in enumerate(regs):
        if b > a and (x0 + a < 0 or x0 + b > S):
            flags[:, i] = 0.0
    poolcorr = np.ones((128, 2, 16), np.float32)
    wins = (2, 4, 8, 16)
    cp0, cp1 = (HALO, n_out - HALO - 8) if ext else (0, n_out - 8)
    for g, w in enumerate(wins):
        pc, half = g // 2, g % 2
        for i in range(8):
            for (pos, t) in ((i, t0 + cp0 + i), (8 + i, t0 + cp1 + i)):
                lo = min(max(t - w // 2, 0), S)
                hi = min(max(t + w // 2, 0), S)
                if hi > lo:
                    poolcorr[half * 64:(half + 1) * 64, pc, pos] = np.float32(w) / np.float32(hi - lo)
    tok = t0 + np.arange(NTO)[None, :] * 128 + np.arange(128)[:, None]
    ok = (tok >= 0) & (tok < S)
    tokval = np.stack([ok.astype(np.float32), np.where(ok, 0.0, 1.0e6).astype(np.float32)], 1)
    return rowbias.reshape(128, NTO * 14), flags, poolcorr, np.ascontiguousarray(tokval)


def _bias_index():
    j = np.arange(7)[:, None, None]
    key = np.arange(128)[None, :, None]
    q = np.arange(128)[None, None, :]
    kr2, kc = key // 64, key % 64
    rr, c = q // 64, q % 64
    dr = (2 * j + kr2 - 6) - rr
    dc = kc - c
    sc = np.clip(c - 8, 0, GRID_W - 16)
    valid = (kc >= sc) & (kc < sc + 16) & (np.abs(dr) <= 7) & (np.abs(dc) <= 15)
    ri = np.clip(dr + 7, 0, 14)
    ci = np.clip(dc + 15, 0, 30)
    ri, ci, valid = np.broadcast_arrays(ri, ci, valid)
    return ri, ci, valid


_PROG_CACHE = {}
LAYER_CFG = ((S_OWN + 2 * HALO, 512, True), (S_OWN, 384, False))


def _get_prog():
    if "full" not in _PROG_CACHE:
        _PROG_CACHE["full"] = build_program()
    return _PROG_CACHE["full"]


def _layer_common(l, P, C):
    f32 = np.float32
    ri, ci, valid = _bias_index()
    rpb = P["rpb"][l]
    biasT = np.where(valid[None], rpb[:, ri, ci], f32(NEG)).astype(f32)
    w_pool = P["w_pool"][l]
    wpool_bd = np.zeros((2, 128, 128), f32)
    for g in range(4):
        pc, half = g // 2, g % 2
        wpool_bd[pc, half * 64:(half + 1) * 64, half * 64:(half + 1) * 64] = w_pool[g]
    return {
        "w_in": np.ascontiguousarray(P["w_in"][l]),
        "b_in_pc": np.ascontiguousarray(P["b_in"][l].reshape(18, 128).T),
        "bv_bc": np.ascontiguousarray(np.broadcast_to(P["b_in"][l][1280:1792], (128, 512))),
        "wpool_bd": wpool_bd,
        "pool_scale_pc": np.ascontiguousarray(P["pool_scale"][l].reshape(2, 128).T),
        "biasT": biasT,
        "conv_dw_pc": np.ascontiguousarray(P["conv_dw"][l][:, 0, :].reshape(31, 2, 128).transpose(2, 1, 0)),
        "conv_vec_pc": np.ascontiguousarray(np.stack([P["conv_dw_b"][l], P["conv_ln_g"][l], P["conv_ln_b"][l],
                                                      P["b_conv_pw"][l]], 0).reshape(4, 2, 128).transpose(2, 0, 1)),
        "w_pw": np.ascontiguousarray(P["w_conv_pw"][l]),
        "w_out": np.ascontiguousarray(P["w_out"][l]),
        "vec_bc": np.ascontiguousarray(np.broadcast_to(
            np.stack([P["b_out"][l], P["ln1_g"][l], P["ln1_b"][l], P["ln2_g"][l], P["ln2_b"][l]], 0)[:, None, :], (5, 128, D))),
        "w_router": np.ascontiguousarray(P["w_router"][l]),
        "b_router_bc": np.ascontiguousarray(np.broadcast_to(P["b_router"][l], (128, NE))),
        "ec_bc": np.ascontiguousarray(np.broadcast_to((np.arange(NE) * C).astype(f32), (128, NE))),
        "b_gate_pc": np.ascontiguousarray(P["b_gate"][l].reshape(NE, 8, 128).transpose(2, 0, 1)),
        "b_up_pc": np.ascontiguousarray(P["b_up"][l].reshape(NE, 8, 128).transpose(2, 0, 1)),
        "w_gate": np.ascontiguousarray(P["w_gate"][l]),
        "w_up": np.ascontiguousarray(P["w_up"][l]),
        "w_down": np.ascontiguousarray(P["w_down"][l]),
        "b_down": np.ascontiguousarray(P["b_down"][l]),
    }


def _in_maps(P):
    f32 = np.float32
    x_full = P["x"]
    B, S, _ = x_full.shape
    nseg = S // S_OWN
    shared = {}
    for l, (n_out, C, ext) in enumerate(LAYER_CFG):
        for k, v in _layer_common(l, P, C).items():
            shared["%s_%d" % (k, l)] = v
    in_maps = []
    for core in range(8):
        b, seg = core // nseg, core % nseg
        t0 = seg * S_OWN
        m = dict(shared)
        xe = np.zeros((S_OWN + 4 * HALO, D), f32)
        lo, hi = max(t0 - 2 * HALO, 0), min(t0 + S_OWN + 2 * HALO, S)
        xe[lo - (t0 - 2 * HALO):hi - (t0 - 2 * HALO)] = x_full[b, lo:hi]
        m["x_ext"] = xe
        for l, (n_out, C, ext) in enumerate(LAYER_CFG):
            rowbias, flags, poolcorr, tokval = _static_tables(n_out, seg, ext)
            m["tokval_%d" % l] = tokval
            m["rowbias_%d" % l] = rowbias
            m["flags_%d" % l] = flags
            m["poolcorr_%d" % l] = poolcorr
        in_maps.append(m)
    return in_maps


def kernel(**inputs):
    P = {k: np.asarray(v, dtype=np.float32) for k, v in inputs.items()}
    x = P["x"]
    B, S, _ = x.shape
    nc = _get_prog()
    res = run_bass_kernel_spmd(nc, _in_maps(P), core_ids=list(range(8)))
    out = np.empty_like(x)
    nseg = S // S_OWN
    for core in range(8):
        b, seg = core // nseg, core % nseg
        out[b, seg * S_OWN:(seg + 1) * S_OWN] = res.results[core]["y_out"]
    return out
```
